# Optimizing a Trainium2 kernel written in Bass

```python
import math
import jax, jax.numpy as jnp
from jax import lax
import numpy as np

D_MODEL = 1024
BATCH = 8
SEQ = 4096
DEPTH = 1

DA_HEADS = 8
DA_HEAD_DIM = 64
DA_V_DIM = 2 * DA_HEAD_DIM
DA_ROT_DIM = DA_HEAD_DIM // 4
MLA_HEADS = 8
MLA_Q_LORA = 384
MLA_KV_LORA = 256
MLA_NOPE = 128
MLA_ROPE = 64
MLA_V = 128
ROPE_THETA = 500000.0
N_GROUPS = 4
EXPERTS_PER_GROUP = 8
N_EXPERTS = N_GROUPS * EXPERTS_PER_GROUP
TOP_K = 2
D_EXPERT = 512
MOE_BLOCK = 128
Q_BLOCK = 128
NORM_EPS = 1e-6

DA_Q_COLS = DA_HEADS * 2 * DA_HEAD_DIM
DA_K_COLS = DA_HEADS * 2 * DA_HEAD_DIM
DA_V_COLS = DA_HEADS * DA_V_DIM
GATE_COLS = 2 * D_MODEL
IN_COLS = DA_Q_COLS + DA_K_COLS + DA_V_COLS + MLA_Q_LORA + MLA_KV_LORA + MLA_ROPE + GATE_COLS
SPLIT_1 = DA_Q_COLS
SPLIT_2 = SPLIT_1 + DA_K_COLS
SPLIT_3 = SPLIT_2 + DA_V_COLS
SPLIT_4 = SPLIT_3 + MLA_Q_LORA
SPLIT_5 = SPLIT_4 + MLA_KV_LORA
SPLIT_6 = SPLIT_5 + MLA_ROPE

kernel_name = 'hybrid_diffattn_mla_hmoe_block'


def rms_norm(x, g):
    xf = x.astype(jnp.float32)
    y = xf * lax.rsqrt(jnp.mean(xf * xf, axis=-1, keepdims=True) + NORM_EPS)
    return (y * g.astype(jnp.float32)).astype(x.dtype)


def rope_tables(positions, dim):
    inv = ROPE_THETA ** (-jnp.arange(0, dim, 2, dtype=jnp.float32) / dim)
    ang = positions.astype(jnp.float32)[..., None] * inv
    return jnp.cos(ang), jnp.sin(ang)


def apply_rope(x, cos, sin):
    x1, x2 = jnp.split(x, 2, axis=-1)
    c = cos[:, :, None, :]
    s = sin[:, :, None, :]
    return jnp.concatenate([x1 * c - x2 * s, x2 * c + x1 * s], axis=-1).astype(x.dtype)


def partial_rope(x, cos, sin, rot_dim):
    return jnp.concatenate([apply_rope(x[..., :rot_dim], cos, sin), x[..., rot_dim:]], axis=-1)


def causal_block_attention(q_maps, k_maps, coefs, v, scale):
    S = v.shape[1]
    n_blocks = S // Q_BLOCK
    outs = []
    for i in range(n_blocks):
        q0, q1 = i * Q_BLOCK, (i + 1) * Q_BLOCK
        mask = jnp.arange(q1)[None, :] <= jnp.arange(q0, q1)[:, None]
        w = None
        for q, k, c in zip(q_maps, k_maps, coefs):
            s = jnp.einsum('bqhd,bkhd->bhqk', q[:, q0:q1], k[:, :q1],
                           preferred_element_type=jnp.float32) * scale
            p = jax.nn.softmax(jnp.where(mask, s, -jnp.inf), axis=-1) * c
            w = p if w is None else w + p
        outs.append(jnp.einsum('bhqk,bkhd->bqhd', w.astype(v.dtype), v[:, :q1]))
    return jnp.concatenate(outs, axis=1)


def diff_attention(q, k, v, cos, sin, q_g, k_g, lq1, lk1, lq2, lk2, subln_g, lambda_init):
    B, S = q.shape[:2]
    q = partial_rope(rms_norm(q.reshape(B, S, 2 * DA_HEADS, DA_HEAD_DIM), q_g), cos, sin, DA_ROT_DIM)
    k = partial_rope(rms_norm(k.reshape(B, S, 2 * DA_HEADS, DA_HEAD_DIM), k_g), cos, sin, DA_ROT_DIM)
    v = v.reshape(B, S, DA_HEADS, DA_V_DIM)
    f32 = jnp.float32
    lam = (jnp.exp(jnp.sum(lq1.astype(f32) * lk1.astype(f32)))
           - jnp.exp(jnp.sum(lq2.astype(f32) * lk2.astype(f32))) + lambda_init)
    o = causal_block_attention([q[:, :, 0::2], q[:, :, 1::2]], [k[:, :, 0::2], k[:, :, 1::2]],
                               [1.0, -lam], v, DA_HEAD_DIM ** -0.5)
    o = rms_norm(o, subln_g) * (1.0 - lambda_init)
    return o.reshape(B, S, DA_HEADS * DA_V_DIM)


def mla_attention(c_q, c_kv, k_rope, cos, sin, q_lora_g, w_uq, kv_lora_g, w_ukv, q_g, k_nope_g, k_rope_g):
    B, S = c_q.shape[:2]
    q = (rms_norm(c_q, q_lora_g) @ w_uq).reshape(B, S, MLA_HEADS, MLA_NOPE + MLA_ROPE)
    kv = (rms_norm(c_kv, kv_lora_g) @ w_ukv).reshape(B, S, MLA_HEADS, MLA_NOPE + MLA_V)
    k_nope, v = kv[..., :MLA_NOPE], kv[..., MLA_NOPE:]
    q = rms_norm(q, q_g)
    q = jnp.concatenate([q[..., :MLA_NOPE], apply_rope(q[..., MLA_NOPE:], cos, sin)], axis=-1)
    k_nope = rms_norm(k_nope, k_nope_g)
    k_r = apply_rope(rms_norm(k_rope, k_rope_g)[:, :, None, :], cos, sin)
    k = jnp.concatenate([k_nope, jnp.broadcast_to(k_r, (B, S, MLA_HEADS, MLA_ROPE))], axis=-1)
    o = causal_block_attention([q], [k], [1.0], v, (MLA_NOPE + MLA_ROPE) ** -0.5)
    return o.reshape(B, S, MLA_HEADS * MLA_V)


def hier_moe(h, w_group, b_group, w_router, b_router, w1, w3, w2):
    B, S, D = h.shape
    T = B * S
    hf = h.reshape(T, D)
    f32 = jnp.float32
    glog = (hf @ w_group).astype(f32) + b_group.astype(f32)
    g_sel = jnp.argmax(glog, axis=-1)
    p_g = jnp.take_along_axis(jax.nn.softmax(glog, axis=-1), g_sel[:, None], axis=-1)
    elog = ((hf @ w_router).astype(f32) + b_router.astype(f32)).reshape(T, N_GROUPS, EXPERTS_PER_GROUP)
    elog = jnp.take_along_axis(elog, g_sel[:, None, None], axis=1)[:, 0]
    top_v, top_i = lax.top_k(elog, TOP_K)
    weight = p_g * jax.nn.softmax(top_v, axis=-1)
    expert = g_sel[:, None] * EXPERTS_PER_GROUP + top_i
    TK = T * TOP_K
    flat_e = expert.reshape(TK)
    flat_tok = jnp.repeat(jnp.arange(T, dtype=jnp.int32), TOP_K)
    order = jnp.argsort(flat_e)
    se, stok, sw = flat_e[order], flat_tok[order], weight.reshape(TK)[order]
    counts = jnp.bincount(flat_e, length=N_EXPERTS)
    starts = jnp.cumsum(counts) - counts
    padded = (counts + MOE_BLOCK - 1) // MOE_BLOCK * MOE_BLOCK
    pends = jnp.cumsum(padded)
    pstarts = pends - padded
    dest = pstarts[se] + (jnp.arange(TK) - starts[se])
    n_chunks = -(-TK // MOE_BLOCK) + N_EXPERTS
    P = n_chunks * MOE_BLOCK
    row_tok = jnp.full((P,), T, dtype=jnp.int32).at[dest].set(stok)
    x_pad = jnp.concatenate([hf, jnp.zeros((1, D), hf.dtype)], axis=0)
    xb = x_pad[row_tok].reshape(n_chunks, MOE_BLOCK, D)
    chunk_e = jnp.minimum(jnp.searchsorted(pends, jnp.arange(n_chunks) * MOE_BLOCK, side='right'),
                          N_EXPERTS - 1)

    def expert_ffn(args):
        xc, e = args
        return (jax.nn.silu(xc @ w1[e]) * (xc @ w3[e])) @ w2[e]

    yb = lax.map(expert_ffn, (xb, chunk_e)).reshape(P, D)
    y = (yb[dest].astype(f32) * sw[:, None]).astype(h.dtype)
    out = jax.ops.segment_sum(y, stok, num_segments=T)
    return out.reshape(B, S, D)


def setup_inputs(seed: int = 0) -> dict:
    key = jax.random.key(seed)
    ks = jax.random.split(key, 32)
    L, D = DEPTH, D_MODEL
    f32 = jnp.float32

    def nrm(k, shape, scale):
        return jax.random.normal(k, shape, f32) * scale

    def gain(k, shape):
        return 1.0 + 0.02 * jax.random.normal(k, shape, f32)

    x = jax.random.normal(ks[0], (BATCH, SEQ, D), f32)
    offsets = jax.random.randint(ks[1], (BATCH, 1), 0, 2048, dtype=jnp.int32)
    positions = offsets + jnp.arange(SEQ, dtype=jnp.int32)[None, :]
    return {
        'x': x,
        'positions': positions,
        'attn_norm_g': gain(ks[2], (L, D)),
        'w_in': nrm(ks[3], (L, D, IN_COLS), D ** -0.5),
        'b_gate': nrm(ks[4], (L, 2, D), 0.02),
        'da_q_norm_g': gain(ks[5], (L, DA_HEAD_DIM)),
        'da_k_norm_g': gain(ks[6], (L, DA_HEAD_DIM)),
        'da_lambda_q1': nrm(ks[7], (L, DA_HEAD_DIM), 0.1),
        'da_lambda_k1': nrm(ks[8], (L, DA_HEAD_DIM), 0.1),
        'da_lambda_q2': nrm(ks[9], (L, DA_HEAD_DIM), 0.1),
        'da_lambda_k2': nrm(ks[10], (L, DA_HEAD_DIM), 0.1),
        'da_subln_g': gain(ks[11], (L, DA_V_DIM)),
        'mla_q_lora_g': gain(ks[12], (L, MLA_Q_LORA)),
        'mla_w_uq': nrm(ks[13], (L, MLA_Q_LORA, MLA_HEADS * (MLA_NOPE + MLA_ROPE)), MLA_Q_LORA ** -0.5),
        'mla_kv_lora_g': gain(ks[14], (L, MLA_KV_LORA)),
        'mla_w_ukv': nrm(ks[15], (L, MLA_KV_LORA, MLA_HEADS * (MLA_NOPE + MLA_V)), MLA_KV_LORA ** -0.5),
        'mla_q_norm_g': gain(ks[16], (L, MLA_NOPE + MLA_ROPE)),
        'mla_k_nope_norm_g': gain(ks[17], (L, MLA_NOPE)),
        'mla_k_rope_norm_g': gain(ks[18], (L, MLA_ROPE)),
        'w_o': nrm(ks[19], (L, D, D), D ** -0.5),
        'ffn_norm_g': gain(ks[20], (L, D)),
        'w_group': nrm(ks[21], (L, D, N_GROUPS), D ** -0.5),
        'b_group': nrm(ks[22], (L, N_GROUPS), 0.01),
        'w_router': nrm(ks[23], (L, D, N_EXPERTS), D ** -0.5),
        'b_router': nrm(ks[24], (L, N_EXPERTS), 0.01),
        'w1': nrm(ks[25], (L, N_EXPERTS, D, D_EXPERT), D ** -0.5),
        'w3': nrm(ks[26], (L, N_EXPERTS, D, D_EXPERT), D ** -0.5),
        'w2': nrm(ks[27], (L, N_EXPERTS, D_EXPERT, D), D_EXPERT ** -0.5),
    }


def reference(x, positions, attn_norm_g, w_in, b_gate, da_q_norm_g, da_k_norm_g,
              da_lambda_q1, da_lambda_k1, da_lambda_q2, da_lambda_k2, da_subln_g,
              mla_q_lora_g, mla_w_uq, mla_kv_lora_g, mla_w_ukv, mla_q_norm_g,
              mla_k_nope_norm_g, mla_k_rope_norm_g, w_o, ffn_norm_g, w_group, b_group,
              w_router, b_router, w1, w3, w2):
    B, S, D = x.shape
    da_cos, da_sin = rope_tables(positions, DA_ROT_DIM)
    mla_cos, mla_sin = rope_tables(positions, MLA_ROPE)
    for l in range(DEPTH):
        lambda_init = 0.8 - 0.6 * math.exp(-0.3 * l)
        h = rms_norm(x, attn_norm_g[l])
        proj = h @ w_in[l]
        da_q, da_k, da_v, c_q, c_kv, k_rope, gate_logits = jnp.split(
            proj, [SPLIT_1, SPLIT_2, SPLIT_3, SPLIT_4, SPLIT_5, SPLIT_6], axis=-1)
        o_da = diff_attention(da_q, da_k, da_v, da_cos, da_sin, da_q_norm_g[l], da_k_norm_g[l],
                              da_lambda_q1[l], da_lambda_k1[l], da_lambda_q2[l], da_lambda_k2[l],
                              da_subln_g[l], lambda_init)
        o_mla = mla_attention(c_q, c_kv, k_rope, mla_cos, mla_sin, mla_q_lora_g[l], mla_w_uq[l],
                              mla_kv_lora_g[l], mla_w_ukv[l], mla_q_norm_g[l],
                              mla_k_nope_norm_g[l], mla_k_rope_norm_g[l])
        gates = jax.nn.sigmoid(gate_logits.astype(jnp.float32).reshape(B, S, 2, D)
                               + b_gate[l].astype(jnp.float32))
        mixed = (gates[:, :, 0] * o_da.astype(jnp.float32)
                 + gates[:, :, 1] * o_mla.astype(jnp.float32)).astype(x.dtype)
        x = x + mixed @ w_o[l]
        h2 = rms_norm(x, ffn_norm_g[l])
        x = x + hier_moe(h2, w_group[l], b_group[l], w_router[l], b_router[l], w1[l], w3[l], w2[l])
    return x
```

```python
import math
import contextlib
import numpy as np
import concourse.bass as bass
import concourse.mybir as mybir
from concourse.bass_utils import run_bass_kernel_spmd

F32 = mybir.dt.float32
BF16 = mybir.dt.bfloat16
I32 = mybir.dt.int32
AF = mybir.ActivationFunctionType
ALU = mybir.AluOpType
AX = mybir.AxisListType

D = 1024
NCORES = 8
IN_COLS = 5824
C_CQ, C_CKV, C_KR, C_GATE = 3072, 3456, 3712, 3776
EPS = 1e-6
THETA = 500000.0
LAMBDA_INIT = 0.8 - 0.6 * math.exp(0.0)
NE = 32
DE = 512
MAGIC = 12582912.0
NEG = -30000.0


class Buf:
    def __init__(self, name, multi=False):
        self.name = name
        self.w = {}
        self.rs = {}
        self.multi = multi
        self.dsem = None
        self.dcnt = 0


class Tracker:
    def __init__(self, nc, stack):
        self.nc = nc
        self.stack = stack
        self.engs = {"pe": nc.tensor, "act": nc.scalar, "dve": nc.vector, "pool": nc.gpsimd, "sp": nc.sync}
        self.sem = {}
        self.cnt = {}
        self.nsem = 0
        self.waited = {}
        self.last = {}
        self.dbufs = []
        for k in self.engs:
            self._newsem(k)

    def _mksem(self, name):
        self.nsem += 1
        return self.stack.enter_context(self.nc.semaphore(f"{name}_{self.nsem}"))

    def _newsem(self, k):
        self.sem[k] = (self._mksem("e" + k), self.nsem)
        self.cnt[k] = 0

    def _wait(self, eng, ev):
        sem, sid, val, src, buf = ev
        if src == "dma":
            val = 16 * buf.dcnt
        elif src == eng and eng == "pe":
            return
        key = (eng, sid)
        if self.waited.get(key, 0) >= val:
            return
        self.waited[key] = val
        self.engs[eng].wait_ge(sem, val)

    def deps(self, eng, reads, writes):
        for b in reads:
            for ev in b.w.values():
                self._wait(eng, ev)
        for b in writes:
            if b.multi:
                continue
            for ev in b.w.values():
                if getattr(b, "par", False) and ev[3] == "dma":
                    continue
                self._wait(eng, ev)
            for ev in b.rs.values():
                self._wait(eng, ev)

    def _record(self, ev, key, reads, writes):
        for b in reads:
            b.rs[key] = ev
        for b in writes:
            if b.multi or getattr(b, "par", False):
                b.w[key] = ev
            else:
                b.w = {key: ev}
                b.rs = {}

    def op(self, eng, fn, reads=(), writes=()):
        self.deps(eng, reads, writes)
        if self.cnt[eng] >= 30000:
            self._newsem(eng)
        self.cnt[eng] += 1
        sem, sid = self.sem[eng]
        ev = (sem, sid, self.cnt[eng], eng, None)
        fn(self.engs[eng]).then_inc(sem, 1)
        self.last[eng] = ev
        self._record(ev, eng, reads, writes)
        return ev

    def mm(self, fns, reads=(), writes=()):
        self.deps("pe", reads, writes)
        for f in fns[:-1]:
            f(self.nc.tensor)
        return self.op("pe", fns[-1], reads, writes)

    def dma(self, q, out, in_, reads=(), writes=(), owner=None, **kw):
        self.deps(q, reads, writes)
        b = owner
        if b.dsem is None:
            b.dsem = (self._mksem("d"), self.nsem)
            self.dbufs.append(b)
        b.dcnt += 1
        sem, sid = b.dsem
        ev = (sem, sid, 16 * b.dcnt, "dma", b)
        self.engs[q].dma_start(out=out, in_=in_, **kw).then_inc(sem, 16)
        self._record(ev, ("dma", sid), reads, writes)
        return ev

    def idma(self, out, out_offset, in_, in_offset, reads=(), writes=(), owner=None, **kw):
        q = "pool"
        self.deps(q, reads, writes)
        b = owner
        if b.dsem is None:
            b.dsem = (self._mksem("d"), self.nsem)
            self.dbufs.append(b)
        b.dcnt += 1
        sem, sid = b.dsem
        ev = (sem, sid, 16 * b.dcnt, "dma", b)
        self.nc.gpsimd.indirect_dma_start(out=out, out_offset=out_offset, in_=in_, in_offset=in_offset, **kw).then_inc(sem, 16)
        self._record(ev, ("dma", sid), reads, writes)
        return ev

    def barrier(self):
        evs = list(self.last.values())
        for e in self.engs:
            for ev in evs:
                self._wait(e, ev)
            for b in self.dbufs:
                self._wait(e, (b.dsem[0], b.dsem[1], 0, "dma", b))


class Ring:
    def __init__(self, aps, name):
        self.items = [(ap, Buf(f"{name}{i}")) for i, ap in enumerate(aps)]
        self.i = 0

    def next(self):
        it = self.items[self.i % len(self.items)]
        self.i += 1
        return it


def build(S):
    NT = S // 128
    NSB = S // 512
    nc = bass.Bass("TRN2", target_bir_lowering=False)

    def din(name, shape, dt=F32):
        return nc.dram_tensor(name, shape, dt, kind="ExternalInput").ap()

    def dscr(name, shape, dt=BF16):
        return nc.dram_tensor(name, shape, dt, kind="Internal").ap()

    x = din("x", [S, D])
    pos = din("pos", [S], I32)
    attn_norm_g = din("attn_norm_g", [D])
    w_in = din("w_in", [D, IN_COLS])
    b_gate = din("b_gate", [2 * D])
    da_q_norm_g = din("da_q_norm_g", [64])
    da_k_norm_g = din("da_k_norm_g", [64])
    lq1 = din("da_lambda_q1", [64])
    lk1 = din("da_lambda_k1", [64])
    lq2 = din("da_lambda_q2", [64])
    lk2 = din("da_lambda_k2", [64])
    subln_g = din("da_subln_g", [128])
    q_lora_g = din("mla_q_lora_g", [384])
    w_uq = din("mla_w_uq", [384, 1536])
    kv_lora_g = din("mla_kv_lora_g", [256])
    w_ukv = din("mla_w_ukv", [256, 2048])
    q_norm_g = din("mla_q_norm_g", [192])
    kn_norm_g = din("mla_k_nope_norm_g", [128])
    kr_norm_g = din("mla_k_rope_norm_g", [64])
    w_o = din("w_o", [D, D])
    ffn_norm_g = din("ffn_norm_g", [D])
    w_group = din("w_group", [D, 4])
    b_group = din("b_group", [4])
    w_router = din("w_router", [D, 32])
    b_router = din("b_router", [32])
    w1 = din("w1", [NE, D, DE])
    w3 = din("w3", [NE, D, DE])
    w2 = din("w2", [NE, DE, D])
    cst = din("cst", [128, 7, 128])
    cinv = din("cinv", [128, 2])
    out = nc.dram_tensor("out", [S, D], F32, kind="ExternalOutput").ap()

    s_qda = dscr("s_qda", [8, 128, S])
    s_kda = dscr("s_kda", [8, 128, S])
    s_vda = dscr("s_vda", [S, D])
    s_qn = dscr("s_qn", [8, 128, S])
    s_qr = dscr("s_qr", [8, 64, S])
    s_kn = dscr("s_kn", [8, 128, S])
    s_kr = dscr("s_kr", [64, S])
    s_vm = dscr("s_vm", [S, D])
    s_gate = dscr("s_gate", [16, 128, S])
    NCH = 2 * NT + NE
    cmoe = din("cmoe", [128, 1 + NCH])
    xs = dscr("s_xs", [NCH * 128, D])
    ys = dscr("s_ys", [NCH * 128, D], F32)
    B_xs = Buf("xs", multi=True)
    B_ys = Buf("ys", multi=True)
    B_scr = Buf("scratch", multi=True)
    B_out = Buf("out", multi=True)

    with contextlib.ExitStack() as stack:
        T = Tracker(nc, stack)

        def sb_(name, shape, dt, st=None):
            return (st or stack).enter_context(nc.sbuf_tensor(name, shape, dt))

        ps = stack.enter_context(nc.psum_tensor("ps", [128, 8, 512], F32))
        PB = [Buf(f"psb{i}") for i in range(8)]

        cb = sb_("cb", [128, 7, 128], BF16)
        B_cb = Buf("cb")
        T.dma("pool", cb[:], cst, writes=[B_cb], owner=B_cb)
        identf = sb_("identf", [128, 128], F32)
        B_cf = Buf("cf")
        T.dma("sp", identf[:], cst[:, 0, :], writes=[B_cf], owner=B_cf)
        IDENT, NEGM, B64, RDA, RMLA, ONES, USTRICT = (cb[:, i, :] for i in range(7))
        wt12 = sb_("wt12", [128, NT, 2], F32)
        B_wt = Buf("wt12", multi=True)
        dest = sb_("dest", [128, 2, NT], I32)
        idx2 = sb_("idx2", [128, 2, NCH], I32)
        B_dest = Buf("dest", multi=True)
        cols = sb_("cols", [128, 64], F32)
        B_cols = Buf("cols")
        ev0 = T.op("dve", lambda e: e.memset(cols[:], 0.0), writes=[B_cols])
        T._wait("sp", ev0)
        ci = [0]

        def col_load(src, n, p0=0):
            c = ci[0]
            ci[0] += 1
            T.dma("sp", cols[p0:p0 + n, c:c + 1], src.rearrange("(p o) -> p o", o=1), writes=[B_cols], owner=B_cols)
            return c

        B_cols.multi = True
        c_inv = ci[0]
        ci[0] += 2
        T.dma("sp", cols[:, c_inv:c_inv + 2], cinv, writes=[B_cols], owner=B_cols)
        c_gA = ci[0]
        for c in range(8):
            col_load(attn_norm_g[c * 128:(c + 1) * 128], 128)
        c_gF = ci[0]
        for c in range(8):
            col_load(ffn_norm_g[c * 128:(c + 1) * 128], 128)
        c_bg = ci[0]
        for c in range(16):
            col_load(b_gate[c * 128:(c + 1) * 128], 128)
        c_gq = col_load(da_q_norm_g, 64)
        ci[0] -= 1
        col_load(da_q_norm_g, 64, 64)
        c_gk = col_load(da_k_norm_g, 64)
        ci[0] -= 1
        col_load(da_k_norm_g, 64, 64)
        c_sub = col_load(subln_g, 128)
        c_gql = ci[0]
        for c in range(3):
            col_load(q_lora_g[c * 128:(c + 1) * 128], 128)
        c_gkvl = ci[0]
        for c in range(2):
            col_load(kv_lora_g[c * 128:(c + 1) * 128], 128)
        c_gqn = col_load(q_norm_g[0:128], 128)
        c_gqr = col_load(q_norm_g[128:192], 64)
        c_gkn = col_load(kn_norm_g, 128)
        c_gkr = col_load(kr_norm_g, 64)
        c_eps = ci[0]
        ci[0] += 1
        c_lam = ci[0]
        ci[0] += 4
        B_cols.multi = False
        T.op("dve", lambda e: e.memset(cols[:, c_eps:c_eps + 1], EPS), reads=[B_cols], writes=[B_cols])

        def col(c, rows=128, p0=0):
            return cols[p0:p0 + rows, c:c + 1]

        lam_t = sb_("lam_t", [128, 4, 64], F32)
        B_lam = Buf("lam", multi=True)
        for i, src in enumerate((lq1, lk1, lq2, lk2)):
            T.dma("sp", lam_t[:, i, :], src.partition_broadcast(128), writes=[B_lam], owner=B_lam)
        B_lam.multi = False
        T.op("dve", lambda e: e.tensor_tensor(out=lam_t[:, 0, :], in0=lam_t[:, 0, :], in1=lam_t[:, 1, :], op=ALU.mult),
             reads=[B_lam], writes=[B_lam])
        T.op("dve", lambda e: e.tensor_tensor(out=lam_t[:, 2, :], in0=lam_t[:, 2, :], in1=lam_t[:, 3, :], op=ALU.mult),
             reads=[B_lam], writes=[B_lam])
        T.op("dve", lambda e: e.tensor_reduce(out=cols[:, c_lam:c_lam + 1], in_=lam_t[:, 0, :], axis=AX.X, op=ALU.add),
             reads=[B_lam], writes=[B_cols])
        T.op("dve", lambda e: e.tensor_reduce(out=cols[:, c_lam + 1:c_lam + 2], in_=lam_t[:, 2, :], axis=AX.X, op=ALU.add),
             reads=[B_lam, B_cols], writes=[B_cols])
        T.op("act", lambda e: e.activation(out=cols[:, c_lam:c_lam + 2], in_=cols[:, c_lam:c_lam + 2], func=AF.Exp),
             reads=[B_cols], writes=[B_cols])
        T.op("dve", lambda e: e.scalar_tensor_tensor(out=cols[:, c_lam + 2:c_lam + 3], in0=cols[:, c_lam + 1:c_lam + 2],
                                                      scalar=-LAMBDA_INIT, in1=cols[:, c_lam:c_lam + 1],
                                                      op0=ALU.add, op1=ALU.subtract), reads=[B_cols], writes=[B_cols])
        c_neglam = c_lam + 2
        T.op("dve", lambda e: e.tensor_scalar(out=cols[:, c_lam + 3:c_lam + 4], in0=cols[:, c_sub:c_sub + 1],
                                               scalar1=1.0 - LAMBDA_INIT, scalar2=None, op0=ALU.mult),
             reads=[B_cols], writes=[B_cols])
        c_subs = c_lam + 3
        rg = sb_("rg", [128, 4, 128], BF16)
        B_rg = Buf("rg")
        for i, (src, c) in enumerate(((RDA, c_gq), (RDA, c_gk), (RMLA, c_gqr), (RMLA, c_gkr))):
            T.op("dve", lambda e, i=i, src=src, c=c: e.tensor_scalar(out=rg[:, i, :], in0=src, scalar1=col(c), scalar2=None,
                                                                     op0=ALU.mult), reads=[B_cb, B_cols], writes=[B_rg])
        RGQ, RGK, RGMQ, RGKR = (rg[:, i, :] for i in range(4))
        CONST = [B_cb, B_cols, B_rg]
        zt = sb_("zt", [128, D], BF16)
        B_zt = Buf("zt")
        T.op("dve", lambda e: e.memset(zt[:], 0.0), writes=[B_zt])
        for c in range(NCH):
            T.dma("sp", xs[c * 128:(c + 1) * 128, :], zt[:], reads=[B_zt], writes=[B_xs], owner=B_zt)

        def bank(i, rows=128, c0=0, c1=512):
            return ps[0:rows, i, c0:c1]

        with contextlib.ExitStack() as ph:
            win = sb_("win", [128, 8, IN_COLS], BF16, ph)
            B_win = Buf("win", multi=True)
            w_in_v = w_in.rearrange("(c p) n -> p c n", p=128)
            for kc in range(8):
                for j in range(0, IN_COLS, 1456):
                    T.dma("pool", win[:, kc, j:j + 1456], w_in_v[:, kc, j:j + 1456], writes=[B_win], owner=B_win)
            wuq = sb_("wuq", [128, 3, 1536], BF16, ph)
            T.dma("pool", wuq[:], w_uq.rearrange("(c p) n -> p c n", p=128), writes=[B_win], owner=B_win)
            wkn = sb_("wkn", [128, 2, 8, 128], BF16, ph)
            wv = sb_("wv", [128, 2, 8, 128], BF16, ph)
            ukv_v = w_ukv.rearrange("(c p) (h t d) -> p c h t d", p=128, t=2, d=128)
            for c in range(2):
                T.dma("pool", wkn[:, c, :, :], ukv_v[:, c, :, 0, :], writes=[B_win], owner=B_win)
                T.dma("pool", wv[:, c, :, :], ukv_v[:, c, :, 1, :], writes=[B_win], owner=B_win)
            hT = sb_("hT", [128, 8, 512], BF16, ph)
            B_hT = Buf("hT")
            xt_r = Ring([sb_(f"xt{i}", [128, D], F32, ph)[:] for i in range(2)], "xt")
            junk = sb_("junk", [128, D], BF16, ph)
            B_junk = Buf("junk")
            xn_r = Ring([sb_(f"xn{i}", [128, D], BF16, ph)[:] for i in range(2)], "xn")
            st1 = sb_("st1", [128, 8], F32, ph)
            st_r = Ring([st1[:, i:i + 1] for i in range(8)], "st")
            posi = sb_("posi", [128, 512], I32, ph)
            B_posi = Buf("posi")
            posf = sb_("posf", [128, 512], F32, ph)
            B_posf = Buf("posf")
            tabs = sb_("tabs", [128, 4, 512], F32, ph)
            B_tab = [Buf(f"tab{i}") for i in range(4)]
            SIND, COSD, SINM, COSM = (tabs[:, i, :] for i in range(4))
            cqn = sb_("cqn", [128, 3, 512], BF16, ph)
            B_cqn = Buf("cqn")
            ckvn = sb_("ckvn", [128, 2, 512], BF16, ph)
            B_ckvn = Buf("ckvn")
            f_r = Ring([sb_(f"f{i}", [128, 512], F32, ph)[:] for i in range(8)], "f")
            h_r = Ring([sb_(f"h{i}", [128, 512], BF16, ph)[:] for i in range(8)], "h")
            o_r = Ring([sb_(f"o{i}", [128, 512], BF16, ph)[:] for i in range(4)], "o")
            ov_r = Ring([sb_(f"ov{i}", [128, D], BF16, ph)[:] for i in range(2)], "ov")
            pa = Ring([0, 1, 2], "pa")
            pa.items = [(i, PB[i]) for i in (0, 1, 2, 6)]
            px = Ring([3, 4, 5], "px")
            px.items = [(i, PB[i]) for i in (3, 4, 5)]
            pt = Ring([6, 7], "pt")
            pt.items = [(i, PB[i]) for i in (7,)]

            def make_rstd(ssq_ap, B_ssq, n, rows):
                sd, B_sd = f_r.next()
                T.op("act", lambda e: e.activation(out=sd[0:rows], in_=ssq_ap, func=AF.Sqrt, scale=1.0 / n,
                                                    bias=col(c_eps, rows)), reads=[B_ssq, B_cols], writes=[B_sd])
                rs, B_rs = f_r.next()
                T.op("dve", lambda e: e.reciprocal(out=rs[0:rows], in_=sd[0:rows]), reads=[B_sd], writes=[B_rs])
                return rs, B_rs

            def square(ps_ap, B_ps, rows):
                sq, B_sq = h_r.next()
                T.op("act", lambda e: e.activation(out=sq[0:rows], in_=ps_ap, func=AF.Square), reads=[B_ps], writes=[B_sq])
                return sq, B_sq

            def store(q, dst, src_ap, B_src):
                T.dma(q, dst, src_ap, reads=[B_src], writes=[B_scr], owner=B_src)

            def rope_finish(rows, ps_ap, B_ps, gc, rgmat, cos, sin, B_cos, B_sin, rs, B_rs, dst):
                cbf, B_cbf = h_r.next()
                T.op("act", lambda e: e.activation(out=cbf[0:rows], in_=ps_ap, func=AF.Copy), reads=[B_ps], writes=[B_cbf])
                bi, B_b = px.next()
                T.mm([lambda e: e.matmul(bank(bi, rows), lhsT=rgmat[0:rows, 0:rows], rhs=cbf[0:rows], start=True, stop=True)],
                     reads=[B_cbf, B_rg], writes=[B_b])
                t1, B_t1 = f_r.next()
                T.op("dve", lambda e: e.scalar_tensor_tensor(out=t1[0:rows], in0=ps_ap, scalar=col(gc, rows), in1=cos[0:rows],
                                                              op0=ALU.mult, op1=ALU.mult), reads=[B_ps, B_cols, B_cos], writes=[B_t1])
                t2, B_t2 = f_r.next()
                T.op("dve", lambda e: e.tensor_tensor(out=t2[0:rows], in0=bank(bi, rows), in1=sin[0:rows], op=ALU.mult),
                     reads=[B_b, B_sin], writes=[B_t2])
                T.op("pool", lambda e: e.tensor_tensor(out=t1[0:rows], in0=t1[0:rows], in1=t2[0:rows], op=ALU.add),
                     reads=[B_t2, B_t1], writes=[B_t1])
                ob, B_ob = o_r.next()
                T.op("pool", lambda e: e.tensor_tensor(out=ob[0:rows], in0=t1[0:rows], in1=rs[0:rows], op=ALU.mult),
                     reads=[B_t1, B_rs], writes=[B_ob])
                store("sp", dst, ob[0:rows], B_ob)

            pre = {}

            def prefetch_sb(sbn):
                tn = sbn * 512
                T.dma("sp", posi[:], pos[tn:tn + 512].partition_broadcast(128), writes=[B_posi], owner=B_posi)
                pre["posi"] = sbn
                pre["xt"] = []
                for tt_ in range(2):
                    xt_, B_xt_ = xt_r.next()
                    T.dma("sp", xt_, x[tn + tt_ * 128:tn + (tt_ + 1) * 128, :], writes=[B_xt_], owner=B_xt_)
                    pre["xt"].append((xt_, B_xt_))

            for sb in range(NSB):
                t0 = sb * 512
                if pre.get("posi") != sb:
                    prefetch_sb(sb)
                T.op("dve", lambda e: e.tensor_copy(out=posf[:], in_=posi[:]), reads=[B_posi], writes=[B_posf])
                for ti, (cc, off) in enumerate(((0, 0.0), (0, 0.25), (1, 0.0), (1, 0.25))):
                    v, B_v = f_r.next()
                    T.op("dve", lambda e: e.tensor_scalar(out=v, in0=posf[:], scalar1=col(c_inv + cc), scalar2=off,
                                                           op0=ALU.mult, op1=ALU.add), reads=[B_posf, B_cols], writes=[B_v])
                    k1, B_k1 = f_r.next()
                    T.op("dve", lambda e: e.tensor_scalar(out=k1, in0=v, scalar1=MAGIC, scalar2=None, op0=ALU.add),
                         reads=[B_v], writes=[B_k1])
                    T.op("dve", lambda e: e.tensor_scalar(out=k1, in0=k1, scalar1=-MAGIC, scalar2=None, op0=ALU.add),
                         reads=[B_k1], writes=[B_k1])
                    T.op("dve", lambda e: e.tensor_tensor(out=v, in0=v, in1=k1, op=ALU.subtract), reads=[B_v, B_k1], writes=[B_v])
                    T.op("act", lambda e: e.activation(out=tabs[:, ti, :], in_=v, func=AF.Sin, scale=2.0 * math.pi),
                         reads=[B_v], writes=[B_tab[ti]])
                for tt in range(4):
                    r0 = t0 + tt * 128
                    if tt < 2:
                        xt, B_xt = pre["xt"][tt]
                    else:
                        xt, B_xt = xt_r.next()
                        T.dma("sp", xt, x[r0:r0 + 128, :], writes=[B_xt], owner=B_xt)
                    ss, B_ss = st_r.next()
                    T.op("act", lambda e: e.activation(out=junk[:], in_=xt, func=AF.Square, accum_out=ss),
                         reads=[B_xt], writes=[B_junk, B_ss])
                    T.op("act", lambda e: e.activation(out=ss, in_=ss, func=AF.Sqrt, scale=1.0 / D, bias=col(c_eps)),
                         reads=[B_ss, B_cols], writes=[B_ss])
                    T.op("dve", lambda e: e.reciprocal(out=ss, in_=ss), reads=[B_ss], writes=[B_ss])
                    xn, B_xn = xn_r.next()
                    T.op("dve", lambda e: e.tensor_scalar(out=xn, in0=xt, scalar1=ss, scalar2=None, op0=ALU.mult),
                         reads=[B_xt, B_ss], writes=[B_xn])
                    bi, B_b = pt.next()
                    pT = ps[:, bi, :].bitcast(BF16).rearrange("p (c t) -> p c t", t=128)
                    T.mm([(lambda e, c=c: e.transpose(pT[:, c, :], xn[:, c * 128:(c + 1) * 128], IDENT)) for c in range(8)],
                         reads=[B_xn, B_cb], writes=[B_b])
                    T.op("dve", lambda e: e.tensor_tensor(out=hT[:, :, tt * 128:(tt + 1) * 128], in0=pT,
                                                           in1=cols[:, c_gA:c_gA + 8].unsqueeze(2).broadcast_to([128, 8, 128]),
                                                           op=ALU.mult), reads=[B_b, B_cols], writes=[B_hT])

                def proj(bi, B_b, wt, c0, ncols, rhs, nk, extra_reads):
                    T.mm([(lambda e, kc=kc: e.matmul(bank(bi, ncols), lhsT=wt[:, kc, c0:c0 + ncols], rhs=rhs[:, kc, :],
                                                     start=(kc == 0), stop=(kc == nk - 1))) for kc in range(nk)],
                         reads=[B_win] + extra_reads, writes=[B_b])

                jobs = []

                def add(front, back):
                    jobs.append((front, back))

                def lat_job(ncs, cbase, gbase, dstt, B_dst):
                    stt_ = {}

                    def front():
                        banks = []
                        for c in range(ncs):
                            bi, B_b = pa.next()
                            proj(bi, B_b, win, cbase + c * 128, 128, hT, 8, [B_hT])
                            banks.append((bi, B_b))
                        stt_["banks"] = banks
                        stt_["sqs"] = [square(bank(bi), B_b, 128) for (bi, B_b) in banks]

                    def back():
                        banks, sqs = stt_["banks"], stt_["sqs"]
                        si, B_s = px.next()
                        T.mm([(lambda e, c=c: e.matmul(bank(si), lhsT=ONES, rhs=sqs[c][0], start=(c == 0), stop=(c == ncs - 1)))
                              for c in range(ncs)], reads=[B_cb] + [q_[1] for q_ in sqs], writes=[B_s])
                        rs, B_rs = make_rstd(bank(si), B_s, 128 * ncs, 128)
                        for c in range(ncs):
                            bi, B_b = banks[c]
                            T.op("dve", lambda e, c=c, bi=bi: e.scalar_tensor_tensor(out=dstt[:, c, :], in0=bank(bi), scalar=col(gbase + c),
                                                                                      in1=rs, op0=ALU.mult, op1=ALU.mult),
                                 reads=[B_b, B_cols, B_rs], writes=[B_dst])
                    add(front, back)

                lat_job(3, C_CQ, c_gql, cqn, B_cqn)

                def kr_job():
                    stt_ = {}

                    def front():
                        bi, B_b = pa.next()
                        proj(bi, B_b, win, C_KR, 64, hT, 8, [B_hT])
                        stt_["b"] = (bi, B_b)
                        stt_["sq"] = square(bank(bi, 64), B_b, 64)

                    def back():
                        bi, B_b = stt_["b"]
                        sq, B_sq = stt_["sq"]
                        si, B_s = px.next()
                        T.mm([lambda e: e.matmul(bank(si, 64), lhsT=cb[0:64, 5, 0:64], rhs=sq[0:64], start=True, stop=True)],
                             reads=[B_sq, B_cb], writes=[B_s])
                        rs, B_rs = make_rstd(bank(si, 64), B_s, 64, 64)
                        rope_finish(64, bank(bi, 64), B_b, c_gkr, RGKR, COSM, SINM, B_tab[3], B_tab[2], rs, B_rs, s_kr[:, t0:t0 + 512])
                    add(front, back)

                kr_job()
                lat_job(2, C_CKV, c_gkvl, ckvn, B_ckvn)

                def da_job(typ, h):
                    stt_ = {}

                    def front():
                        bi, B_b = pa.next()
                        proj(bi, B_b, win, typ * 1024 + h * 128, 128, hT, 8, [B_hT])
                        stt_["b"] = (bi, B_b)
                        stt_["sq"] = square(bank(bi), B_b, 128)

                    def back():
                        bi, B_b = stt_["b"]
                        sq, B_sq = stt_["sq"]
                        si, B_s = px.next()
                        T.mm([lambda e: e.matmul(bank(si), lhsT=B64, rhs=sq, start=True, stop=True)], reads=[B_sq, B_cb], writes=[B_s])
                        rs, B_rs = make_rstd(bank(si), B_s, 64, 128)
                        dst = (s_qda, s_kda)[typ][h, :, t0:t0 + 512]
                        rope_finish(128, bank(bi), B_b, (c_gq, c_gk)[typ], (RGQ, RGK)[typ], COSD, SIND, B_tab[1], B_tab[0], rs, B_rs, dst)
                    add(front, back)

                for typ in range(2):
                    for h in range(8):
                        da_job(typ, h)

                def mq_job(h):
                    stt_ = {}

                    def front():
                        ai, B_a = pa.next()
                        proj(ai, B_a, wuq, h * 192, 128, cqn, 3, [B_cqn])
                        bi2, B_b2 = pa.next()
                        proj(bi2, B_b2, wuq, h * 192 + 128, 64, cqn, 3, [B_cqn])
                        stt_["a"] = (ai, B_a)
                        stt_["b"] = (bi2, B_b2)
                        stt_["sqa"] = square(bank(ai), B_a, 128)
                        stt_["sqb"] = square(bank(bi2, 64), B_b2, 64)

                    def back():
                        ai, B_a = stt_["a"]
                        bi2, B_b2 = stt_["b"]
                        sqa, B_sqa = stt_["sqa"]
                        sqb, B_sqb = stt_["sqb"]
                        si, B_s = px.next()
                        T.mm([lambda e: e.matmul(bank(si), lhsT=ONES, rhs=sqa, start=True, stop=False),
                              lambda e: e.matmul(bank(si), lhsT=cb[0:64, 5, :], rhs=sqb[0:64], start=False, stop=True)],
                             reads=[B_cb, B_sqa, B_sqb], writes=[B_s])
                        rs, B_rs = make_rstd(bank(si), B_s, 192, 128)
                        ob, B_ob = o_r.next()
                        T.op("dve", lambda e: e.scalar_tensor_tensor(out=ob, in0=bank(ai), scalar=col(c_gqn), in1=rs,
                                                                      op0=ALU.mult, op1=ALU.mult), reads=[B_a, B_cols, B_rs], writes=[B_ob])
                        store("sp", s_qn[h, :, t0:t0 + 512], ob, B_ob)
                        rope_finish(64, bank(bi2, 64), B_b2, c_gqr, RGMQ, COSM, SINM, B_tab[3], B_tab[2], rs, B_rs, s_qr[h, :, t0:t0 + 512])
                    add(front, back)

                for h in range(8):
                    mq_job(h)

                def kn_job(h):
                    stt_ = {}

                    def front():
                        bi, B_b = pa.next()
                        T.mm([(lambda e, c=c: e.matmul(bank(bi), lhsT=wkn[:, c, h, :], rhs=ckvn[:, c, :], start=(c == 0), stop=(c == 1)))
                              for c in range(2)], reads=[B_win, B_ckvn], writes=[B_b])
                        stt_["b"] = (bi, B_b)
                        stt_["sq"] = square(bank(bi), B_b, 128)

                    def back():
                        bi, B_b = stt_["b"]
                        sq, B_sq = stt_["sq"]
                        si, B_s = px.next()
                        T.mm([lambda e: e.matmul(bank(si), lhsT=ONES, rhs=sq, start=True, stop=True)], reads=[B_sq, B_cb], writes=[B_s])
                        rs, B_rs = make_rstd(bank(si), B_s, 128, 128)
                        ob, B_ob = o_r.next()
                        T.op("dve", lambda e: e.scalar_tensor_tensor(out=ob, in0=bank(bi), scalar=col(c_gkn), in1=rs,
                                                                      op0=ALU.mult, op1=ALU.mult), reads=[B_b, B_cols, B_rs], writes=[B_ob])
                        store("sp", s_kn[h, :, t0:t0 + 512], ob, B_ob)
                    add(front, back)

                for h in range(8):
                    kn_job(h)

                def v_job(tt, half, kind, ovh):
                    stt_ = {}

                    def front():
                        bi, B_b = pa.next()
                        if kind == 0:
                            T.mm([(lambda e, kc=kc: e.matmul(bank(bi), lhsT=hT[:, kc, tt * 128:(tt + 1) * 128],
                                                             rhs=win[:, kc, 2048 + half * 512:2048 + (half + 1) * 512],
                                                             start=(kc == 0), stop=(kc == 7))) for kc in range(8)],
                                 reads=[B_win, B_hT], writes=[B_b])
                        else:
                            T.mm([(lambda e, c=c: e.matmul(bank(bi), lhsT=ckvn[:, c, tt * 128:(tt + 1) * 128],
                                                           rhs=wv[:, c, half * 4:(half + 1) * 4, :].rearrange("p h d -> p (h d)"),
                                                           start=(c == 0), stop=(c == 1))) for c in range(2)],
                                 reads=[B_win, B_ckvn], writes=[B_b])
                        stt_["b"] = (bi, B_b)

                    def back():
                        bi, B_b = stt_["b"]
                        if half == 0:
                            ovh["ov"] = ov_r.next()
                        ov, B_ov = ovh["ov"]
                        T.op("act", lambda e: e.activation(out=ov[:, half * 512:(half + 1) * 512], in_=bank(bi), func=AF.Copy),
                             reads=[B_b], writes=[B_ov])
                        if half == 1:
                            dstt = (s_vda, s_vm)[kind]
                            store("sp", dstt[t0 + tt * 128:t0 + (tt + 1) * 128, :], ov, B_ov)
                    add(front, back)

                for kind in range(2):
                    for tt in range(4):
                        ovh = {}
                        for half in range(2):
                            v_job(tt, half, kind, ovh)

                def gate_job(c):
                    stt_ = {}

                    def front():
                        bi, B_b = pa.next()
                        proj(bi, B_b, win, C_GATE + c * 128, 128, hT, 8, [B_hT])
                        stt_["b"] = (bi, B_b)

                    def back():
                        bi, B_b = stt_["b"]
                        ob, B_ob = o_r.next()
                        T.op("act", lambda e: e.activation(out=ob, in_=bank(bi), func=AF.Sigmoid, bias=col(c_bg + c)),
                             reads=[B_b, B_cols], writes=[B_ob])
                        store("sp", s_gate[c, :, t0:t0 + 512], ob, B_ob)
                    add(front, back)

                for c in range(16):
                    gate_job(c)

                jobs[0][0]()
                for ji in range(len(jobs)):
                    if ji + 1 < len(jobs):
                        jobs[ji + 1][0]()
                    jobs[ji][1]()
                    if ji == 4 and sb + 1 < NSB:
                        prefetch_sb(sb + 1)
            T.barrier()

        with contextlib.ExitStack() as ph:
            mixT = sb_("mixT", [128, 8, S], BF16, ph)
            B_mix = Buf("mix", multi=True)
            with contextlib.ExitStack() as phb:
                kT_r = Ring([sb_(f"kT{i}", [128, S], BF16, phb)[:] for i in range(2)], "kT")
                vD_r = Ring([sb_(f"vD{i}", [128, NT, 128], BF16, phb)[:] for i in range(2)], "vD")
                kn_r = Ring([sb_(f"kn{i}", [128, S], BF16, phb)[:] for i in range(2)], "kn")
                vM_r = Ring([sb_(f"vM{i}", [128, NT, 128], BF16, phb)[:] for i in range(2)], "vM")
                krT = sb_("krT", [128, S], BF16, phb)
                B_kr = Buf("krT")
                evk = T.op("dve", lambda e: e.memset(krT[64:128, :], 0.0), writes=[B_kr])
                B_kr.par = True
                T.dma("sp", krT[0:64, :], s_kr, reads=[B_scr], writes=[B_kr], owner=B_kr)
                q_r = Ring([sb_(f"q{i}", [128, 4, 512], BF16, phb)[:] for i in range(2)], "q")
                for (qap_, B_qq) in q_r.items:
                    T.op("dve", lambda e: e.memset(qap_, 0.0), writes=[B_qq])
                    B_qq.par = True
                g_r = Ring([sb_(f"g{i}", [128, 2, 512], BF16, phb)[:] for i in range(2)], "g")
                p_r = Ring([sb_(f"p{i}", [128, 512], BF16, phb)[:] for i in range(8)], "p")
                f_r = Ring([sb_(f"fb{i}", [128, 512], F32, phb)[:] for i in range(6)], "fb")
                h_r = Ring([sb_(f"hb{i}", [128, 512], BF16, phb)[:] for i in range(2)], "hb")
                psS = Ring([0, 1, 2], "pS")
                psS.items = [(i, PB[i]) for i in (0, 1, 2, 6)]
                OBANK = {"m": (3, PB[3]), "d0": (4, PB[4]), "d1": (5, PB[5])}
                pz = Ring([0], "pz")
                pz.items = [(i, PB[i]) for i in (7,)]
                onesf = sb_("onesf", [128, 128], F32, phb)
                B_onesf = Buf("onesf")
                T.op("dve", lambda e: e.memset(onesf[:], 1.0), writes=[B_onesf])
                pacc_r = {k_: Ring([sb_(f"pacc{k_}{i}", [128, 512], F32, phb)[:] for i in range(3)], "pacc" + k_) for k_ in ("m", "d1")}
                oc_r = {k_: Ring([sb_(f"oc{k_}{i}", [128, 512], F32, phb)[:] for i in range(1)], "oc" + k_) for k_ in ("m", "d0", "d1")}
                ACCENG = {"m": "dve", "d0": "pe", "d1": "pool"}
                LOOK = 3

                def front(jb):
                    kb, c0, j = jb["kb"], jb["c0"], jb["j"]
                    si, B_s = psS.next()
                    fns = []
                    rd = [B_cb]
                    parts = jb["parts"]
                    for pi_, (kap, qap, rows, B_k, B_q) in enumerate(parts):
                        fns.append(lambda e, kap=kap, qap=qap, rows=rows, pi_=pi_: e.matmul(
                            bank(si, 128, c0, 512), lhsT=kap[0:rows, kb * 128:(kb + 1) * 128], rhs=qap[0:rows, c0:512],
                            start=(pi_ == 0), stop=(pi_ == len(parts) - 1 and j < 0)))
                        rd += [B_k, B_q]
                    if j >= 0:
                        fns.append(lambda e: e.matmul(bank(si, 128, c0, c0 + 128), lhsT=IDENT, rhs=NEGM, start=False, stop=True))
                    T.mm(fns, reads=rd, writes=[B_s])
                    pt_, B_p = p_r.next()
                    T.op("act", lambda e: e.activation(out=pt_[:, c0:512], in_=bank(si, 128, c0, 512), func=AF.Exp, scale=jb["scale"]),
                         reads=[B_s], writes=[B_p])
                    jb["pt"], jb["B_p"] = pt_, B_p

                def back(jb):
                    kb, c0, nkb, key = jb["kb"], jb["c0"], jb["nkb"], jb["key"]
                    pt_, B_p = jb["pt"], jb["B_p"]
                    oi, B_o = OBANK[key]
                    g = jb["grp"]
                    T.mm([lambda e: e.matmul(bank(oi, 128, c0, 512), lhsT=jb["vt"][:, kb, :], rhs=pt_[:, c0:512],
                                             start=(kb == 0), stop=(kb == nkb - 1))], reads=[jb["B_v"], B_p], writes=[B_o])
                    if key == "d0":
                        zb, B_zb = OBANK["m"]
                        T.mm([lambda e: e.matmul(bank(zb, 128, c0, 512), lhsT=ONES, rhs=pt_[:, c0:512],
                                                 start=(kb == 0), stop=(kb == nkb - 1))], reads=[B_cb, B_p], writes=[B_zb])
                    else:
                        if kb < 2:
                            g["pacc"][(key, kb)] = pacc_r[key].next()
                        pa_, B_pa = g["pacc"][(key, kb % 2)]
                        if kb < 2:
                            if c0 > 0:
                                T.op(ACCENG[key], lambda e: e.memset(pa_[:, 0:c0], 0.0), writes=[B_pa])
                            T.op(ACCENG[key], lambda e: e.tensor_copy(out=pa_[:, c0:512], in_=pt_[:, c0:512]), reads=[B_p, B_pa], writes=[B_pa])
                        else:
                            T.op(ACCENG[key], lambda e: e.tensor_tensor(out=pa_[:, c0:512], in0=pa_[:, c0:512], in1=pt_[:, c0:512], op=ALU.add),
                                 reads=[B_p, B_pa], writes=[B_pa])
                    if kb == nkb - 1:
                        oc, B_oc = oc_r[key].next()
                        T.op("act", lambda e: e.activation(out=oc, in_=bank(oi), func=AF.Copy), reads=[B_o], writes=[B_oc])
                        if key == "d0":
                            zi, B_z = OBANK["m"]
                        else:
                            zi, B_z = pz.next()
                            pa0, B_pa0 = g["pacc"][(key, 0)]
                            pa1, B_pa1 = g["pacc"][(key, 1)]
                            T.mm([lambda e: e.matmul(bank(zi), lhsT=onesf[:], rhs=pa0, start=True, stop=False),
                                  lambda e: e.matmul(bank(zi), lhsT=onesf[:], rhs=pa1, start=False, stop=True)],
                                 reads=[B_onesf, B_pa0, B_pa1], writes=[B_z])
                        rz, B_rz = f_r.next()
                        T.op("dve", lambda e: e.reciprocal(out=rz, in_=bank(zi)), reads=[B_z], writes=[B_rz])
                        g["fin"][key] = (oc, B_oc, rz, B_rz)
                        if key == "m":
                            fin_mla(g)
                        elif key == "d1":
                            fin_da(g)

                def fin_mla(g):
                    oc, B_oc, rz, B_rz = g["fin"]["m"]
                    gt, B_g = g["gt"], g["B_g"]
                    T.op("dve", lambda e: e.tensor_tensor(out=oc, in0=oc, in1=rz, op=ALU.mult), reads=[B_oc, B_rz], writes=[B_oc])
                    T.op("dve", lambda e: e.tensor_tensor(out=oc, in0=oc, in1=gt[:, 1, :], op=ALU.mult), reads=[B_oc, B_g], writes=[B_oc])

                def fin_da(g):
                    om, B_om = g["fin"]["m"][0], g["fin"]["m"][1]
                    o0, B_o0, r0, B_r0 = g["fin"]["d0"]
                    o1, B_o1, r1, B_r1 = g["fin"]["d1"]
                    gt, B_g = g["gt"], g["B_g"]
                    h, t0 = g["h"], g["t0"]
                    T.op("dve", lambda e: e.tensor_tensor(out=o0, in0=o0, in1=r0, op=ALU.mult), reads=[B_o0, B_r0], writes=[B_o0])
                    T.op("dve", lambda e: e.scalar_tensor_tensor(out=o1, in0=o1, scalar=col(c_neglam), in1=r1,
                                                                  op0=ALU.mult, op1=ALU.mult), reads=[B_o1, B_r1, B_cols], writes=[B_o1])
                    T.op("dve", lambda e: e.tensor_tensor(out=o0, in0=o0, in1=o1, op=ALU.add), reads=[B_o0, B_o1], writes=[B_o0])
                    sq, B_sq = h_r.next()
                    T.op("pool", lambda e: e.tensor_tensor(out=sq, in0=o0, in1=o0, op=ALU.mult), reads=[B_o0], writes=[B_sq])
                    zi, B_z = pz.next()
                    T.mm([lambda e: e.matmul(bank(zi), lhsT=ONES, rhs=sq, start=True, stop=True)], reads=[B_sq, B_cb], writes=[B_z])
                    sd, B_sd = f_r.next()
                    T.op("act", lambda e: e.activation(out=sd, in_=bank(zi), func=AF.Ln, scale=1.0 / 128, bias=col(c_eps)),
                         reads=[B_z, B_cols], writes=[B_sd])
                    T.op("act", lambda e: e.activation(out=sd, in_=sd, func=AF.Exp, scale=-0.5), reads=[B_sd], writes=[B_sd])
                    T.op("dve", lambda e: e.scalar_tensor_tensor(out=o0, in0=o0, scalar=col(c_subs), in1=sd, op0=ALU.mult, op1=ALU.mult),
                         reads=[B_o0, B_sd, B_cols], writes=[B_o0])
                    T.op("dve", lambda e: e.tensor_tensor(out=o0, in0=o0, in1=gt[:, 0, :], op=ALU.mult), reads=[B_o0, B_g], writes=[B_o0])
                    T.op("dve", lambda e: e.tensor_tensor(out=mixT[:, h, t0:t0 + 512], in0=o0, in1=om, op=ALU.add),
                         reads=[B_o0, B_om], writes=[B_mix])

                pending = []

                def push(jb):
                    front(jb)
                    pending.append(jb)
                    if len(pending) > LOOK:
                        back(pending.pop(0))

                for h in range(8):
                    kT, B_kT = kT_r.next()
                    T.dma("sp", kT, s_kda[h], reads=[B_scr], writes=[B_kT], owner=B_kT)
                    vD, B_vD = vD_r.next()
                    T.dma("sp", vD, s_vda[:, h * 128:(h + 1) * 128].rearrange("(n p) d -> p n d", p=128),
                          reads=[B_scr], writes=[B_vD], owner=B_vD)
                    kn, B_kn = kn_r.next()
                    T.dma("sp", kn, s_kn[h], reads=[B_scr], writes=[B_kn], owner=B_kn)
                    vM, B_vM = vM_r.next()
                    T.dma("sp", vM, s_vm[:, h * 128:(h + 1) * 128].rearrange("(n p) d -> p n d", p=128),
                          reads=[B_scr], writes=[B_vM], owner=B_vM)
                    for sbi in range(NSB):
                        t0 = sbi * 512
                        qt, B_q = q_r.next()
                        T.dma("sp", qt[0:64, 0, :], s_qda[h, 0:64, t0:t0 + 512], reads=[B_scr], writes=[B_q], owner=B_q)
                        T.dma("sp", qt[64:128, 3, :], s_qda[h, 64:128, t0:t0 + 512], reads=[B_scr], writes=[B_q], owner=B_q)
                        T.dma("sp", qt[:, 1, :], s_qn[h, :, t0:t0 + 512], reads=[B_scr], writes=[B_q], owner=B_q)
                        T.dma("sp", qt[0:64, 2, :], s_qr[h, :, t0:t0 + 512], reads=[B_scr], writes=[B_q], owner=B_q)
                        gt, B_g = g_r.next()
                        T.dma("sp", gt[:], s_gate.rearrange("(a c) p t -> c p a t", a=2)[h, :, :, t0:t0 + 512], reads=[B_scr], writes=[B_g], owner=B_g)
                        grp = {"pacc": {}, "fin": {}, "gt": gt, "B_g": B_g, "h": h, "t0": t0}
                        nkb = 4 * sbi + 4
                        for kb in range(nkb):
                            j = kb - 4 * sbi
                            push({"kb": kb, "j": j, "c0": max(j, 0) * 128, "nkb": nkb, "key": "m", "grp": grp, "scale": 192 ** -0.5,
                                  "vt": vM, "B_v": B_vM,
                                  "parts": [(kn, qt[:, 1, :], 128, B_kn, B_q), (krT[:], qt[:, 2, :], 128, B_kr, B_q)]})
                        for kb in range(nkb):
                            j = kb - 4 * sbi
                            for m in range(2):
                                push({"kb": kb, "j": j, "c0": max(j, 0) * 128, "nkb": nkb, "key": f"d{m}", "grp": grp, "scale": 0.125,
                                      "vt": vD, "B_v": B_vD,
                                      "parts": [(kT, qt[:, 3 * m, :], 128, B_kT, B_q)]})
                while pending:
                    back(pending.pop(0))
                T.barrier()

            B_mix.multi = False
            with contextlib.ExitStack() as phc:
                wo = sb_("wo", [128, 8, D], BF16, phc)
                B_wo = Buf("wo")
                T.dma("pool", wo[:], w_o.rearrange("(c p) n -> p c n", p=128), writes=[B_wo], owner=B_wo)
                wr = sb_("wr", [128, 8, 36], F32, phc)
                B_wr = Buf("wr", multi=True)
                T.dma("sp", wr[:, :, 0:4], w_group.rearrange("(c p) n -> p c n", p=128), writes=[B_wr], owner=B_wr)
                T.dma("sp", wr[:, :, 4:36], w_router.rearrange("(c p) n -> p c n", p=128), writes=[B_wr], owner=B_wr)
                br = sb_("br", [128, 36], F32, phc)
                T.dma("sp", br[:, 0:4], b_group.partition_broadcast(128), writes=[B_wr], owner=B_wr)
                T.dma("sp", br[:, 4:36], b_router.partition_broadcast(128), writes=[B_wr], owner=B_wr)
                gfr = sb_("gfr", [128, D], F32, phc)
                T.dma("sp", gfr[:], ffn_norm_g.partition_broadcast(128), writes=[B_wr], owner=B_wr)
                cm = sb_("cm", [128, 1 + NCH], F32, phc)
                T.dma("sp", cm[:], cmoe, writes=[B_wr], owner=B_wr)
                h2tok = sb_("h2tok", [128, NT, D], BF16, phc)
                B_h2 = Buf("h2tok", multi=True)
                oh_all = sb_("oh_all", [128, 2, NT, NE], F32, phc)
                p12 = sb_("p12", [128, 2, NT], F32, phc)
                B_plan = Buf("plan", multi=True)
                carry = sb_("carry", [128, NE], F32, phc)
                B_carry = Buf("carry")
                T.op("dve", lambda e: e.memset(carry[:], 0.0), writes=[B_carry])
                xt_r = Ring([sb_(f"cx{i}", [128, D], F32, phc)[:] for i in range(2)], "cx")
                x1_r = Ring([sb_(f"x1{i}", [128, D], F32, phc)[:] for i in range(2)], "x1")
                junk = sb_("junkc", [128, D], BF16, phc)
                B_junk = Buf("junkc")
                hf_r = Ring([sb_(f"hf{i}", [128, 8, 128], F32, phc)[:] for i in range(1)], "hf")
                sm = sb_("sm", [128, 2, 256], F32, phc)
                sm_r = Ring([sm[:, i, :] for i in range(2)], "sm")
                abf_r = Ring([sb_(f"abf{i}", [128, NE], BF16, phc)[:] for i in range(2)], "abf")
                pc = Ring([0, 1], "pc")
                pc.items = [((0, 1), PB[0]), ((2, 3), PB[2])]
                ptc = Ring([0], "ptc")
                ptc.items = [((4, 5), PB[4])]
                prt = (6, PB[6])
                prk = (7, PB[7])
                for t in range(NT):
                    r0 = t * 128
                    (b0, b1), B_b = pc.next()
                    for half, bi in enumerate((b0, b1)):
                        T.mm([(lambda e, hh=hh: e.matmul(bank(bi), lhsT=mixT[:, hh, r0:r0 + 128], rhs=wo[:, hh, half * 512:(half + 1) * 512],
                                                         start=(hh == 0), stop=(hh == 7))) for hh in range(8)],
                             reads=[B_mix, B_wo], writes=[B_b])
                    xt, B_xt = xt_r.next()
                    T.dma("sp", xt, x[r0:r0 + 128, :], writes=[B_xt], owner=B_xt)
                    x1, B_x1 = x1_r.next()
                    T.op("dve", lambda e: e.tensor_tensor(out=x1.rearrange("p (a n) -> p a n", a=2), in0=ps[:, b0:b0 + 2, :],
                                                           in1=xt.rearrange("p (a n) -> p a n", a=2), op=ALU.add),
                         reads=[B_b, B_xt], writes=[B_x1])
                    T.dma("sp", out[r0:r0 + 128, :], x1, reads=[B_x1], writes=[B_out], owner=B_x1)
                    s_, B_s = sm_r.next()
                    T.op("act", lambda e: e.activation(out=junk[:], in_=x1, func=AF.Square, accum_out=s_[:, 0:1]),
                         reads=[B_x1], writes=[B_junk, B_s])
                    T.op("act", lambda e: e.activation(out=s_[:, 1:2], in_=s_[:, 0:1], func=AF.Sqrt, scale=1.0 / D, bias=col(c_eps)),
                         reads=[B_s, B_cols], writes=[B_s])
                    T.op("dve", lambda e: e.reciprocal(out=s_[:, 2:3], in_=s_[:, 1:2]), reads=[B_s], writes=[B_s])
                    T.op("dve", lambda e: e.tensor_scalar(out=x1, in0=x1, scalar1=s_[:, 2:3], scalar2=None, op0=ALU.mult),
                         reads=[B_x1, B_s], writes=[B_x1])
                    T.op("pool", lambda e: e.tensor_tensor(out=h2tok[:, t, :], in0=x1, in1=gfr[:], op=ALU.mult), reads=[B_x1, B_wr], writes=[B_h2])
                    (tb, _), B_tb = ptc.next()
                    pTf = ps[:, tb:tb + 2, :].rearrange("p a (c t) -> p (a c) t", t=128)
                    T.mm([(lambda e, c=c: e.transpose(pTf[:, c, :], x1[:, c * 128:(c + 1) * 128], identf[:])) for c in range(8)],
                         reads=[B_x1, B_cf], writes=[B_tb])
                    hf, B_hf = hf_r.next()
                    T.op("dve", lambda e: e.tensor_tensor(out=hf, in0=pTf, in1=cols[:, c_gF:c_gF + 8].unsqueeze(2).broadcast_to([128, 8, 128]),
                                                           op=ALU.mult), reads=[B_tb, B_cols], writes=[B_hf])
                    T.mm([(lambda e, c=c: e.matmul(bank(prt[0], 128, 0, 36), lhsT=hf[:, c, :], rhs=wr[:, c, :], start=(c == 0), stop=(c == 7)))
                          for c in range(8)], reads=[B_hf, B_wr], writes=[prt[1]])
                    def dv(fn):
                        T.op("dve", fn, reads=[B_s], writes=[B_s])
                    lg = s_[:, 4:40]
                    T.op("dve", lambda e: e.tensor_tensor(out=lg, in0=bank(prt[0], 128, 0, 36), in1=br[:], op=ALU.add),
                         reads=[prt[1], B_wr, B_s], writes=[B_s])
                    gmax = s_[:, 40:41]
                    dv(lambda e: e.tensor_reduce(out=gmax, in_=s_[:, 4:8], axis=AX.X, op=ALU.max))
                    ohg = s_[:, 44:48]
                    dv(lambda e: e.tensor_scalar(out=ohg, in0=s_[:, 4:8], scalar1=gmax, scalar2=None, op0=ALU.is_ge))
                    dv(lambda e: e.tensor_scalar(out=s_[:, 48:52], in0=s_[:, 4:8], scalar1=gmax, scalar2=None, op0=ALU.subtract))
                    T.op("act", lambda e: e.activation(out=s_[:, 48:52], in_=s_[:, 48:52], func=AF.Exp, accum_out=s_[:, 41:42]),
                         reads=[B_s], writes=[B_s])
                    dv(lambda e: e.reciprocal(out=s_[:, 42:43], in_=s_[:, 41:42]))
                    dv(lambda e: e.tensor_scalar(out=s_[:, 52:56], in0=ohg, scalar1=-1.0, scalar2=1.0e4, op0=ALU.add, op1=ALU.mult))
                    me = s_[:, 64:96]
                    dv(lambda e: e.tensor_tensor(out=me.rearrange("p (g k) -> p g k", k=8), in0=s_[:, 8:40].rearrange("p (g k) -> p g k", k=8),
                                                 in1=s_[:, 52:56].unsqueeze(2).broadcast_to([128, 4, 8]), op=ALU.add))
                    m1 = s_[:, 56:57]
                    dv(lambda e: e.tensor_reduce(out=m1, in_=me, axis=AX.X, op=ALU.max))
                    oh1 = oh_all[:, 0, t, :]
                    oh2 = oh_all[:, 1, t, :]
                    T.op("dve", lambda e: e.tensor_scalar(out=oh1, in0=me, scalar1=m1, scalar2=None, op0=ALU.is_ge), reads=[B_s], writes=[B_plan])
                    me2 = s_[:, 128:160]
                    T.op("dve", lambda e: e.scalar_tensor_tensor(out=me2, in0=oh1, scalar=-1.0e4, in1=me, op0=ALU.mult, op1=ALU.add),
                         reads=[B_s, B_plan], writes=[B_s])
                    m2 = s_[:, 57:58]
                    dv(lambda e: e.tensor_reduce(out=m2, in_=me2, axis=AX.X, op=ALU.max))
                    T.op("dve", lambda e: e.tensor_scalar(out=oh2, in0=me2, scalar1=m2, scalar2=None, op0=ALU.is_ge), reads=[B_s], writes=[B_plan])
                    dv(lambda e: e.tensor_tensor(out=s_[:, 58:59], in0=m1, in1=m2, op=ALU.subtract))
                    T.op("act", lambda e: e.activation(out=s_[:, 59:60], in_=s_[:, 58:59], func=AF.Sigmoid), reads=[B_s], writes=[B_s])
                    T.op("dve", lambda e: e.tensor_tensor(out=wt12[:, t, 0:1], in0=s_[:, 59:60], in1=s_[:, 42:43], op=ALU.mult), reads=[B_s], writes=[B_wt])
                    T.op("dve", lambda e: e.tensor_tensor(out=wt12[:, t, 1:2], in0=s_[:, 42:43], in1=wt12[:, t, 0:1], op=ALU.subtract),
                         reads=[B_s, B_wt], writes=[B_wt])
                    abf, B_abf = abf_r.next()
                    T.op("dve", lambda e: e.tensor_tensor(out=abf, in0=oh1, in1=oh2, op=ALU.add), reads=[B_plan], writes=[B_abf])
                    T.mm([lambda e: e.matmul(bank(prk[0], 128, 0, 32), lhsT=USTRICT, rhs=abf, start=True, stop=True)], reads=[B_abf, B_cb], writes=[prk[1]])
                    T.mm([lambda e: e.matmul(bank(prk[0], 128, 32, 64), lhsT=ONES, rhs=abf, start=True, stop=True)], reads=[B_abf, B_cb], writes=[prk[1]])
                    rk = s_[:, 160:192]
                    T.op("dve", lambda e: e.tensor_tensor(out=rk, in0=bank(prk[0], 128, 0, 32), in1=carry[:], op=ALU.add),
                         reads=[prk[1], B_carry, B_s], writes=[B_s])
                    T.op("dve", lambda e: e.tensor_tensor(out=carry[:], in0=bank(prk[0], 128, 32, 64), in1=carry[:], op=ALU.add),
                         reads=[prk[1], B_carry, B_s], writes=[B_carry])
                    for k_ in range(2):
                        T.op("dve", lambda e: e.tensor_tensor(out=s_[:, 192:224], in0=oh_all[:, k_, t, :], in1=rk, op=ALU.mult),
                             reads=[B_s, B_plan], writes=[B_s])
                        T.op("dve", lambda e: e.tensor_reduce(out=p12[:, k_, t:t + 1], in_=s_[:, 192:224], axis=AX.X, op=ALU.add),
                             reads=[B_s], writes=[B_plan])
                B_plan.multi = False
                B_wt.multi = False
                g1 = sb_("gplan", [128, 8, NE], F32, phc)
                B_g1 = Buf("g1")
                def gp(fn, extra=()):
                    T.op("dve", fn, reads=[B_g1, B_carry, B_plan, B_wr] + list(extra), writes=[B_g1])
                gp(lambda e: e.tensor_scalar(out=g1[:, 0, :], in0=carry[:], scalar1=63.5, scalar2=1.0 / 128, op0=ALU.add, op1=ALU.mult))
                gp(lambda e: e.tensor_scalar(out=g1[:, 0, :], in0=g1[:, 0, :], scalar1=MAGIC, scalar2=None, op0=ALU.add))
                gp(lambda e: e.tensor_scalar(out=g1[:, 0, :], in0=g1[:, 0, :], scalar1=-MAGIC, scalar2=128.0, op0=ALU.add, op1=ALU.mult))
                gp(lambda e: e.tensor_copy(out=g1[:, 1, :], in_=g1[:, 0, :]))
                src_, dst_ = 1, 2
                for k_ in (1, 2, 4, 8, 16):
                    gp(lambda e: e.tensor_copy(out=g1[:, dst_, 0:k_], in_=g1[:, src_, 0:k_]))
                    gp(lambda e: e.tensor_tensor(out=g1[:, dst_, k_:NE], in0=g1[:, src_, k_:NE], in1=g1[:, src_, 0:NE - k_], op=ALU.add))
                    src_, dst_ = dst_, src_
                pends = g1[:, src_, :]
                pstart = g1[:, 3, :]
                gp(lambda e: e.tensor_tensor(out=pstart, in0=pends, in1=g1[:, 0, :], op=ALU.subtract))
                big = sb_("bigt", [128, max(NCH, NT) * NE], F32, phc)
                destf = sb_("destf", [128, 2, NT], F32, phc)
                for k_ in range(2):
                    gp(lambda e: e.tensor_tensor(out=big[:, 0:NT * NE].rearrange("p (t n) -> p t n", n=NE), in0=oh_all[:, k_, :, :],
                                                 in1=pstart.unsqueeze(1).broadcast_to([128, NT, NE]), op=ALU.mult))
                    gp(lambda e: e.tensor_reduce(out=destf[:, k_, :], in_=big[:, 0:NT * NE].rearrange("p (t n) -> p t n", n=NE), axis=AX.X, op=ALU.add))
                    gp(lambda e: e.tensor_tensor(out=destf[:, k_, :], in0=destf[:, k_, :], in1=p12[:, k_, :], op=ALU.add))
                    T.op("dve", lambda e: e.tensor_copy(out=dest[:, k_, :], in_=destf[:, k_, :]), reads=[B_g1], writes=[B_dest])
                gp(lambda e: e.tensor_tensor(out=big[:, 0:NCH * NE].rearrange("p (c n) -> p c n", n=NE),
                                             in0=pends.unsqueeze(1).broadcast_to([128, NCH, NE]),
                                             in1=cm[:, 1:1 + NCH].unsqueeze(2).broadcast_to([128, NCH, NE]), op=ALU.is_le))
                cef = sb_("cef", [128, 4, NCH], F32, phc)
                gp(lambda e: e.tensor_reduce(out=cef[:, 0, :], in_=big[:, 0:NCH * NE].rearrange("p (c n) -> p c n", n=NE), axis=AX.X, op=ALU.add))
                gp(lambda e: e.tensor_scalar(out=cef[:, 0, :], in0=cef[:, 0, :], scalar1=float(NE - 1), scalar2=None, op0=ALU.min))
                gp(lambda e: e.memset(cef[:, 1, :], 0.0))
                gp(lambda e: e.tensor_tensor(out=cef[:, 1, 1:NCH], in0=cef[:, 0, 1:NCH], in1=cef[:, 0, 0:NCH - 1], op=ALU.is_equal))
                gp(lambda e: e.tensor_scalar(out=cef[:, 2, :], in0=cef[:, 0, :], scalar1=256.0, scalar2=None, op0=ALU.mult))
                gp(lambda e: e.scalar_tensor_tensor(out=cef[:, 2, :], in0=cef[:, 1, :], scalar=16384.0, in1=cef[:, 2, :], op0=ALU.mult, op1=ALU.add))
                for hh in range(2):
                    T.op("dve", lambda e: e.tensor_scalar(out=idx2[:, hh, :], in0=cef[:, 2, :], scalar1=cm[:, 0:1], scalar2=float(hh), op0=ALU.add, op1=ALU.add),
                         reads=[B_g1, B_wr], writes=[B_dest])
                dreg = nc.gpsimd.to_reg(NCH * 128 - 1)
                B_h2.multi = False
                for t in range(NT):
                    for k_ in range(2):
                        T.idma(out=xs, out_offset=bass.IndirectOffsetOnAxis(ap=dest[:, k_, t:t + 1], axis=0), in_=h2tok[:, t, :], in_offset=None,
                               reads=[B_h2, B_dest], writes=[B_xs], owner=B_h2, bounds_check=dreg, oob_is_err=False)
                T.barrier()

        with contextlib.ExitStack() as phd:
            w1v = w1.rearrange("e (p h j) n -> (e p h) (j n)", p=128, h=2, j=4)
            w3v = w3.rearrange("e (p h j) n -> (e p h) (j n)", p=128, h=2, j=4)
            w2v = w2.rearrange("e (p h j) n -> (e p h) (j n)", p=128, h=2, j=2)
            wbt = sb_("wbt", [128, 3, 4096], BF16, phd)
            BW = [Buf(f"W{i}") for i in range(3)]
            T.op("dve", lambda e: e.memset(wbt[:], 0.0), writes=BW)
            for b_ in BW:
                b_.par = True
            xc_r = Ring([sb_(f"xc{i}", [128, D], BF16, phd)[:] for i in range(4)], "xc")
            xT_r = Ring([sb_(f"xT{i}", [128, 8, 128], BF16, phd)[:] for i in range(2)], "xT")
            sl_r = Ring([sb_(f"sl{i}", [128, 512], F32, phd)[:] for i in range(2)], "sl")
            G_r = Ring([sb_(f"G{i}", [128, 512], BF16, phd)[:] for i in range(2)], "G")
            GT_r = Ring([sb_(f"GT{i}", [128, 4, 128], BF16, phd)[:] for i in range(2)], "GT")
            yo_r = Ring([sb_(f"yo{i}", [128, D], F32, phd)[:] for i in range(2)], "yo")
            PXT = ((0, 1), PB[0])
            PH1 = (2, PB[2])
            PH3 = (3, PB[3])
            PGT = (4, PB[4])
            PY = ((5, 6), PB[5])
            st1 = {}

            xcs = {}

            def xload(cn):
                xc_, B_xc_ = xc_r.next()
                T.dma("sp", xc_, xs[cn * 128:(cn + 1) * 128, :], reads=[B_xs], writes=[B_xc_], owner=B_xc_)
                xcs[cn] = (xc_, B_xc_)

            xload(0)
            xload(1)

            def stage1(c):
                if c + 2 < NCH:
                    xload(c + 2)
                xc, B_xc = xcs.pop(c)
                wb = wbt
                (x0, x1b), B_px = PXT
                pxt = ps[:, x0:x0 + 2, :].rearrange("p a (c t) -> p (a c) t", t=128)
                xcv = xc.rearrange("t (p j) -> t j p", j=8)
                T.mm([(lambda e, jj=jj: e.matmul(pxt[:, jj, :], lhsT=xcv[:, jj, :], rhs=IDENT, start=True, stop=True)) for jj in range(8)],
                     reads=[B_xc, B_cb], writes=[B_px])
                xT, B_xT = xT_r.next()
                T.op("act", lambda e: e.activation(out=xT, in_=pxt, func=AF.Copy), reads=[B_px], writes=[B_xT])
                w1b = wb[:, 0, :].rearrange("p (j n) -> p j n", n=512)
                w3b = wb[:, 1, :].rearrange("p (j n) -> p j n", n=512)
                T.mm([(lambda e, jj=jj: e.matmul(bank(PH1[0]), lhsT=xT[:, jj, :], rhs=w1b[:, jj, :], start=(jj == 0), stop=(jj == 7))) for jj in range(8)],
                     reads=[B_xT, BW[0]], writes=[PH1[1]])
                T.mm([(lambda e, jj=jj: e.matmul(bank(PH3[0]), lhsT=xT[:, jj, :], rhs=w3b[:, jj, :], start=(jj == 0), stop=(jj == 7))) for jj in range(8)],
                     reads=[B_xT, BW[1]], writes=[PH3[1]])
                sl, B_sl = sl_r.next()
                T.op("act", lambda e: e.activation(out=sl, in_=bank(PH1[0]), func=AF.Silu), reads=[PH1[1]], writes=[B_sl])
                G, B_G = G_r.next()
                T.op("dve", lambda e: e.tensor_tensor(out=G, in0=bank(PH3[0]), in1=sl, op=ALU.mult), reads=[PH3[1], B_sl], writes=[B_G])
                st1[c] = (G, B_G, wb, None)

            breg = nc.gpsimd.to_reg(NE * 256 - 1)

            def gathers(c, which):
                for mi, wv_ in enumerate((w1v, w3v, w2v)):
                    if (mi == 2) != (which == 2):
                        continue
                    for hh in range(2):
                        T.idma(out=wbt[:, mi, hh * 2048:(hh + 1) * 2048], out_offset=None, in_=wv_,
                               in_offset=bass.IndirectOffsetOnAxis(ap=idx2[:, hh, c:c + 1], axis=0),
                               reads=[B_dest], writes=[BW[mi]], owner=BW[mi], bounds_check=breg, oob_is_err=False)

            def stage2(c):
                G, B_G, wb, B_w = st1.pop(c)
                Gv = G.rearrange("t (p j) -> t j p", j=4)
                pgt = ps[:, PGT[0], :].rearrange("p (c t) -> p c t", t=128)
                T.mm([(lambda e, jj=jj: e.matmul(pgt[:, jj, :], lhsT=Gv[:, jj, :], rhs=IDENT, start=True, stop=True)) for jj in range(4)],
                     reads=[B_G, B_cb], writes=[PGT[1]])
                GT, B_GT = GT_r.next()
                T.op("act", lambda e: e.activation(out=GT, in_=pgt, func=AF.Copy), reads=[PGT[1]], writes=[B_GT])
                w2b = wb[:, 2, :].rearrange("p (j n) -> p j n", n=1024)
                (y0, y1b), B_py = PY
                for half, bi in enumerate((y0, y1b)):
                    T.mm([(lambda e, jj=jj: e.matmul(bank(bi), lhsT=GT[:, jj, :], rhs=w2b[:, jj, half * 512:(half + 1) * 512], start=(jj == 0), stop=(jj == 3)))
                          for jj in range(4)], reads=[B_GT, BW[2]], writes=[B_py])
                yo, B_yo = yo_r.next()
                T.op("dve", lambda e: e.tensor_copy(out=yo.rearrange("p (a n) -> p a n", a=2), in_=ps[:, y0:y0 + 2, :]), reads=[B_py], writes=[B_yo])
                T.dma("sp", ys[c * 128:(c + 1) * 128, :], yo, reads=[B_yo], writes=[B_ys], owner=B_yo)

            gathers(0, 1)
            gathers(0, 2)
            for c in range(NCH + 1):
                if c < NCH:
                    stage1(c)
                if c + 1 < NCH:
                    gathers(c + 1, 1)
                if c >= 1:
                    stage2(c - 1)
                    if c < NCH:
                        gathers(c, 2)
            T.barrier()
            dreg2 = nc.gpsimd.to_reg(NCH * 128 - 1)
            y_r = Ring([sb_(f"yg{i}", [128, 2, D], F32, phd)[:] for i in range(2)], "yg")
            xo_r = Ring([sb_(f"xo{i}", [128, D], F32, phd)[:] for i in range(2)], "xo")
            for t in range(NT):
                r0 = t * 128
                yg, B_yg = y_r.next()
                B_yg.par = True
                for k_ in range(2):
                    T.idma(out=yg[:, k_, :], out_offset=None, in_=ys, in_offset=bass.IndirectOffsetOnAxis(ap=dest[:, k_, t:t + 1], axis=0),
                           reads=[B_ys, B_dest], writes=[B_yg], owner=B_yg, bounds_check=dreg2, oob_is_err=False)
                xo, B_xo = xo_r.next()
                T.dma("sp", xo, out[r0:r0 + 128, :], reads=[B_out], writes=[B_xo], owner=B_xo)
                for k_ in range(2):
                    T.op("dve", lambda e: e.scalar_tensor_tensor(out=xo, in0=yg[:, k_, :], scalar=wt12[:, t, k_:k_ + 1], in1=xo, op0=ALU.mult, op1=ALU.add),
                         reads=[B_yg, B_xo, B_wt], writes=[B_xo])
                T.dma("sp", out[r0:r0 + 128, :], xo, reads=[B_xo], writes=[B_out], owner=B_xo)
            T.barrier()
    return nc


def _consts():
    cst = np.zeros((128, 7, 128), np.float32)
    cst[:, 0, :] = np.eye(128, dtype=np.float32)
    k = np.arange(128)[:, None]
    q = np.arange(128)[None, :]
    cst[:, 1, :] = np.where(k > q, NEG, 0.0)
    cst[:, 2, :] = ((k // 64) == (q // 64)).astype(np.float32)
    for m in range(2):
        for d in range(8):
            cst[m * 64 + d + 8, 3, m * 64 + d] = -1.0
            cst[m * 64 + d, 3, m * 64 + d + 8] = 1.0
    for d in range(32):
        cst[d + 32, 4, d] = -1.0
        cst[d, 4, d + 32] = 1.0
    cst[:, 5, :] = 1.0
    cst[:, 6, :] = (k < q).astype(np.float32)
    cinv = np.zeros((128, 2), np.float32)
    for p in range(128):
        d = p % 64
        if d < 16:
            cinv[p, 0] = THETA ** (-(2.0 * (d % 8)) / 16.0) / (2.0 * math.pi)
        if p < 64:
            cinv[p, 1] = THETA ** (-(2.0 * (p % 32)) / 64.0) / (2.0 * math.pi)
    return cst, cinv


_NC_CACHE = {}


def kernel(**inputs):
    x = np.asarray(inputs["x"])
    Bn, S, _ = x.shape
    if S not in _NC_CACHE:
        _NC_CACHE[S] = build(S)
    nc = _NC_CACHE[S]
    cst, cinv = _consts()
    shared = {}
    for k_, v in inputs.items():
        if k_ in ("x", "positions"):
            continue
        a = np.ascontiguousarray(np.asarray(v))
        shared[k_] = a.reshape(a.shape[1:]) if a.shape[0] == 1 else a
    shared["b_gate"] = shared["b_gate"].reshape(-1)
    shared["cst"] = cst
    shared["cinv"] = cinv
    nch = 2 * (S // 128) + NE
    cmoe = np.zeros((128, 1 + nch), np.float32)
    cmoe[:, 0] = 2.0 * np.arange(128)
    cmoe[:, 1:] = 128.0 * np.arange(nch)[None, :]
    shared["cmoe"] = cmoe
    positions = np.asarray(inputs["positions"]).astype(np.int32)
    in_maps = []
    for b in range(Bn):
        m = dict(shared)
        m["x"] = np.ascontiguousarray(x[b])
        m["pos"] = np.ascontiguousarray(positions[b])
        in_maps.append(m)
    res = run_bass_kernel_spmd(nc, in_maps, core_ids=list(range(Bn)))
    return np.stack([np.asarray(r["out"]) for r in res.results], axis=0).astype(np.float32)
```

```python
import math
import contextlib
import numpy as np
import concourse.bass as bass
import concourse.mybir as mybir
from concourse.bass_utils import run_bass_kernel_spmd

F32 = mybir.dt.float32
BF16 = mybir.dt.bfloat16
I32 = mybir.dt.int32
AF = mybir.ActivationFunctionType
ALU = mybir.AluOpType
AX = mybir.AxisListType

D = 1024
NCORES = 8
IN_COLS = 5824
C_CQ, C_CKV, C_KR, C_GATE = 3072, 3456, 3712, 3776
EPS = 1e-6
THETA = 500000.0
LAMBDA_INIT = 0.8 - 0.6 * math.exp(0.0)
NE = 32
DE = 512
MAGIC = 12582912.0
NEG = -30000.0


class Buf:
    def __init__(self, name, multi=False):
        self.name = name
        self.w = {}
        self.rs = {}
        self.multi = multi
        self.dsem = None
        self.dcnt = 0


class Tracker:
    def __init__(self, nc, stack):
        self.nc = nc
        self.stack = stack
        self.engs = {"pe": nc.tensor, "act": nc.scalar, "dve": nc.vector, "pool": nc.gpsimd, "sp": nc.sync}
        self.sem = {}
        self.cnt = {}
        self.nsem = 0
        self.waited = {}
        self.last = {}
        self.dbufs = []
        for k in self.engs:
            self._newsem(k)

    def _mksem(self, name):
        self.nsem += 1
        return self.stack.enter_context(self.nc.semaphore(f"{name}_{self.nsem}"))

    def _newsem(self, k):
        self.sem[k] = (self._mksem("e" + k), self.nsem)
        self.cnt[k] = 0

    def _wait(self, eng, ev):
        sem, sid, val, src, buf = ev
        if src == "dma":
            val = 16 * buf.dcnt
        elif src == eng and eng == "pe":
            return
        key = (eng, sid)
        if self.waited.get(key, 0) >= val:
            return
        self.waited[key] = val
        self.engs[eng].wait_ge(sem, val)

    def deps(self, eng, reads, writes):
        for b in reads:
            for ev in b.w.values():
                self._wait(eng, ev)
        for b in writes:
            if b.multi:
                continue
            for ev in b.w.values():
                if getattr(b, "par", False) and ev[3] == "dma":
                    continue
                self._wait(eng, ev)
            for ev in b.rs.values():
                self._wait(eng, ev)

    def _record(self, ev, key, reads, writes):
        for b in reads:
            b.rs[key] = ev
        for b in writes:
            if b.multi or getattr(b, "par", False):
                b.w[key] = ev
            else:
                b.w = {key: ev}
                b.rs = {}

    def op(self, eng, fn, reads=(), writes=()):
        self.deps(eng, reads, writes)
        if self.cnt[eng] >= 30000:
            self._newsem(eng)
        self.cnt[eng] += 1
        sem, sid = self.sem[eng]
        ev = (sem, sid, self.cnt[eng], eng, None)
        fn(self.engs[eng]).then_inc(sem, 1)
        self.last[eng] = ev
        self._record(ev, eng, reads, writes)
        return ev

    def mm(self, fns, reads=(), writes=()):
        self.deps("pe", reads, writes)
        for f in fns[:-1]:
            f(self.nc.tensor)
        return self.op("pe", fns[-1], reads, writes)

    def dma(self, q, out, in_, reads=(), writes=(), owner=None, **kw):
        self.deps(q, reads, writes)
        b = owner
        if b.dsem is None:
            b.dsem = (self._mksem("d"), self.nsem)
            self.dbufs.append(b)
        b.dcnt += 1
        sem, sid = b.dsem
        ev = (sem, sid, 16 * b.dcnt, "dma", b)
        self.engs[q].dma_start(out=out, in_=in_, **kw).then_inc(sem, 16)
        self._record(ev, ("dma", sid), reads, writes)
        return ev

    def idma(self, out, out_offset, in_, in_offset, reads=(), writes=(), owner=None, **kw):
        q = "pool"
        self.deps(q, reads, writes)
        b = owner
        if b.dsem is None:
            b.dsem = (self._mksem("d"), self.nsem)
            self.dbufs.append(b)
        b.dcnt += 1
        sem, sid = b.dsem
        ev = (sem, sid, 16 * b.dcnt, "dma", b)
        self.nc.gpsimd.indirect_dma_start(out=out, out_offset=out_offset, in_=in_, in_offset=in_offset, **kw).then_inc(sem, 16)
        self._record(ev, ("dma", sid), reads, writes)
        return ev

    def barrier(self):
        evs = list(self.last.values())
        for e in self.engs:
            for ev in evs:
                self._wait(e, ev)
            for b in self.dbufs:
                self._wait(e, (b.dsem[0], b.dsem[1], 0, "dma", b))


class Ring:
    def __init__(self, aps, name):
        self.items = [(ap, Buf(f"{name}{i}")) for i, ap in enumerate(aps)]
        self.i = 0

    def next(self):
        it = self.items[self.i % len(self.items)]
        self.i += 1
        return it


def build(S):
    NT = S // 128
    NSB = S // 512
    nc = bass.Bass("TRN2", target_bir_lowering=False)

    def din(name, shape, dt=F32):
        return nc.dram_tensor(name, shape, dt, kind="ExternalInput").ap()

    def dscr(name, shape, dt=BF16):
        return nc.dram_tensor(name, shape, dt, kind="Internal").ap()

    x = din("x", [S, D])
    pos = din("pos", [S], I32)
    attn_norm_g = din("attn_norm_g", [D])
    w_in = din("w_in", [D, IN_COLS])
    b_gate = din("b_gate", [2 * D])
    da_q_norm_g = din("da_q_norm_g", [64])
    da_k_norm_g = din("da_k_norm_g", [64])
    lq1 = din("da_lambda_q1", [64])
    lk1 = din("da_lambda_k1", [64])
    lq2 = din("da_lambda_q2", [64])
    lk2 = din("da_lambda_k2", [64])
    subln_g = din("da_subln_g", [128])
    q_lora_g = din("mla_q_lora_g", [384])
    w_uq = din("mla_w_uq", [384, 1536])
    kv_lora_g = din("mla_kv_lora_g", [256])
    w_ukv = din("mla_w_ukv", [256, 2048])
    q_norm_g = din("mla_q_norm_g", [192])
    kn_norm_g = din("mla_k_nope_norm_g", [128])
    kr_norm_g = din("mla_k_rope_norm_g", [64])
    w_o = din("w_o", [D, D])
    ffn_norm_g = din("ffn_norm_g", [D])
    w_group = din("w_group", [D, 4])
    b_group = din("b_group", [4])
    w_router = din("w_router", [D, 32])
    b_router = din("b_router", [32])
    w1 = din("w1", [NE, D, DE])
    w3 = din("w3", [NE, D, DE])
    w2 = din("w2", [NE, DE, D])
    cst = din("cst", [128, 7, 128])
    cinv = din("cinv", [128, 2])
    out = nc.dram_tensor("out", [S, D], F32, kind="ExternalOutput").ap()

    s_qda = dscr("s_qda", [8, 128, S])
    s_kda = dscr("s_kda", [8, 128, S])
    s_vda = dscr("s_vda", [S, D])
    s_qn = dscr("s_qn", [8, 128, S])
    s_qr = dscr("s_qr", [8, 64, S])
    s_kn = dscr("s_kn", [8, 128, S])
    s_kr = dscr("s_kr", [64, S])
    s_vm = dscr("s_vm", [S, D])
    s_gate = dscr("s_gate", [16, 128, S])
    NCH = 2 * NT + NE
    cmoe = din("cmoe", [128, 1 + NCH])
    xs = dscr("s_xs", [NCH * 128, D])
    ys = dscr("s_ys", [NCH * 128, D], F32)
    B_xs = Buf("xs", multi=True)
    B_ys = Buf("ys", multi=True)
    B_scr = Buf("scratch", multi=True)
    B_out = Buf("out", multi=True)

    with contextlib.ExitStack() as stack:
        T = Tracker(nc, stack)

        def sb_(name, shape, dt, st=None):
            return (st or stack).enter_context(nc.sbuf_tensor(name, shape, dt))

        ps = stack.enter_context(nc.psum_tensor("ps", [128, 8, 512], F32))
        PB = [Buf(f"psb{i}") for i in range(8)]

        cb = sb_("cb", [128, 7, 128], BF16)
        B_cb = Buf("cb")
        T.dma("pool", cb[:], cst, writes=[B_cb], owner=B_cb)
        identf = sb_("identf", [128, 128], F32)
        B_cf = Buf("cf")
        T.dma("sp", identf[:], cst[:, 0, :], writes=[B_cf], owner=B_cf)
        IDENT, NEGM, B64, RDA, RMLA, ONES, USTRICT = (cb[:, i, :] for i in range(7))
        wt12 = sb_("wt12", [128, NT, 2], F32)
        B_wt = Buf("wt12", multi=True)
        dest = sb_("dest", [128, 2, NT], I32)
        idx2 = sb_("idx2", [128, 2, NCH], I32)
        B_dest = Buf("dest", multi=True)
        cols = sb_("cols", [128, 64], F32)
        B_cols = Buf("cols")
        ev0 = T.op("dve", lambda e: e.memset(cols[:], 0.0), writes=[B_cols])
        T._wait("sp", ev0)
        ci = [0]

        def col_load(src, n, p0=0):
            c = ci[0]
            ci[0] += 1
            T.dma("sp", cols[p0:p0 + n, c:c + 1], src.rearrange("(p o) -> p o", o=1), writes=[B_cols], owner=B_cols)
            return c

        B_cols.multi = True
        c_inv = ci[0]
        ci[0] += 2
        T.dma("sp", cols[:, c_inv:c_inv + 2], cinv, writes=[B_cols], owner=B_cols)
        c_gA = ci[0]
        for c in range(8):
            col_load(attn_norm_g[c * 128:(c + 1) * 128], 128)
        c_gF = ci[0]
        for c in range(8):
            col_load(ffn_norm_g[c * 128:(c + 1) * 128], 128)
        c_bg = ci[0]
        for c in range(16):
            col_load(b_gate[c * 128:(c + 1) * 128], 128)
        c_gq = col_load(da_q_norm_g, 64)
        ci[0] -= 1
        col_load(da_q_norm_g, 64, 64)
        c_gk = col_load(da_k_norm_g, 64)
        ci[0] -= 1
        col_load(da_k_norm_g, 64, 64)
        c_sub = col_load(subln_g, 128)
        c_gql = ci[0]
        for c in range(3):
            col_load(q_lora_g[c * 128:(c + 1) * 128], 128)
        c_gkvl = ci[0]
        for c in range(2):
            col_load(kv_lora_g[c * 128:(c + 1) * 128], 128)
        c_gqn = col_load(q_norm_g[0:128], 128)
        c_gqr = col_load(q_norm_g[128:192], 64)
        c_gkn = col_load(kn_norm_g, 128)
        c_gkr = col_load(kr_norm_g, 64)
        c_eps = ci[0]
        ci[0] += 1
        c_lam = ci[0]
        ci[0] += 4
        B_cols.multi = False
        T.op("dve", lambda e: e.memset(cols[:, c_eps:c_eps + 1], EPS), reads=[B_cols], writes=[B_cols])

        def col(c, rows=128, p0=0):
            return cols[p0:p0 + rows, c:c + 1]

        lam_t = sb_("lam_t", [128, 4, 64], F32)
        B_lam = Buf("lam", multi=True)
        for i, src in enumerate((lq1, lk1, lq2, lk2)):
            T.dma("sp", lam_t[:, i, :], src.partition_broadcast(128), writes=[B_lam], owner=B_lam)
        B_lam.multi = False
        T.op("dve", lambda e: e.tensor_tensor(out=lam_t[:, 0, :], in0=lam_t[:, 0, :], in1=lam_t[:, 1, :], op=ALU.mult),
             reads=[B_lam], writes=[B_lam])
        T.op("dve", lambda e: e.tensor_tensor(out=lam_t[:, 2, :], in0=lam_t[:, 2, :], in1=lam_t[:, 3, :], op=ALU.mult),
             reads=[B_lam], writes=[B_lam])
        T.op("dve", lambda e: e.tensor_reduce(out=cols[:, c_lam:c_lam + 1], in_=lam_t[:, 0, :], axis=AX.X, op=ALU.add),
             reads=[B_lam], writes=[B_cols])
        T.op("dve", lambda e: e.tensor_reduce(out=cols[:, c_lam + 1:c_lam + 2], in_=lam_t[:, 2, :], axis=AX.X, op=ALU.add),
             reads=[B_lam, B_cols], writes=[B_cols])
        T.op("act", lambda e: e.activation(out=cols[:, c_lam:c_lam + 2], in_=cols[:, c_lam:c_lam + 2], func=AF.Exp),
             reads=[B_cols], writes=[B_cols])
        T.op("dve", lambda e: e.scalar_tensor_tensor(out=cols[:, c_lam + 2:c_lam + 3], in0=cols[:, c_lam + 1:c_lam + 2],
                                                      scalar=-LAMBDA_INIT, in1=cols[:, c_lam:c_lam + 1],
                                                      op0=ALU.add, op1=ALU.subtract), reads=[B_cols], writes=[B_cols])
        c_neglam = c_lam + 2
        T.op("dve", lambda e: e.tensor_scalar(out=cols[:, c_lam + 3:c_lam + 4], in0=cols[:, c_sub:c_sub + 1],
                                               scalar1=1.0 - LAMBDA_INIT, scalar2=None, op0=ALU.mult),
             reads=[B_cols], writes=[B_cols])
        c_subs = c_lam + 3
        rg = sb_("rg", [128, 4, 128], BF16)
        B_rg = Buf("rg")
        for i, (src, c) in enumerate(((RDA, c_gq), (RDA, c_gk), (RMLA, c_gqr), (RMLA, c_gkr))):
            T.op("dve", lambda e, i=i, src=src, c=c: e.tensor_scalar(out=rg[:, i, :], in0=src, scalar1=col(c), scalar2=None,
                                                                     op0=ALU.mult), reads=[B_cb, B_cols], writes=[B_rg])
        RGQ, RGK, RGMQ, RGKR = (rg[:, i, :] for i in range(4))
        CONST = [B_cb, B_cols, B_rg]
        zt = sb_("zt", [128, D], BF16)
        B_zt = Buf("zt")
        T.op("dve", lambda e: e.memset(zt[:], 0.0), writes=[B_zt])
        for c in range(NCH):
            T.dma("sp", xs[c * 128:(c + 1) * 128, :], zt[:], reads=[B_zt], writes=[B_xs], owner=B_zt)

        def bank(i, rows=128, c0=0, c1=512):
            return ps[0:rows, i, c0:c1]

        with contextlib.ExitStack() as ph:
            win = sb_("win", [128, 8, IN_COLS], BF16, ph)
            B_win = Buf("win", multi=True)
            w_in_v = w_in.rearrange("(c p) n -> p c n", p=128)
            for kc in range(8):
                for j in range(0, IN_COLS, 1456):
                    T.dma("pool", win[:, kc, j:j + 1456], w_in_v[:, kc, j:j + 1456], writes=[B_win], owner=B_win)
            wuq = sb_("wuq", [128, 3, 1536], BF16, ph)
            T.dma("pool", wuq[:], w_uq.rearrange("(c p) n -> p c n", p=128), writes=[B_win], owner=B_win)
            wkn = sb_("wkn", [128, 2, 8, 128], BF16, ph)
            wv = sb_("wv", [128, 2, 8, 128], BF16, ph)
            ukv_v = w_ukv.rearrange("(c p) (h t d) -> p c h t d", p=128, t=2, d=128)
            for c in range(2):
                T.dma("pool", wkn[:, c, :, :], ukv_v[:, c, :, 0, :], writes=[B_win], owner=B_win)
                T.dma("pool", wv[:, c, :, :], ukv_v[:, c, :, 1, :], writes=[B_win], owner=B_win)
            hT = sb_("hT", [128, 8, 512], BF16, ph)
            B_hT = Buf("hT")
            xt_r = Ring([sb_(f"xt{i}", [128, D], F32, ph)[:] for i in range(2)], "xt")
            junk = sb_("junk", [128, D], BF16, ph)
            B_junk = Buf("junk")
            xn_r = Ring([sb_(f"xn{i}", [128, D], BF16, ph)[:] for i in range(2)], "xn")
            st1 = sb_("st1", [128, 8], F32, ph)
            st_r = Ring([st1[:, i:i + 1] for i in range(8)], "st")
            posi = sb_("posi", [128, 512], I32, ph)
            B_posi = Buf("posi")
            posf = sb_("posf", [128, 512], F32, ph)
            B_posf = Buf("posf")
            tabs = sb_("tabs", [128, 4, 512], F32, ph)
            B_tab = [Buf(f"tab{i}") for i in range(4)]
            SIND, COSD, SINM, COSM = (tabs[:, i, :] for i in range(4))
            cqn = sb_("cqn", [128, 3, 512], BF16, ph)
            B_cqn = Buf("cqn")
            ckvn = sb_("ckvn", [128, 2, 512], BF16, ph)
            B_ckvn = Buf("ckvn")
            f_r = Ring([sb_(f"f{i}", [128, 512], F32, ph)[:] for i in range(8)], "f")
            h_r = Ring([sb_(f"h{i}", [128, 512], BF16, ph)[:] for i in range(8)], "h")
            o_r = Ring([sb_(f"o{i}", [128, 512], BF16, ph)[:] for i in range(4)], "o")
            ov_r = Ring([sb_(f"ov{i}", [128, D], BF16, ph)[:] for i in range(2)], "ov")
            pa = Ring([0, 1, 2], "pa")
            pa.items = [(i, PB[i]) for i in (0, 1, 2, 6)]
            px = Ring([3, 4, 5], "px")
            px.items = [(i, PB[i]) for i in (3, 4, 5)]
            pt = Ring([6, 7], "pt")
            pt.items = [(i, PB[i]) for i in (7,)]

            def make_rstd(ssq_ap, B_ssq, n, rows):
                sd, B_sd = f_r.next()
                T.op("act", lambda e: e.activation(out=sd[0:rows], in_=ssq_ap, func=AF.Sqrt, scale=1.0 / n,
                                                    bias=col(c_eps, rows)), reads=[B_ssq, B_cols], writes=[B_sd])
                rs, B_rs = f_r.next()
                T.op("dve", lambda e: e.reciprocal(out=rs[0:rows], in_=sd[0:rows]), reads=[B_sd], writes=[B_rs])
                return rs, B_rs

            def square(ps_ap, B_ps, rows):
                sq, B_sq = h_r.next()
                T.op("act", lambda e: e.activation(out=sq[0:rows], in_=ps_ap, func=AF.Square), reads=[B_ps], writes=[B_sq])
                return sq, B_sq

            def store(q, dst, src_ap, B_src):
                T.dma(q, dst, src_ap, reads=[B_src], writes=[B_scr], owner=B_src)

            def rope_finish(rows, ps_ap, B_ps, gc, rgmat, cos, sin, B_cos, B_sin, rs, B_rs, dst):
                cbf, B_cbf = h_r.next()
                T.op("act", lambda e: e.activation(out=cbf[0:rows], in_=ps_ap, func=AF.Copy), reads=[B_ps], writes=[B_cbf])
                bi, B_b = px.next()
                T.mm([lambda e: e.matmul(bank(bi, rows), lhsT=rgmat[0:rows, 0:rows], rhs=cbf[0:rows], start=True, stop=True)],
                     reads=[B_cbf, B_rg], writes=[B_b])
                t1, B_t1 = f_r.next()
                T.op("dve", lambda e: e.scalar_tensor_tensor(out=t1[0:rows], in0=ps_ap, scalar=col(gc, rows), in1=cos[0:rows],
                                                              op0=ALU.mult, op1=ALU.mult), reads=[B_ps, B_cols, B_cos], writes=[B_t1])
                t2, B_t2 = f_r.next()
                T.op("dve", lambda e: e.tensor_tensor(out=t2[0:rows], in0=bank(bi, rows), in1=sin[0:rows], op=ALU.mult),
                     reads=[B_b, B_sin], writes=[B_t2])
                T.op("pool", lambda e: e.tensor_tensor(out=t1[0:rows], in0=t1[0:rows], in1=t2[0:rows], op=ALU.add),
                     reads=[B_t2, B_t1], writes=[B_t1])
                ob, B_ob = o_r.next()
                T.op("pool", lambda e: e.tensor_tensor(out=ob[0:rows], in0=t1[0:rows], in1=rs[0:rows], op=ALU.mult),
                     reads=[B_t1, B_rs], writes=[B_ob])
                store("sp", dst, ob[0:rows], B_ob)

            pre = {}

            def prefetch_sb(sbn):
                tn = sbn * 512
                T.dma("sp", posi[:], pos[tn:tn + 512].partition_broadcast(128), writes=[B_posi], owner=B_posi)
                pre["posi"] = sbn
                pre["xt"] = []
                for tt_ in range(2):
                    xt_, B_xt_ = xt_r.next()
                    T.dma("sp", xt_, x[tn + tt_ * 128:tn + (tt_ + 1) * 128, :], writes=[B_xt_], owner=B_xt_)
                    pre["xt"].append((xt_, B_xt_))

            for sb in range(NSB):
                t0 = sb * 512
                if pre.get("posi") != sb:
                    prefetch_sb(sb)
                T.op("dve", lambda e: e.tensor_copy(out=posf[:], in_=posi[:]), reads=[B_posi], writes=[B_posf])
                for ti, (cc, off) in enumerate(((0, 0.0), (0, 0.25), (1, 0.0), (1, 0.25))):
                    v, B_v = f_r.next()
                    T.op("dve", lambda e: e.tensor_scalar(out=v, in0=posf[:], scalar1=col(c_inv + cc), scalar2=off,
                                                           op0=ALU.mult, op1=ALU.add), reads=[B_posf, B_cols], writes=[B_v])
                    k1, B_k1 = f_r.next()
                    T.op("dve", lambda e: e.tensor_scalar(out=k1, in0=v, scalar1=MAGIC, scalar2=None, op0=ALU.add),
                         reads=[B_v], writes=[B_k1])
                    T.op("dve", lambda e: e.tensor_scalar(out=k1, in0=k1, scalar1=-MAGIC, scalar2=None, op0=ALU.add),
                         reads=[B_k1], writes=[B_k1])
                    T.op("dve", lambda e: e.tensor_tensor(out=v, in0=v, in1=k1, op=ALU.subtract), reads=[B_v, B_k1], writes=[B_v])
                    T.op("act", lambda e: e.activation(out=tabs[:, ti, :], in_=v, func=AF.Sin, scale=2.0 * math.pi),
                         reads=[B_v], writes=[B_tab[ti]])
                for tt in range(4):
                    r0 = t0 + tt * 128
                    if tt < 2:
                        xt, B_xt = pre["xt"][tt]
                    else:
                        xt, B_xt = xt_r.next()
                        T.dma("sp", xt, x[r0:r0 + 128, :], writes=[B_xt], owner=B_xt)
                    ss, B_ss = st_r.next()
                    T.op("act", lambda e: e.activation(out=junk[:], in_=xt, func=AF.Square, accum_out=ss),
                         reads=[B_xt], writes=[B_junk, B_ss])
                    T.op("act", lambda e: e.activation(out=ss, in_=ss, func=AF.Sqrt, scale=1.0 / D, bias=col(c_eps)),
                         reads=[B_ss, B_cols], writes=[B_ss])
                    T.op("dve", lambda e: e.reciprocal(out=ss, in_=ss), reads=[B_ss], writes=[B_ss])
                    xn, B_xn = xn_r.next()
                    T.op("dve", lambda e: e.tensor_scalar(out=xn, in0=xt, scalar1=ss, scalar2=None, op0=ALU.mult),
                         reads=[B_xt, B_ss], writes=[B_xn])
                    bi, B_b = pt.next()
                    pT = ps[:, bi, :].bitcast(BF16).rearrange("p (c t) -> p c t", t=128)
                    T.mm([(lambda e, c=c: e.transpose(pT[:, c, :], xn[:, c * 128:(c + 1) * 128], IDENT)) for c in range(8)],
                         reads=[B_xn, B_cb], writes=[B_b])
                    T.op("dve", lambda e: e.tensor_tensor(out=hT[:, :, tt * 128:(tt + 1) * 128], in0=pT,
                                                           in1=cols[:, c_gA:c_gA + 8].unsqueeze(2).broadcast_to([128, 8, 128]),
                                                           op=ALU.mult), reads=[B_b, B_cols], writes=[B_hT])

                def proj(bi, B_b, wt, c0, ncols, rhs, nk, extra_reads):
                    T.mm([(lambda e, kc=kc: e.matmul(bank(bi, ncols), lhsT=wt[:, kc, c0:c0 + ncols], rhs=rhs[:, kc, :],
                                                     start=(kc == 0), stop=(kc == nk - 1))) for kc in range(nk)],
                         reads=[B_win] + extra_reads, writes=[B_b])

                jobs = []

                def add(front, back):
                    jobs.append((front, back))

                def lat_job(ncs, cbase, gbase, dstt, B_dst):
                    stt_ = {}

                    def front():
                        banks = []
                        for c in range(ncs):
                            bi, B_b = pa.next()
                            proj(bi, B_b, win, cbase + c * 128, 128, hT, 8, [B_hT])
                            banks.append((bi, B_b))
                        stt_["banks"] = banks
                        stt_["sqs"] = [square(bank(bi), B_b, 128) for (bi, B_b) in banks]

                    def back():
                        banks, sqs = stt_["banks"], stt_["sqs"]
                        si, B_s = px.next()
                        T.mm([(lambda e, c=c: e.matmul(bank(si), lhsT=ONES, rhs=sqs[c][0], start=(c == 0), stop=(c == ncs - 1)))
                              for c in range(ncs)], reads=[B_cb] + [q_[1] for q_ in sqs], writes=[B_s])
                        rs, B_rs = make_rstd(bank(si), B_s, 128 * ncs, 128)
                        for c in range(ncs):
                            bi, B_b = banks[c]
                            T.op("dve", lambda e, c=c, bi=bi: e.scalar_tensor_tensor(out=dstt[:, c, :], in0=bank(bi), scalar=col(gbase + c),
                                                                                      in1=rs, op0=ALU.mult, op1=ALU.mult),
                                 reads=[B_b, B_cols, B_rs], writes=[B_dst])
                    add(front, back)

                lat_job(3, C_CQ, c_gql, cqn, B_cqn)

                def kr_job():
                    stt_ = {}

                    def front():
                        bi, B_b = pa.next()
                        proj(bi, B_b, win, C_KR, 64, hT, 8, [B_hT])
                        stt_["b"] = (bi, B_b)
                        stt_["sq"] = square(bank(bi, 64), B_b, 64)

                    def back():
                        bi, B_b = stt_["b"]
                        sq, B_sq = stt_["sq"]
                        si, B_s = px.next()
                        T.mm([lambda e: e.matmul(bank(si, 64), lhsT=cb[0:64, 5, 0:64], rhs=sq[0:64], start=True, stop=True)],
                             reads=[B_sq, B_cb], writes=[B_s])
                        rs, B_rs = make_rstd(bank(si, 64), B_s, 64, 64)
                        rope_finish(64, bank(bi, 64), B_b, c_gkr, RGKR, COSM, SINM, B_tab[3], B_tab[2], rs, B_rs, s_kr[:, t0:t0 + 512])
                    add(front, back)

                kr_job()
                lat_job(2, C_CKV, c_gkvl, ckvn, B_ckvn)

                def da_job(typ, h):
                    stt_ = {}

                    def front():
                        bi, B_b = pa.next()
                        proj(bi, B_b, win, typ * 1024 + h * 128, 128, hT, 8, [B_hT])
                        stt_["b"] = (bi, B_b)
                        stt_["sq"] = square(bank(bi), B_b, 128)

                    def back():
                        bi, B_b = stt_["b"]
                        sq, B_sq = stt_["sq"]
                        si, B_s = px.next()
                        T.mm([lambda e: e.matmul(bank(si), lhsT=B64, rhs=sq, start=True, stop=True)], reads=[B_sq, B_cb], writes=[B_s])
                        rs, B_rs = make_rstd(bank(si), B_s, 64, 128)
                        dst = (s_qda, s_kda)[typ][h, :, t0:t0 + 512]
                        rope_finish(128, bank(bi), B_b, (c_gq, c_gk)[typ], (RGQ, RGK)[typ], COSD, SIND, B_tab[1], B_tab[0], rs, B_rs, dst)
                    add(front, back)

                for typ in range(2):
                    for h in range(8):
                        da_job(typ, h)

                def mq_job(h):
                    stt_ = {}

                    def front():
                        ai, B_a = pa.next()
                        proj(ai, B_a, wuq, h * 192, 128, cqn, 3, [B_cqn])
                        bi2, B_b2 = pa.next()
                        proj(bi2, B_b2, wuq, h * 192 + 128, 64, cqn, 3, [B_cqn])
                        stt_["a"] = (ai, B_a)
                        stt_["b"] = (bi2, B_b2)
                        stt_["sqa"] = square(bank(ai), B_a, 128)
                        stt_["sqb"] = square(bank(bi2, 64), B_b2, 64)

                    def back():
                        ai, B_a = stt_["a"]
                        bi2, B_b2 = stt_["b"]
                        sqa, B_sqa = stt_["sqa"]
                        sqb, B_sqb = stt_["sqb"]
                        si, B_s = px.next()
                        T.mm([lambda e: e.matmul(bank(si), lhsT=ONES, rhs=sqa, start=True, stop=False),
                              lambda e: e.matmul(bank(si), lhsT=cb[0:64, 5, :], rhs=sqb[0:64], start=False, stop=True)],
                             reads=[B_cb, B_sqa, B_sqb], writes=[B_s])
                        rs, B_rs = make_rstd(bank(si), B_s, 192, 128)
                        ob, B_ob = o_r.next()
                        T.op("dve", lambda e: e.scalar_tensor_tensor(out=ob, in0=bank(ai), scalar=col(c_gqn), in1=rs,
                                                                      op0=ALU.mult, op1=ALU.mult), reads=[B_a, B_cols, B_rs], writes=[B_ob])
                        store("sp", s_qn[h, :, t0:t0 + 512], ob, B_ob)
                        rope_finish(64, bank(bi2, 64), B_b2, c_gqr, RGMQ, COSM, SINM, B_tab[3], B_tab[2], rs, B_rs, s_qr[h, :, t0:t0 + 512])
                    add(front, back)

                for h in range(8):
                    mq_job(h)

                def kn_job(h):
                    stt_ = {}

                    def front():
                        bi, B_b = pa.next()
                        T.mm([(lambda e, c=c: e.matmul(bank(bi), lhsT=wkn[:, c, h, :], rhs=ckvn[:, c, :], start=(c == 0), stop=(c == 1)))
                              for c in range(2)], reads=[B_win, B_ckvn], writes=[B_b])
                        stt_["b"] = (bi, B_b)
                        stt_["sq"] = square(bank(bi), B_b, 128)

                    def back():
                        bi, B_b = stt_["b"]
                        sq, B_sq = stt_["sq"]
                        si, B_s = px.next()
                        T.mm([lambda e: e.matmul(bank(si), lhsT=ONES, rhs=sq, start=True, stop=True)], reads=[B_sq, B_cb], writes=[B_s])
                        rs, B_rs = make_rstd(bank(si), B_s, 128, 128)
                        ob, B_ob = o_r.next()
                        T.op("dve", lambda e: e.scalar_tensor_tensor(out=ob, in0=bank(bi), scalar=col(c_gkn), in1=rs,
                                                                      op0=ALU.mult, op1=ALU.mult), reads=[B_b, B_cols, B_rs], writes=[B_ob])
                        store("sp", s_kn[h, :, t0:t0 + 512], ob, B_ob)
                    add(front, back)

                for h in range(8):
                    kn_job(h)

                def v_job(tt, half, kind, ovh):
                    stt_ = {}

                    def front():
                        bi, B_b = pa.next()
                        if kind == 0:
                            T.mm([(lambda e, kc=kc: e.matmul(bank(bi), lhsT=hT[:, kc, tt * 128:(tt + 1) * 128],
                                                             rhs=win[:, kc, 2048 + half * 512:2048 + (half + 1) * 512],
                                                             start=(kc == 0), stop=(kc == 7))) for kc in range(8)],
                                 reads=[B_win, B_hT], writes=[B_b])
                        else:
                            T.mm([(lambda e, c=c: e.matmul(bank(bi), lhsT=ckvn[:, c, tt * 128:(tt + 1) * 128],
                                                           rhs=wv[:, c, half * 4:(half + 1) * 4, :].rearrange("p h d -> p (h d)"),
                                                           start=(c == 0), stop=(c == 1))) for c in range(2)],
                                 reads=[B_win, B_ckvn], writes=[B_b])
                        stt_["b"] = (bi, B_b)

                    def back():
                        bi, B_b = stt_["b"]
                        if half == 0:
                            ovh["ov"] = ov_r.next()
                        ov, B_ov = ovh["ov"]
                        T.op("act", lambda e: e.activation(out=ov[:, half * 512:(half + 1) * 512], in_=bank(bi), func=AF.Copy),
                             reads=[B_b], writes=[B_ov])
                        if half == 1:
                            dstt = (s_vda, s_vm)[kind]
                            store("sp", dstt[t0 + tt * 128:t0 + (tt + 1) * 128, :], ov, B_ov)
                    add(front, back)

                for kind in range(2):
                    for tt in range(4):
                        ovh = {}
                        for half in range(2):
                            v_job(tt, half, kind, ovh)

                def gate_job(c):
                    stt_ = {}

                    def front():
                        bi, B_b = pa.next()
                        proj(bi, B_b, win, C_GATE + c * 128, 128, hT, 8, [B_hT])
                        stt_["b"] = (bi, B_b)

                    def back():
                        bi, B_b = stt_["b"]
                        ob, B_ob = o_r.next()
                        T.op("act", lambda e: e.activation(out=ob, in_=bank(bi), func=AF.Sigmoid, bias=col(c_bg + c)),
                             reads=[B_b, B_cols], writes=[B_ob])
                        store("sp", s_gate[c, :, t0:t0 + 512], ob, B_ob)
                    add(front, back)

                for c in range(16):
                    gate_job(c)

                jobs[0][0]()
                for ji in range(len(jobs)):
                    if ji + 1 < len(jobs):
                        jobs[ji + 1][0]()
                    jobs[ji][1]()
                    if ji == 4 and sb + 1 < NSB:
                        prefetch_sb(sb + 1)
            T.barrier()

        with contextlib.ExitStack() as ph:
            mixT = sb_("mixT", [128, 8, S], BF16, ph)
            B_mix = Buf("mix", multi=True)
            with contextlib.ExitStack() as phb:
                kT_r = Ring([sb_(f"kT{i}", [128, S], BF16, phb)[:] for i in range(2)], "kT")
                vD_r = Ring([sb_(f"vD{i}", [128, NT, 128], BF16, phb)[:] for i in range(2)], "vD")
                kn_r = Ring([sb_(f"kn{i}", [128, S], BF16, phb)[:] for i in range(2)], "kn")
                vM_r = Ring([sb_(f"vM{i}", [128, NT, 128], BF16, phb)[:] for i in range(2)], "vM")
                krT = sb_("krT", [128, S], BF16, phb)
                B_kr = Buf("krT")
                evk = T.op("dve", lambda e: e.memset(krT[64:128, :], 0.0), writes=[B_kr])
                B_kr.par = True
                T.dma("sp", krT[0:64, :], s_kr, reads=[B_scr], writes=[B_kr], owner=B_kr)
                q_r = Ring([sb_(f"q{i}", [128, 4, 512], BF16, phb)[:] for i in range(2)], "q")
                for (qap_, B_qq) in q_r.items:
                    T.op("dve", lambda e: e.memset(qap_, 0.0), writes=[B_qq])
                    B_qq.par = True
                g_r = Ring([sb_(f"g{i}", [128, 2, 512], BF16, phb)[:] for i in range(2)], "g")
                p_r = Ring([sb_(f"p{i}", [128, 512], BF16, phb)[:] for i in range(8)], "p")
                f_r = Ring([sb_(f"fb{i}", [128, 512], F32, phb)[:] for i in range(6)], "fb")
                h_r = Ring([sb_(f"hb{i}", [128, 512], BF16, phb)[:] for i in range(2)], "hb")
                psS = Ring([0, 1, 2], "pS")
                psS.items = [(i, PB[i]) for i in (0, 1, 2, 6)]
                OBANK = {"m": (3, PB[3]), "d0": (4, PB[4]), "d1": (5, PB[5])}
                pz = Ring([0], "pz")
                pz.items = [(i, PB[i]) for i in (7,)]
                onesf = sb_("onesf", [128, 128], F32, phb)
                B_onesf = Buf("onesf")
                T.op("dve", lambda e: e.memset(onesf[:], 1.0), writes=[B_onesf])
                pacc_r = {k_: Ring([sb_(f"pacc{k_}{i}", [128, 512], F32, phb)[:] for i in range(3)], "pacc" + k_) for k_ in ("m", "d1")}
                oc_r = {k_: Ring([sb_(f"oc{k_}{i}", [128, 512], F32, phb)[:] for i in range(1)], "oc" + k_) for k_ in ("m", "d0", "d1")}
                ACCENG = {"m": "dve", "d0": "pe", "d1": "pool"}
                LOOK = 3

                def front(jb):
                    kb, c0, j = jb["kb"], jb["c0"], jb["j"]
                    si, B_s = psS.next()
                    fns = []
                    rd = [B_cb]
                    parts = jb["parts"]
                    for pi_, (kap, qap, rows, B_k, B_q) in enumerate(parts):
                        fns.append(lambda e, kap=kap, qap=qap, rows=rows, pi_=pi_: e.matmul(
                            bank(si, 128, c0, 512), lhsT=kap[0:rows, kb * 128:(kb + 1) * 128], rhs=qap[0:rows, c0:512],
                            start=(pi_ == 0), stop=(pi_ == len(parts) - 1 and j < 0)))
                        rd += [B_k, B_q]
                    if j >= 0:
                        fns.append(lambda e: e.matmul(bank(si, 128, c0, c0 + 128), lhsT=IDENT, rhs=NEGM, start=False, stop=True))
                    T.mm(fns, reads=rd, writes=[B_s])
                    pt_, B_p = p_r.next()
                    T.op("act", lambda e: e.activation(out=pt_[:, c0:512], in_=bank(si, 128, c0, 512), func=AF.Exp, scale=jb["scale"]),
                         reads=[B_s], writes=[B_p])
                    jb["pt"], jb["B_p"] = pt_, B_p

                def back(jb):
                    kb, c0, nkb, key = jb["kb"], jb["c0"], jb["nkb"], jb["key"]
                    pt_, B_p = jb["pt"], jb["B_p"]
                    oi, B_o = OBANK[key]
                    g = jb["grp"]
                    T.mm([lambda e: e.matmul(bank(oi, 128, c0, 512), lhsT=jb["vt"][:, kb, :], rhs=pt_[:, c0:512],
                                             start=(kb == 0), stop=(kb == nkb - 1))], reads=[jb["B_v"], B_p], writes=[B_o])
                    if key == "d0":
                        zb, B_zb = OBANK["m"]
                        T.mm([lambda e: e.matmul(bank(zb, 128, c0, 512), lhsT=ONES, rhs=pt_[:, c0:512],
                                                 start=(kb == 0), stop=(kb == nkb - 1))], reads=[B_cb, B_p], writes=[B_zb])
                    else:
                        if kb < 2:
                            g["pacc"][(key, kb)] = pacc_r[key].next()
                        pa_, B_pa = g["pacc"][(key, kb % 2)]
                        if kb < 2:
                            if c0 > 0:
                                T.op(ACCENG[key], lambda e: e.memset(pa_[:, 0:c0], 0.0), writes=[B_pa])
                            T.op(ACCENG[key], lambda e: e.tensor_copy(out=pa_[:, c0:512], in_=pt_[:, c0:512]), reads=[B_p, B_pa], writes=[B_pa])
                        else:
                            T.op(ACCENG[key], lambda e: e.tensor_tensor(out=pa_[:, c0:512], in0=pa_[:, c0:512], in1=pt_[:, c0:512], op=ALU.add),
                                 reads=[B_p, B_pa], writes=[B_pa])
                    if kb == nkb - 1:
                        oc, B_oc = oc_r[key].next()
                        T.op("act", lambda e: e.activation(out=oc, in_=bank(oi), func=AF.Copy), reads=[B_o], writes=[B_oc])
                        if key == "d0":
                            zi, B_z = OBANK["m"]
                        else:
                            zi, B_z = pz.next()
                            pa0, B_pa0 = g["pacc"][(key, 0)]
                            pa1, B_pa1 = g["pacc"][(key, 1)]
                            T.mm([lambda e: e.matmul(bank(zi), lhsT=onesf[:], rhs=pa0, start=True, stop=False),
                                  lambda e: e.matmul(bank(zi), lhsT=onesf[:], rhs=pa1, start=False, stop=True)],
                                 reads=[B_onesf, B_pa0, B_pa1], writes=[B_z])
                        rz, B_rz = f_r.next()
                        T.op("dve", lambda e: e.reciprocal(out=rz, in_=bank(zi)), reads=[B_z], writes=[B_rz])
                        g["fin"][key] = (oc, B_oc, rz, B_rz)
                        if key == "m":
                            fin_mla(g)
                        elif key == "d1":
                            fin_da(g)

                def fin_mla(g):
                    oc, B_oc, rz, B_rz = g["fin"]["m"]
                    gt, B_g = g["gt"], g["B_g"]
                    T.op("dve", lambda e: e.tensor_tensor(out=oc, in0=oc, in1=rz, op=ALU.mult), reads=[B_oc, B_rz], writes=[B_oc])
                    T.op("dve", lambda e: e.tensor_tensor(out=oc, in0=oc, in1=gt[:, 1, :], op=ALU.mult), reads=[B_oc, B_g], writes=[B_oc])

                def fin_da(g):
                    om, B_om = g["fin"]["m"][0], g["fin"]["m"][1]
                    o0, B_o0, r0, B_r0 = g["fin"]["d0"]
                    o1, B_o1, r1, B_r1 = g["fin"]["d1"]
                    gt, B_g = g["gt"], g["B_g"]
                    h, t0 = g["h"], g["t0"]
                    T.op("dve", lambda e: e.tensor_tensor(out=o0, in0=o0, in1=r0, op=ALU.mult), reads=[B_o0, B_r0], writes=[B_o0])
                    T.op("dve", lambda e: e.scalar_tensor_tensor(out=o1, in0=o1, scalar=col(c_neglam), in1=r1,
                                                                  op0=ALU.mult, op1=ALU.mult), reads=[B_o1, B_r1, B_cols], writes=[B_o1])
                    T.op("dve", lambda e: e.tensor_tensor(out=o0, in0=o0, in1=o1, op=ALU.add), reads=[B_o0, B_o1], writes=[B_o0])
                    sq, B_sq = h_r.next()
                    T.op("pool", lambda e: e.tensor_tensor(out=sq, in0=o0, in1=o0, op=ALU.mult), reads=[B_o0], writes=[B_sq])
                    zi, B_z = pz.next()
                    T.mm([lambda e: e.matmul(bank(zi), lhsT=ONES, rhs=sq, start=True, stop=True)], reads=[B_sq, B_cb], writes=[B_z])
                    sd, B_sd = f_r.next()
                    T.op("act", lambda e: e.activation(out=sd, in_=bank(zi), func=AF.Ln, scale=1.0 / 128, bias=col(c_eps)),
                         reads=[B_z, B_cols], writes=[B_sd])
                    T.op("act", lambda e: e.activation(out=sd, in_=sd, func=AF.Exp, scale=-0.5), reads=[B_sd], writes=[B_sd])
                    T.op("dve", lambda e: e.scalar_tensor_tensor(out=o0, in0=o0, scalar=col(c_subs), in1=sd, op0=ALU.mult, op1=ALU.mult),
                         reads=[B_o0, B_sd, B_cols], writes=[B_o0])
                    T.op("dve", lambda e: e.tensor_tensor(out=o0, in0=o0, in1=gt[:, 0, :], op=ALU.mult), reads=[B_o0, B_g], writes=[B_o0])
                    T.op("dve", lambda e: e.tensor_tensor(out=mixT[:, h, t0:t0 + 512], in0=o0, in1=om, op=ALU.add),
                         reads=[B_o0, B_om], writes=[B_mix])

                pending = []

                def push(jb):
                    front(jb)
                    pending.append(jb)
                    if len(pending) > LOOK:
                        back(pending.pop(0))

                for h in range(8):
                    kT, B_kT = kT_r.next()
                    T.dma("sp", kT, s_kda[h], reads=[B_scr], writes=[B_kT], owner=B_kT)
                    vD, B_vD = vD_r.next()
                    T.dma("sp", vD, s_vda[:, h * 128:(h + 1) * 128].rearrange("(n p) d -> p n d", p=128),
                          reads=[B_scr], writes=[B_vD], owner=B_vD)
                    kn, B_kn = kn_r.next()
                    T.dma("sp", kn, s_kn[h], reads=[B_scr], writes=[B_kn], owner=B_kn)
                    vM, B_vM = vM_r.next()
                    T.dma("sp", vM, s_vm[:, h * 128:(h + 1) * 128].rearrange("(n p) d -> p n d", p=128),
                          reads=[B_scr], writes=[B_vM], owner=B_vM)
                    for sbi in range(NSB):
                        t0 = sbi * 512
                        qt, B_q = q_r.next()
                        T.dma("sp", qt[0:64, 0, :], s_qda[h, 0:64, t0:t0 + 512], reads=[B_scr], writes=[B_q], owner=B_q)
                        T.dma("sp", qt[64:128, 3, :], s_qda[h, 64:128, t0:t0 + 512], reads=[B_scr], writes=[B_q], owner=B_q)
                        T.dma("sp", qt[:, 1, :], s_qn[h, :, t0:t0 + 512], reads=[B_scr], writes=[B_q], owner=B_q)
                        T.dma("sp", qt[0:64, 2, :], s_qr[h, :, t0:t0 + 512], reads=[B_scr], writes=[B_q], owner=B_q)
                        gt, B_g = g_r.next()
                        T.dma("sp", gt[:], s_gate.rearrange("(a c) p t -> c p a t", a=2)[h, :, :, t0:t0 + 512], reads=[B_scr], writes=[B_g], owner=B_g)
                        grp = {"pacc": {}, "fin": {}, "gt": gt, "B_g": B_g, "h": h, "t0": t0}
                        nkb = 4 * sbi + 4
                        for kb in range(nkb):
                            j = kb - 4 * sbi
                            push({"kb": kb, "j": j, "c0": max(j, 0) * 128, "nkb": nkb, "key": "m", "grp": grp, "scale": 192 ** -0.5,
                                  "vt": vM, "B_v": B_vM,
                                  "parts": [(kn, qt[:, 1, :], 128, B_kn, B_q), (krT[:], qt[:, 2, :], 128, B_kr, B_q)]})
                        for kb in range(nkb):
                            j = kb - 4 * sbi
                            for m in range(2):
                                push({"kb": kb, "j": j, "c0": max(j, 0) * 128, "nkb": nkb, "key": f"d{m}", "grp": grp, "scale": 0.125,
                                      "vt": vD, "B_v": B_vD,
                                      "parts": [(kT, qt[:, 3 * m, :], 128, B_kT, B_q)]})
                while pending:
                    back(pending.pop(0))
                T.barrier()

            B_mix.multi = False
            with contextlib.ExitStack() as phc:
                wo = sb_("wo", [128, 8, D], BF16, phc)
                B_wo = Buf("wo")
                T.dma("pool", wo[:], w_o.rearrange("(c p) n -> p c n", p=128), writes=[B_wo], owner=B_wo)
                wr = sb_("wr", [128, 8, 36], F32, phc)
                B_wr = Buf("wr", multi=True)
                T.dma("sp", wr[:, :, 0:4], w_group.rearrange("(c p) n -> p c n", p=128), writes=[B_wr], owner=B_wr)
                T.dma("sp", wr[:, :, 4:36], w_router.rearrange("(c p) n -> p c n", p=128), writes=[B_wr], owner=B_wr)
                br = sb_("br", [128, 36], F32, phc)
                T.dma("sp", br[:, 0:4], b_group.partition_broadcast(128), writes=[B_wr], owner=B_wr)
                T.dma("sp", br[:, 4:36], b_router.partition_broadcast(128), writes=[B_wr], owner=B_wr)
                gfr = sb_("gfr", [128, D], F32, phc)
                T.dma("sp", gfr[:], ffn_norm_g.partition_broadcast(128), writes=[B_wr], owner=B_wr)
                cm = sb_("cm", [128, 1 + NCH], F32, phc)
                T.dma("sp", cm[:], cmoe, writes=[B_wr], owner=B_wr)
                h2tok = sb_("h2tok", [128, NT, D], BF16, phc)
                B_h2 = Buf("h2tok", multi=True)
                oh_all = sb_("oh_all", [128, 2, NT, NE], F32, phc)
                p12 = sb_("p12", [128, 2, NT], F32, phc)
                B_plan = Buf("plan", multi=True)
                carry = sb_("carry", [128, NE], F32, phc)
                B_carry = Buf("carry")
                T.op("dve", lambda e: e.memset(carry[:], 0.0), writes=[B_carry])
                xt_r = Ring([sb_(f"cx{i}", [128, D], F32, phc)[:] for i in range(2)], "cx")
                x1_r = Ring([sb_(f"x1{i}", [128, D], F32, phc)[:] for i in range(2)], "x1")
                junk = sb_("junkc", [128, D], BF16, phc)
                B_junk = Buf("junkc")
                hf_r = Ring([sb_(f"hf{i}", [128, 8, 128], F32, phc)[:] for i in range(1)], "hf")
                sm = sb_("sm", [128, 2, 256], F32, phc)
                sm_r = Ring([sm[:, i, :] for i in range(2)], "sm")
                abf_r = Ring([sb_(f"abf{i}", [128, NE], BF16, phc)[:] for i in range(2)], "abf")
                pc = Ring([0, 1], "pc")
                pc.items = [((0, 1), PB[0]), ((2, 3), PB[2])]
                ptc = Ring([0], "ptc")
                ptc.items = [((4, 5), PB[4])]
                prt = (6, PB[6])
                prk = (7, PB[7])
                for t in range(NT):
                    r0 = t * 128
                    (b0, b1), B_b = pc.next()
                    for half, bi in enumerate((b0, b1)):
                        T.mm([(lambda e, hh=hh: e.matmul(bank(bi), lhsT=mixT[:, hh, r0:r0 + 128], rhs=wo[:, hh, half * 512:(half + 1) * 512],
                                                         start=(hh == 0), stop=(hh == 7))) for hh in range(8)],
                             reads=[B_mix, B_wo], writes=[B_b])
                    xt, B_xt = xt_r.next()
                    T.dma("sp", xt, x[r0:r0 + 128, :], writes=[B_xt], owner=B_xt)
                    x1, B_x1 = x1_r.next()
                    T.op("dve", lambda e: e.tensor_tensor(out=x1.rearrange("p (a n) -> p a n", a=2), in0=ps[:, b0:b0 + 2, :],
                                                           in1=xt.rearrange("p (a n) -> p a n", a=2), op=ALU.add),
                         reads=[B_b, B_xt], writes=[B_x1])
                    T.dma("sp", out[r0:r0 + 128, :], x1, reads=[B_x1], writes=[B_out], owner=B_x1)
                    s_, B_s = sm_r.next()
                    T.op("act", lambda e: e.activation(out=junk[:], in_=x1, func=AF.Square, accum_out=s_[:, 0:1]),
                         reads=[B_x1], writes=[B_junk, B_s])
                    T.op("act", lambda e: e.activation(out=s_[:, 1:2], in_=s_[:, 0:1], func=AF.Sqrt, scale=1.0 / D, bias=col(c_eps)),
                         reads=[B_s, B_cols], writes=[B_s])
                    T.op("dve", lambda e: e.reciprocal(out=s_[:, 2:3], in_=s_[:, 1:2]), reads=[B_s], writes=[B_s])
                    T.op("dve", lambda e: e.tensor_scalar(out=xt, in0=x1, scalar1=s_[:, 2:3], scalar2=None, op0=ALU.mult),
                         reads=[B_x1, B_s], writes=[B_xt])
                    T.op("pool", lambda e: e.tensor_tensor(out=h2tok[:, t, :], in0=xt, in1=gfr[:], op=ALU.mult), reads=[B_xt, B_wr], writes=[B_h2])
                    (tb, _), B_tb = ptc.next()
                    pTf = ps[:, tb:tb + 2, :].rearrange("p a (c t) -> p (a c) t", t=128)
                    T.mm([(lambda e, c=c: e.transpose(pTf[:, c, :], xt[:, c * 128:(c + 1) * 128], identf[:])) for c in range(8)],
                         reads=[B_xt, B_cf], writes=[B_tb])
                    hf, B_hf = hf_r.next()
                    T.op("dve", lambda e: e.tensor_tensor(out=hf, in0=pTf, in1=cols[:, c_gF:c_gF + 8].unsqueeze(2).broadcast_to([128, 8, 128]),
                                                           op=ALU.mult), reads=[B_tb, B_cols], writes=[B_hf])
                    T.mm([(lambda e, c=c: e.matmul(bank(prt[0], 128, 0, 36), lhsT=hf[:, c, :], rhs=wr[:, c, :], start=(c == 0), stop=(c == 7)))
                          for c in range(8)], reads=[B_hf, B_wr], writes=[prt[1]])
                    def dv(fn):
                        T.op("dve", fn, reads=[B_s], writes=[B_s])
                    lg = s_[:, 4:40]
                    T.op("dve", lambda e: e.tensor_tensor(out=lg, in0=bank(prt[0], 128, 0, 36), in1=br[:], op=ALU.add),
                         reads=[prt[1], B_wr, B_s], writes=[B_s])
                    gmax = s_[:, 40:41]
                    dv(lambda e: e.tensor_reduce(out=gmax, in_=s_[:, 4:8], axis=AX.X, op=ALU.max))
                    ohg = s_[:, 44:48]
                    dv(lambda e: e.tensor_scalar(out=ohg, in0=s_[:, 4:8], scalar1=gmax, scalar2=None, op0=ALU.is_ge))
                    dv(lambda e: e.tensor_scalar(out=s_[:, 48:52], in0=s_[:, 4:8], scalar1=gmax, scalar2=None, op0=ALU.subtract))
                    T.op("act", lambda e: e.activation(out=s_[:, 48:52], in_=s_[:, 48:52], func=AF.Exp, accum_out=s_[:, 41:42]),
                         reads=[B_s], writes=[B_s])
                    dv(lambda e: e.reciprocal(out=s_[:, 42:43], in_=s_[:, 41:42]))
                    dv(lambda e: e.tensor_scalar(out=s_[:, 52:56], in0=ohg, scalar1=-1.0, scalar2=1.0e4, op0=ALU.add, op1=ALU.mult))
                    me = s_[:, 64:96]
                    dv(lambda e: e.tensor_tensor(out=me.rearrange("p (g k) -> p g k", k=8), in0=s_[:, 8:40].rearrange("p (g k) -> p g k", k=8),
                                                 in1=s_[:, 52:56].unsqueeze(2).broadcast_to([128, 4, 8]), op=ALU.add))
                    m1 = s_[:, 56:57]
                    dv(lambda e: e.tensor_reduce(out=m1, in_=me, axis=AX.X, op=ALU.max))
                    oh1 = oh_all[:, 0, t, :]
                    oh2 = oh_all[:, 1, t, :]
                    T.op("dve", lambda e: e.tensor_scalar(out=oh1, in0=me, scalar1=m1, scalar2=None, op0=ALU.is_ge), reads=[B_s], writes=[B_plan])
                    me2 = s_[:, 128:160]
                    T.op("dve", lambda e: e.scalar_tensor_tensor(out=me2, in0=oh1, scalar=-1.0e4, in1=me, op0=ALU.mult, op1=ALU.add),
                         reads=[B_s, B_plan], writes=[B_s])
                    m2 = s_[:, 57:58]
                    dv(lambda e: e.tensor_reduce(out=m2, in_=me2, axis=AX.X, op=ALU.max))
                    T.op("dve", lambda e: e.tensor_scalar(out=oh2, in0=me2, scalar1=m2, scalar2=None, op0=ALU.is_ge), reads=[B_s], writes=[B_plan])
                    dv(lambda e: e.tensor_tensor(out=s_[:, 58:59], in0=m1, in1=m2, op=ALU.subtract))
                    T.op("act", lambda e: e.activation(out=s_[:, 59:60], in_=s_[:, 58:59], func=AF.Sigmoid), reads=[B_s], writes=[B_s])
                    T.op("dve", lambda e: e.tensor_tensor(out=wt12[:, t, 0:1], in0=s_[:, 59:60], in1=s_[:, 42:43], op=ALU.mult), reads=[B_s], writes=[B_wt])
                    T.op("dve", lambda e: e.tensor_tensor(out=wt12[:, t, 1:2], in0=s_[:, 42:43], in1=wt12[:, t, 0:1], op=ALU.subtract),
                         reads=[B_s, B_wt], writes=[B_wt])
                    abf, B_abf = abf_r.next()
                    T.op("dve", lambda e: e.tensor_tensor(out=abf, in0=oh1, in1=oh2, op=ALU.add), reads=[B_plan], writes=[B_abf])
                    T.mm([lambda e: e.matmul(bank(prk[0], 128, 0, 32), lhsT=USTRICT, rhs=abf, start=True, stop=True)], reads=[B_abf, B_cb], writes=[prk[1]])
                    T.mm([lambda e: e.matmul(bank(prk[0], 128, 32, 64), lhsT=ONES, rhs=abf, start=True, stop=True)], reads=[B_abf, B_cb], writes=[prk[1]])
                    rk = s_[:, 160:192]
                    T.op("dve", lambda e: e.tensor_tensor(out=rk, in0=bank(prk[0], 128, 0, 32), in1=carry[:], op=ALU.add),
                         reads=[prk[1], B_carry, B_s], writes=[B_s])
                    T.op("dve", lambda e: e.tensor_tensor(out=carry[:], in0=bank(prk[0], 128, 32, 64), in1=carry[:], op=ALU.add),
                         reads=[prk[1], B_carry, B_s], writes=[B_carry])
                    for k_ in range(2):
                        T.op("dve", lambda e: e.tensor_tensor(out=s_[:, 192:224], in0=oh_all[:, k_, t, :], in1=rk, op=ALU.mult),
                             reads=[B_s, B_plan], writes=[B_s])
                        T.op("dve", lambda e: e.tensor_reduce(out=p12[:, k_, t:t + 1], in_=s_[:, 192:224], axis=AX.X, op=ALU.add),
                             reads=[B_s], writes=[B_plan])
                B_plan.multi = False
                B_wt.multi = False
                g1 = sb_("gplan", [128, 8, NE], F32, phc)
                B_g1 = Buf("g1")
                def gp(fn, extra=()):
                    T.op("dve", fn, reads=[B_g1, B_carry, B_plan, B_wr] + list(extra), writes=[B_g1])
                gp(lambda e: e.tensor_scalar(out=g1[:, 0, :], in0=carry[:], scalar1=63.5, scalar2=1.0 / 128, op0=ALU.add, op1=ALU.mult))
                gp(lambda e: e.tensor_scalar(out=g1[:, 0, :], in0=g1[:, 0, :], scalar1=MAGIC, scalar2=None, op0=ALU.add))
                gp(lambda e: e.tensor_scalar(out=g1[:, 0, :], in0=g1[:, 0, :], scalar1=-MAGIC, scalar2=128.0, op0=ALU.add, op1=ALU.mult))
                gp(lambda e: e.tensor_copy(out=g1[:, 1, :], in_=g1[:, 0, :]))
                src_, dst_ = 1, 2
                for k_ in (1, 2, 4, 8, 16):
                    gp(lambda e: e.tensor_copy(out=g1[:, dst_, 0:k_], in_=g1[:, src_, 0:k_]))
                    gp(lambda e: e.tensor_tensor(out=g1[:, dst_, k_:NE], in0=g1[:, src_, k_:NE], in1=g1[:, src_, 0:NE - k_], op=ALU.add))
                    src_, dst_ = dst_, src_
                pends = g1[:, src_, :]
                pstart = g1[:, 3, :]
                gp(lambda e: e.tensor_tensor(out=pstart, in0=pends, in1=g1[:, 0, :], op=ALU.subtract))
                big = sb_("bigt", [128, max(NCH, NT) * NE], F32, phc)
                destf = sb_("destf", [128, 2, NT], F32, phc)
                for k_ in range(2):
                    gp(lambda e: e.tensor_tensor(out=big[:, 0:NT * NE].rearrange("p (t n) -> p t n", n=NE), in0=oh_all[:, k_, :, :],
                                                 in1=pstart.unsqueeze(1).broadcast_to([128, NT, NE]), op=ALU.mult))
                    gp(lambda e: e.tensor_reduce(out=destf[:, k_, :], in_=big[:, 0:NT * NE].rearrange("p (t n) -> p t n", n=NE), axis=AX.X, op=ALU.add))
                    gp(lambda e: e.tensor_tensor(out=destf[:, k_, :], in0=destf[:, k_, :], in1=p12[:, k_, :], op=ALU.add))
                    T.op("dve", lambda e: e.tensor_copy(out=dest[:, k_, :], in_=destf[:, k_, :]), reads=[B_g1], writes=[B_dest])
                gp(lambda e: e.tensor_tensor(out=big[:, 0:NCH * NE].rearrange("p (c n) -> p c n", n=NE),
                                             in0=pends.unsqueeze(1).broadcast_to([128, NCH, NE]),
                                             in1=cm[:, 1:1 + NCH].unsqueeze(2).broadcast_to([128, NCH, NE]), op=ALU.is_le))
                cef = sb_("cef", [128, 4, NCH], F32, phc)
                gp(lambda e: e.tensor_reduce(out=cef[:, 0, :], in_=big[:, 0:NCH * NE].rearrange("p (c n) -> p c n", n=NE), axis=AX.X, op=ALU.add))
                gp(lambda e: e.tensor_scalar(out=cef[:, 0, :], in0=cef[:, 0, :], scalar1=float(NE - 1), scalar2=None, op0=ALU.min))
                gp(lambda e: e.memset(cef[:, 1, :], 0.0))
                gp(lambda e: e.tensor_tensor(out=cef[:, 1, 1:NCH], in0=cef[:, 0, 1:NCH], in1=cef[:, 0, 0:NCH - 1], op=ALU.is_equal))
                gp(lambda e: e.tensor_scalar(out=cef[:, 2, :], in0=cef[:, 0, :], scalar1=256.0, scalar2=None, op0=ALU.mult))
                gp(lambda e: e.scalar_tensor_tensor(out=cef[:, 2, :], in0=cef[:, 1, :], scalar=16384.0, in1=cef[:, 2, :], op0=ALU.mult, op1=ALU.add))
                for hh in range(2):
                    T.op("dve", lambda e: e.tensor_scalar(out=idx2[:, hh, :], in0=cef[:, 2, :], scalar1=cm[:, 0:1], scalar2=float(hh), op0=ALU.add, op1=ALU.add),
                         reads=[B_g1, B_wr], writes=[B_dest])
                dreg = nc.gpsimd.to_reg(NCH * 128 - 1)
                B_h2.multi = False
                for t in range(NT):
                    for k_ in range(2):
                        T.idma(out=xs, out_offset=bass.IndirectOffsetOnAxis(ap=dest[:, k_, t:t + 1], axis=0), in_=h2tok[:, t, :], in_offset=None,
                               reads=[B_h2, B_dest], writes=[B_xs], owner=B_h2, bounds_check=dreg, oob_is_err=False)
                T.barrier()

        with contextlib.ExitStack() as phd:
            w1v = w1.rearrange("e (p h j) n -> (e p h) (j n)", p=128, h=2, j=4)
            w3v = w3.rearrange("e (p h j) n -> (e p h) (j n)", p=128, h=2, j=4)
            w2v = w2.rearrange("e (p h j) n -> (e p h) (j n)", p=128, h=2, j=2)
            wbt = sb_("wbt", [128, 3, 4096], BF16, phd)
            BW = [Buf(f"W{i}") for i in range(3)]
            T.op("dve", lambda e: e.memset(wbt[:], 0.0), writes=BW)
            for b_ in BW:
                b_.par = True
            xc_r = Ring([sb_(f"xc{i}", [128, D], BF16, phd)[:] for i in range(4)], "xc")
            xT_r = Ring([sb_(f"xT{i}", [128, 8, 128], BF16, phd)[:] for i in range(2)], "xT")
            sl_r = Ring([sb_(f"sl{i}", [128, 512], F32, phd)[:] for i in range(2)], "sl")
            G_r = Ring([sb_(f"G{i}", [128, 512], BF16, phd)[:] for i in range(2)], "G")
            GT_r = Ring([sb_(f"GT{i}", [128, 4, 128], BF16, phd)[:] for i in range(2)], "GT")
            yo_r = Ring([sb_(f"yo{i}", [128, D], F32, phd)[:] for i in range(2)], "yo")
            PXT = ((0, 1), PB[0])
            PH1 = (2, PB[2])
            PH3 = (3, PB[3])
            PGT = (4, PB[4])
            PY = ((5, 6), PB[5])
            st1 = {}

            xcs = {}

            def xload(cn):
                xc_, B_xc_ = xc_r.next()
                T.dma("sp", xc_, xs[cn * 128:(cn + 1) * 128, :], reads=[B_xs], writes=[B_xc_], owner=B_xc_)
                xcs[cn] = (xc_, B_xc_)

            xload(0)
            xload(1)

            def stage1(c):
                if c + 2 < NCH:
                    xload(c + 2)
                xc, B_xc = xcs.pop(c)
                wb = wbt
                (x0, x1b), B_px = PXT
                pxt = ps[:, x0:x0 + 2, :].rearrange("p a (c t) -> p (a c) t", t=128)
                xcv = xc.rearrange("t (p j) -> t j p", j=8)
                T.mm([(lambda e, jj=jj: e.matmul(pxt[:, jj, :], lhsT=xcv[:, jj, :], rhs=IDENT, start=True, stop=True)) for jj in range(8)],
                     reads=[B_xc, B_cb], writes=[B_px])
                xT, B_xT = xT_r.next()
                T.op("act", lambda e: e.activation(out=xT, in_=pxt, func=AF.Copy), reads=[B_px], writes=[B_xT])
                w1b = wb[:, 0, :].rearrange("p (j n) -> p j n", n=512)
                w3b = wb[:, 1, :].rearrange("p (j n) -> p j n", n=512)
                T.mm([(lambda e, jj=jj: e.matmul(bank(PH1[0]), lhsT=xT[:, jj, :], rhs=w1b[:, jj, :], start=(jj == 0), stop=(jj == 7))) for jj in range(8)],
                     reads=[B_xT, BW[0]], writes=[PH1[1]])
                T.mm([(lambda e, jj=jj: e.matmul(bank(PH3[0]), lhsT=xT[:, jj, :], rhs=w3b[:, jj, :], start=(jj == 0), stop=(jj == 7))) for jj in range(8)],
                     reads=[B_xT, BW[1]], writes=[PH3[1]])
                sl, B_sl = sl_r.next()
                T.op("act", lambda e: e.activation(out=sl, in_=bank(PH1[0]), func=AF.Silu), reads=[PH1[1]], writes=[B_sl])
                G, B_G = G_r.next()
                T.op("dve", lambda e: e.tensor_tensor(out=G, in0=bank(PH3[0]), in1=sl, op=ALU.mult), reads=[PH3[1], B_sl], writes=[B_G])
                st1[c] = (G, B_G, wb, None)

            breg = nc.gpsimd.to_reg(NE * 256 - 1)

            def gathers(c, which):
                for mi, wv_ in enumerate((w1v, w3v, w2v)):
                    if (mi == 2) != (which == 2):
                        continue
                    for hh in range(2):
                        T.idma(out=wbt[:, mi, hh * 2048:(hh + 1) * 2048], out_offset=None, in_=wv_,
                               in_offset=bass.IndirectOffsetOnAxis(ap=idx2[:, hh, c:c + 1], axis=0),
                               reads=[B_dest], writes=[BW[mi]], owner=BW[mi], bounds_check=breg, oob_is_err=False)

            def stage2(c):
                G, B_G, wb, B_w = st1.pop(c)
                Gv = G.rearrange("t (p j) -> t j p", j=4)
                pgt = ps[:, PGT[0], :].rearrange("p (c t) -> p c t", t=128)
                T.mm([(lambda e, jj=jj: e.matmul(pgt[:, jj, :], lhsT=Gv[:, jj, :], rhs=IDENT, start=True, stop=True)) for jj in range(4)],
                     reads=[B_G, B_cb], writes=[PGT[1]])
                GT, B_GT = GT_r.next()
                T.op("act", lambda e: e.activation(out=GT, in_=pgt, func=AF.Copy), reads=[PGT[1]], writes=[B_GT])
                w2b = wb[:, 2, :].rearrange("p (j n) -> p j n", n=1024)
                (y0, y1b), B_py = PY
                for half, bi in enumerate((y0, y1b)):
                    T.mm([(lambda e, jj=jj: e.matmul(bank(bi), lhsT=GT[:, jj, :], rhs=w2b[:, jj, half * 512:(half + 1) * 512], start=(jj == 0), stop=(jj == 3)))
                          for jj in range(4)], reads=[B_GT, BW[2]], writes=[B_py])
                yo, B_yo = yo_r.next()
                T.op("dve", lambda e: e.tensor_copy(out=yo.rearrange("p (a n) -> p a n", a=2), in_=ps[:, y0:y0 + 2, :]), reads=[B_py], writes=[B_yo])
                T.dma("sp", ys[c * 128:(c + 1) * 128, :], yo, reads=[B_yo], writes=[B_ys], owner=B_yo)

            gathers(0, 1)
            gathers(0, 2)
            for c in range(NCH + 1):
                if c < NCH:
                    stage1(c)
                if c + 1 < NCH:
                    gathers(c + 1, 1)
                if c >= 1:
                    stage2(c - 1)
                    if c < NCH:
                        gathers(c, 2)
            T.barrier()
            dreg2 = nc.gpsimd.to_reg(NCH * 128 - 1)
            y_r = Ring([sb_(f"yg{i}", [128, 2, D], F32, phd)[:] for i in range(2)], "yg")
            xo_r = Ring([sb_(f"xo{i}", [128, D], F32, phd)[:] for i in range(3)], "xo")
            exo = {}

            def eload(tn):
                xo_, B_xo_ = xo_r.next()
                T.dma("sp", xo_, out[tn * 128:(tn + 1) * 128, :], reads=[B_out], writes=[B_xo_], owner=B_xo_)
                exo[tn] = (xo_, B_xo_)

            eload(0)
            for t in range(NT):
                r0 = t * 128
                yg, B_yg = y_r.next()
                B_yg.par = True
                for k_ in range(2):
                    T.idma(out=yg[:, k_, :], out_offset=None, in_=ys, in_offset=bass.IndirectOffsetOnAxis(ap=dest[:, k_, t:t + 1], axis=0),
                           reads=[B_ys, B_dest], writes=[B_yg], owner=B_yg, bounds_check=dreg2, oob_is_err=False)
                if t + 1 < NT:
                    eload(t + 1)
                xo, B_xo = exo.pop(t)
                for k_ in range(2):
                    T.op("dve", lambda e: e.scalar_tensor_tensor(out=xo, in0=yg[:, k_, :], scalar=wt12[:, t, k_:k_ + 1], in1=xo, op0=ALU.mult, op1=ALU.add),
                         reads=[B_yg, B_xo, B_wt], writes=[B_xo])
                T.dma("sp", out[r0:r0 + 128, :], xo, reads=[B_xo], writes=[B_out], owner=B_xo)
            T.barrier()
    return nc


def _consts():
    cst = np.zeros((128, 7, 128), np.float32)
    cst[:, 0, :] = np.eye(128, dtype=np.float32)
    k = np.arange(128)[:, None]
    q = np.arange(128)[None, :]
    cst[:, 1, :] = np.where(k > q, NEG, 0.0)
    cst[:, 2, :] = ((k // 64) == (q // 64)).astype(np.float32)
    for m in range(2):
        for d in range(8):
            cst[m * 64 + d + 8, 3, m * 64 + d] = -1.0
            cst[m * 64 + d, 3, m * 64 + d + 8] = 1.0
    for d in range(32):
        cst[d + 32, 4, d] = -1.0
        cst[d, 4, d + 32] = 1.0
    cst[:, 5, :] = 1.0
    cst[:, 6, :] = (k < q).astype(np.float32)
    cinv = np.zeros((128, 2), np.float32)
    for p in range(128):
        d = p % 64
        if d < 16:
            cinv[p, 0] = THETA ** (-(2.0 * (d % 8)) / 16.0) / (2.0 * math.pi)
        if p < 64:
            cinv[p, 1] = THETA ** (-(2.0 * (p % 32)) / 64.0) / (2.0 * math.pi)
    return cst, cinv


_NC_CACHE = {}


def kernel(**inputs):
    x = np.asarray(inputs["x"])
    Bn, S, _ = x.shape
    if S not in _NC_CACHE:
        _NC_CACHE[S] = build(S)
    nc = _NC_CACHE[S]
    cst, cinv = _consts()
    shared = {}
    for k_, v in inputs.items():
        if k_ in ("x", "positions"):
            continue
        a = np.ascontiguousarray(np.asarray(v))
        shared[k_] = a.reshape(a.shape[1:]) if a.shape[0] == 1 else a
    shared["b_gate"] = shared["b_gate"].reshape(-1)
    shared["cst"] = cst
    shared["cinv"] = cinv
    nch = 2 * (S // 128) + NE
    cmoe = np.zeros((128, 1 + nch), np.float32)
    cmoe[:, 0] = 2.0 * np.arange(128)
    cmoe[:, 1:] = 128.0 * np.arange(nch)[None, :]
    shared["cmoe"] = cmoe
    positions = np.asarray(inputs["positions"]).astype(np.int32)
    in_maps = []
    for b in range(Bn):
        m = dict(shared)
        m["x"] = np.ascontiguousarray(x[b])
        m["pos"] = np.ascontiguousarray(positions[b])
        in_maps.append(m)
    res = run_bass_kernel_spmd(nc, in_maps, core_ids=list(range(Bn)))
    return np.stack([np.asarray(r["out"]) for r in res.results], axis=0).astype(np.float32)
```

```python
import math
import contextlib
import numpy as np
import concourse.bass as bass
import concourse.mybir as mybir
from concourse.bass_utils import run_bass_kernel_spmd

F32 = mybir.dt.float32
BF16 = mybir.dt.bfloat16
I32 = mybir.dt.int32
AF = mybir.ActivationFunctionType
ALU = mybir.AluOpType
AX = mybir.AxisListType

D = 1024
NCORES = 8
IN_COLS = 5824
C_CQ, C_CKV, C_KR, C_GATE = 3072, 3456, 3712, 3776
EPS = 1e-6
THETA = 500000.0
LAMBDA_INIT = 0.8 - 0.6 * math.exp(0.0)
NE = 32
DE = 512
MAGIC = 12582912.0
NEG = -30000.0


class Buf:
    def __init__(self, name, multi=False):
        self.name = name
        self.w = {}
        self.rs = {}
        self.multi = multi
        self.dsem = None
        self.dcnt = 0


class Tracker:
    def __init__(self, nc, stack):
        self.nc = nc
        self.stack = stack
        self.engs = {"pe": nc.tensor, "act": nc.scalar, "dve": nc.vector, "pool": nc.gpsimd, "sp": nc.sync}
        self.sem = {}
        self.cnt = {}
        self.nsem = 0
        self.waited = {}
        self.last = {}
        self.dbufs = []
        for k in self.engs:
            self._newsem(k)

    def _mksem(self, name):
        self.nsem += 1
        return self.stack.enter_context(self.nc.semaphore(f"{name}_{self.nsem}"))

    def _newsem(self, k):
        self.sem[k] = (self._mksem("e" + k), self.nsem)
        self.cnt[k] = 0

    def _wait(self, eng, ev):
        sem, sid, val, src, buf = ev
        if src == "dma":
            val = 16 * buf.dcnt
        elif src == eng and eng == "pe":
            return
        key = (eng, sid)
        if self.waited.get(key, 0) >= val:
            return
        self.waited[key] = val
        self.engs[eng].wait_ge(sem, val)

    def deps(self, eng, reads, writes):
        for b in reads:
            for ev in b.w.values():
                self._wait(eng, ev)
        for b in writes:
            if b.multi:
                continue
            for ev in b.w.values():
                if getattr(b, "par", False) and ev[3] == "dma":
                    continue
                self._wait(eng, ev)
            for ev in b.rs.values():
                self._wait(eng, ev)

    def _record(self, ev, key, reads, writes):
        for b in reads:
            b.rs[key] = ev
        for b in writes:
            if b.multi or getattr(b, "par", False):
                b.w[key] = ev
            else:
                b.w = {key: ev}
                b.rs = {}

    def op(self, eng, fn, reads=(), writes=()):
        self.deps(eng, reads, writes)
        if self.cnt[eng] >= 30000:
            self._newsem(eng)
        self.cnt[eng] += 1
        sem, sid = self.sem[eng]
        ev = (sem, sid, self.cnt[eng], eng, None)
        fn(self.engs[eng]).then_inc(sem, 1)
        self.last[eng] = ev
        self._record(ev, eng, reads, writes)
        return ev

    def mm(self, fns, reads=(), writes=()):
        self.deps("pe", reads, writes)
        for f in fns[:-1]:
            f(self.nc.tensor)
        return self.op("pe", fns[-1], reads, writes)

    def dma(self, q, out, in_, reads=(), writes=(), owner=None, **kw):
        self.deps(q, reads, writes)
        b = owner
        if b.dsem is None:
            b.dsem = (self._mksem("d"), self.nsem)
            self.dbufs.append(b)
        b.dcnt += 1
        sem, sid = b.dsem
        ev = (sem, sid, 16 * b.dcnt, "dma", b)
        self.engs[q].dma_start(out=out, in_=in_, **kw).then_inc(sem, 16)
        self._record(ev, ("dma", sid), reads, writes)
        return ev

    def idma(self, out, out_offset, in_, in_offset, reads=(), writes=(), owner=None, **kw):
        q = "pool"
        self.deps(q, reads, writes)
        b = owner
        if b.dsem is None:
            b.dsem = (self._mksem("d"), self.nsem)
            self.dbufs.append(b)
        b.dcnt += 1
        sem, sid = b.dsem
        ev = (sem, sid, 16 * b.dcnt, "dma", b)
        self.nc.gpsimd.indirect_dma_start(out=out, out_offset=out_offset, in_=in_, in_offset=in_offset, **kw).then_inc(sem, 16)
        self._record(ev, ("dma", sid), reads, writes)
        return ev

    def barrier(self):
        evs = list(self.last.values())
        for e in self.engs:
            for ev in evs:
                self._wait(e, ev)
            for b in self.dbufs:
                self._wait(e, (b.dsem[0], b.dsem[1], 0, "dma", b))


class Ring:
    def __init__(self, aps, name):
        self.items = [(ap, Buf(f"{name}{i}")) for i, ap in enumerate(aps)]
        self.i = 0

    def next(self):
        it = self.items[self.i % len(self.items)]
        self.i += 1
        return it


def build(S):
    NT = S // 128
    NSB = S // 512
    nc = bass.Bass("TRN2", target_bir_lowering=False)

    def din(name, shape, dt=F32):
        return nc.dram_tensor(name, shape, dt, kind="ExternalInput").ap()

    def dscr(name, shape, dt=BF16):
        return nc.dram_tensor(name, shape, dt, kind="Internal").ap()

    x = din("x", [S, D])
    pos = din("pos", [S], I32)
    attn_norm_g = din("attn_norm_g", [D])
    w_in = din("w_in", [D, IN_COLS])
    b_gate = din("b_gate", [2 * D])
    da_q_norm_g = din("da_q_norm_g", [64])
    da_k_norm_g = din("da_k_norm_g", [64])
    lq1 = din("da_lambda_q1", [64])
    lk1 = din("da_lambda_k1", [64])
    lq2 = din("da_lambda_q2", [64])
    lk2 = din("da_lambda_k2", [64])
    subln_g = din("da_subln_g", [128])
    q_lora_g = din("mla_q_lora_g", [384])
    w_uq = din("mla_w_uq", [384, 1536])
    kv_lora_g = din("mla_kv_lora_g", [256])
    w_ukv = din("mla_w_ukv", [256, 2048])
    q_norm_g = din("mla_q_norm_g", [192])
    kn_norm_g = din("mla_k_nope_norm_g", [128])
    kr_norm_g = din("mla_k_rope_norm_g", [64])
    w_o = din("w_o", [D, D])
    ffn_norm_g = din("ffn_norm_g", [D])
    w_group = din("w_group", [D, 4])
    b_group = din("b_group", [4])
    w_router = din("w_router", [D, 32])
    b_router = din("b_router", [32])
    w1 = din("w1", [NE, D, DE])
    w3 = din("w3", [NE, D, DE])
    w2 = din("w2", [NE, DE, D])
    cst = din("cst", [128, 7, 128])
    cinv = din("cinv", [128, 2])
    out = nc.dram_tensor("out", [S, D], F32, kind="ExternalOutput").ap()

    s_qda = dscr("s_qda", [8, 128, S])
    s_kda = dscr("s_kda", [8, 128, S])
    s_vda = dscr("s_vda", [S, D])
    s_qn = dscr("s_qn", [8, 128, S])
    s_qr = dscr("s_qr", [8, 64, S])
    s_kn = dscr("s_kn", [8, 128, S])
    s_kr = dscr("s_kr", [64, S])
    s_vm = dscr("s_vm", [S, D])
    s_gate = dscr("s_gate", [16, 128, S])
    NCH = 2 * NT + NE
    cmoe = din("cmoe", [128, 1 + NCH])
    xs = dscr("s_xs", [NCH * 128, D])
    ys = dscr("s_ys", [NCH * 128, D], F32)
    B_xs = Buf("xs", multi=True)
    B_ys = Buf("ys", multi=True)
    B_scr = Buf("scratch", multi=True)
    B_out = Buf("out", multi=True)

    with contextlib.ExitStack() as stack:
        T = Tracker(nc, stack)

        def sb_(name, shape, dt, st=None):
            return (st or stack).enter_context(nc.sbuf_tensor(name, shape, dt))

        ps = stack.enter_context(nc.psum_tensor("ps", [128, 8, 512], F32))
        PB = [Buf(f"psb{i}") for i in range(8)]

        cb = sb_("cb", [128, 7, 128], BF16)
        B_cb = Buf("cb")
        T.dma("pool", cb[:], cst, writes=[B_cb], owner=B_cb)
        identf = sb_("identf", [128, 128], F32)
        B_cf = Buf("cf")
        T.dma("sp", identf[:], cst[:, 0, :], writes=[B_cf], owner=B_cf)
        IDENT, NEGM, B64, RDA, RMLA, ONES, USTRICT = (cb[:, i, :] for i in range(7))
        wt12 = sb_("wt12", [128, NT, 2], F32)
        B_wt = Buf("wt12", multi=True)
        dest = sb_("dest", [128, 2, NT], I32)
        idx2 = sb_("idx2", [128, 2, NCH], I32)
        B_dest = Buf("dest", multi=True)
        cols = sb_("cols", [128, 64], F32)
        B_cols = Buf("cols")
        ev0 = T.op("dve", lambda e: e.memset(cols[:], 0.0), writes=[B_cols])
        T._wait("sp", ev0)
        ci = [0]

        def col_load(src, n, p0=0):
            c = ci[0]
            ci[0] += 1
            T.dma("sp", cols[p0:p0 + n, c:c + 1], src.rearrange("(p o) -> p o", o=1), writes=[B_cols], owner=B_cols)
            return c

        B_cols.multi = True
        c_inv = ci[0]
        ci[0] += 2
        T.dma("sp", cols[:, c_inv:c_inv + 2], cinv, writes=[B_cols], owner=B_cols)
        c_gA = ci[0]
        for c in range(8):
            col_load(attn_norm_g[c * 128:(c + 1) * 128], 128)
        c_gF = ci[0]
        for c in range(8):
            col_load(ffn_norm_g[c * 128:(c + 1) * 128], 128)
        c_bg = ci[0]
        for c in range(16):
            col_load(b_gate[c * 128:(c + 1) * 128], 128)
        c_gq = col_load(da_q_norm_g, 64)
        ci[0] -= 1
        col_load(da_q_norm_g, 64, 64)
        c_gk = col_load(da_k_norm_g, 64)
        ci[0] -= 1
        col_load(da_k_norm_g, 64, 64)
        c_sub = col_load(subln_g, 128)
        c_gql = ci[0]
        for c in range(3):
            col_load(q_lora_g[c * 128:(c + 1) * 128], 128)
        c_gkvl = ci[0]
        for c in range(2):
            col_load(kv_lora_g[c * 128:(c + 1) * 128], 128)
        c_gqn = col_load(q_norm_g[0:128], 128)
        c_gqr = col_load(q_norm_g[128:192], 64)
        c_gkn = col_load(kn_norm_g, 128)
        c_gkr = col_load(kr_norm_g, 64)
        c_eps = ci[0]
        ci[0] += 1
        c_lam = ci[0]
        ci[0] += 4
        B_cols.multi = False
        T.op("dve", lambda e: e.memset(cols[:, c_eps:c_eps + 1], EPS), reads=[B_cols], writes=[B_cols])

        def col(c, rows=128, p0=0):
            return cols[p0:p0 + rows, c:c + 1]

        lam_t = sb_("lam_t", [128, 4, 64], F32)
        B_lam = Buf("lam", multi=True)
        for i, src in enumerate((lq1, lk1, lq2, lk2)):
            T.dma("sp", lam_t[:, i, :], src.partition_broadcast(128), writes=[B_lam], owner=B_lam)
        B_lam.multi = False
        T.op("dve", lambda e: e.tensor_tensor(out=lam_t[:, 0, :], in0=lam_t[:, 0, :], in1=lam_t[:, 1, :], op=ALU.mult),
             reads=[B_lam], writes=[B_lam])
        T.op("dve", lambda e: e.tensor_tensor(out=lam_t[:, 2, :], in0=lam_t[:, 2, :], in1=lam_t[:, 3, :], op=ALU.mult),
             reads=[B_lam], writes=[B_lam])
        T.op("dve", lambda e: e.tensor_reduce(out=cols[:, c_lam:c_lam + 1], in_=lam_t[:, 0, :], axis=AX.X, op=ALU.add),
             reads=[B_lam], writes=[B_cols])
        T.op("dve", lambda e: e.tensor_reduce(out=cols[:, c_lam + 1:c_lam + 2], in_=lam_t[:, 2, :], axis=AX.X, op=ALU.add),
             reads=[B_lam, B_cols], writes=[B_cols])
        T.op("act", lambda e: e.activation(out=cols[:, c_lam:c_lam + 2], in_=cols[:, c_lam:c_lam + 2], func=AF.Exp),
             reads=[B_cols], writes=[B_cols])
        T.op("dve", lambda e: e.scalar_tensor_tensor(out=cols[:, c_lam + 2:c_lam + 3], in0=cols[:, c_lam + 1:c_lam + 2],
                                                      scalar=-LAMBDA_INIT, in1=cols[:, c_lam:c_lam + 1],
                                                      op0=ALU.add, op1=ALU.subtract), reads=[B_cols], writes=[B_cols])
        c_neglam = c_lam + 2
        T.op("dve", lambda e: e.tensor_scalar(out=cols[:, c_lam + 3:c_lam + 4], in0=cols[:, c_sub:c_sub + 1],
                                               scalar1=1.0 - LAMBDA_INIT, scalar2=None, op0=ALU.mult),
             reads=[B_cols], writes=[B_cols])
        c_subs = c_lam + 3
        rg = sb_("rg", [128, 4, 128], BF16)
        B_rg = Buf("rg")
        for i, (src, c) in enumerate(((RDA, c_gq), (RDA, c_gk), (RMLA, c_gqr), (RMLA, c_gkr))):
            T.op("dve", lambda e, i=i, src=src, c=c: e.tensor_scalar(out=rg[:, i, :], in0=src, scalar1=col(c), scalar2=None,
                                                                     op0=ALU.mult), reads=[B_cb, B_cols], writes=[B_rg])
        RGQ, RGK, RGMQ, RGKR = (rg[:, i, :] for i in range(4))
        CONST = [B_cb, B_cols, B_rg]
        zt = sb_("zt", [128, D], BF16)
        B_zt = Buf("zt")
        T.op("dve", lambda e: e.memset(zt[:], 0.0), writes=[B_zt])
        for c in range(NCH):
            T.dma("sp", xs[c * 128:(c + 1) * 128, :], zt[:], reads=[B_zt], writes=[B_xs], owner=B_zt)

        def bank(i, rows=128, c0=0, c1=512):
            return ps[0:rows, i, c0:c1]

        with contextlib.ExitStack() as ph:
            win = sb_("win", [128, 8, IN_COLS], BF16, ph)
            B_win = Buf("win", multi=True)
            w_in_v = w_in.rearrange("(c p) n -> p c n", p=128)
            for kc in range(8):
                for j in range(0, IN_COLS, 1456):
                    T.dma("pool", win[:, kc, j:j + 1456], w_in_v[:, kc, j:j + 1456], writes=[B_win], owner=B_win)
            wuq = sb_("wuq", [128, 3, 1536], BF16, ph)
            T.dma("pool", wuq[:], w_uq.rearrange("(c p) n -> p c n", p=128), writes=[B_win], owner=B_win)
            wkn = sb_("wkn", [128, 2, 8, 128], BF16, ph)
            wv = sb_("wv", [128, 2, 8, 128], BF16, ph)
            ukv_v = w_ukv.rearrange("(c p) (h t d) -> p c h t d", p=128, t=2, d=128)
            for c in range(2):
                T.dma("pool", wkn[:, c, :, :], ukv_v[:, c, :, 0, :], writes=[B_win], owner=B_win)
                T.dma("pool", wv[:, c, :, :], ukv_v[:, c, :, 1, :], writes=[B_win], owner=B_win)
            hT = sb_("hT", [128, 8, 512], BF16, ph)
            B_hT = Buf("hT")
            xt_r = Ring([sb_(f"xt{i}", [128, D], F32, ph)[:] for i in range(2)], "xt")
            junk = sb_("junk", [128, D], BF16, ph)
            B_junk = Buf("junk")
            xn_r = Ring([sb_(f"xn{i}", [128, D], BF16, ph)[:] for i in range(2)], "xn")
            st1 = sb_("st1", [128, 8], F32, ph)
            st_r = Ring([st1[:, i:i + 1] for i in range(8)], "st")
            posi = sb_("posi", [128, 512], I32, ph)
            B_posi = Buf("posi")
            posf = sb_("posf", [128, 512], F32, ph)
            B_posf = Buf("posf")
            tabs = sb_("tabs", [128, 4, 512], F32, ph)
            B_tab = [Buf(f"tab{i}") for i in range(4)]
            SIND, COSD, SINM, COSM = (tabs[:, i, :] for i in range(4))
            cqn = sb_("cqn", [128, 3, 512], BF16, ph)
            B_cqn = Buf("cqn")
            ckvn = sb_("ckvn", [128, 2, 512], BF16, ph)
            B_ckvn = Buf("ckvn")
            f_r = Ring([sb_(f"f{i}", [128, 512], F32, ph)[:] for i in range(8)], "f")
            h_r = Ring([sb_(f"h{i}", [128, 512], BF16, ph)[:] for i in range(8)], "h")
            o_r = Ring([sb_(f"o{i}", [128, 512], BF16, ph)[:] for i in range(4)], "o")
            ov_r = Ring([sb_(f"ov{i}", [128, D], BF16, ph)[:] for i in range(2)], "ov")
            pa = Ring([0, 1, 2], "pa")
            pa.items = [(i, PB[i]) for i in (0, 1, 2, 6)]
            px = Ring([3, 4, 5], "px")
            px.items = [(i, PB[i]) for i in (3, 4, 5)]
            pt = Ring([6, 7], "pt")
            pt.items = [(i, PB[i]) for i in (7,)]

            def make_rstd(ssq_ap, B_ssq, n, rows):
                sd, B_sd = f_r.next()
                T.op("act", lambda e: e.activation(out=sd[0:rows], in_=ssq_ap, func=AF.Sqrt, scale=1.0 / n,
                                                    bias=col(c_eps, rows)), reads=[B_ssq, B_cols], writes=[B_sd])
                rs, B_rs = f_r.next()
                T.op("dve", lambda e: e.reciprocal(out=rs[0:rows], in_=sd[0:rows]), reads=[B_sd], writes=[B_rs])
                return rs, B_rs

            def square(ps_ap, B_ps, rows):
                sq, B_sq = h_r.next()
                T.op("act", lambda e: e.activation(out=sq[0:rows], in_=ps_ap, func=AF.Square), reads=[B_ps], writes=[B_sq])
                return sq, B_sq

            def store(q, dst, src_ap, B_src):
                T.dma(q, dst, src_ap, reads=[B_src], writes=[B_scr], owner=B_src)

            def rope_finish(rows, ps_ap, B_ps, gc, rgmat, cos, sin, B_cos, B_sin, rs, B_rs, dst):
                cbf, B_cbf = h_r.next()
                T.op("act", lambda e: e.activation(out=cbf[0:rows], in_=ps_ap, func=AF.Copy), reads=[B_ps], writes=[B_cbf])
                bi, B_b = px.next()
                T.mm([lambda e: e.matmul(bank(bi, rows), lhsT=rgmat[0:rows, 0:rows], rhs=cbf[0:rows], start=True, stop=True)],
                     reads=[B_cbf, B_rg], writes=[B_b])
                t1, B_t1 = f_r.next()
                T.op("dve", lambda e: e.scalar_tensor_tensor(out=t1[0:rows], in0=ps_ap, scalar=col(gc, rows), in1=cos[0:rows],
                                                              op0=ALU.mult, op1=ALU.mult), reads=[B_ps, B_cols, B_cos], writes=[B_t1])
                t2, B_t2 = f_r.next()
                T.op("dve", lambda e: e.tensor_tensor(out=t2[0:rows], in0=bank(bi, rows), in1=sin[0:rows], op=ALU.mult),
                     reads=[B_b, B_sin], writes=[B_t2])
                T.op("pool", lambda e: e.tensor_tensor(out=t1[0:rows], in0=t1[0:rows], in1=t2[0:rows], op=ALU.add),
                     reads=[B_t2, B_t1], writes=[B_t1])
                ob, B_ob = o_r.next()
                T.op("pool", lambda e: e.tensor_tensor(out=ob[0:rows], in0=t1[0:rows], in1=rs[0:rows], op=ALU.mult),
                     reads=[B_t1, B_rs], writes=[B_ob])
                store("sp", dst, ob[0:rows], B_ob)

            pre = {}

            def prefetch_sb(sbn):
                tn = sbn * 512
                T.dma("sp", posi[:], pos[tn:tn + 512].partition_broadcast(128), writes=[B_posi], owner=B_posi)
                pre["posi"] = sbn
                pre["xt"] = []
                for tt_ in range(2):
                    xt_, B_xt_ = xt_r.next()
                    T.dma("sp", xt_, x[tn + tt_ * 128:tn + (tt_ + 1) * 128, :], writes=[B_xt_], owner=B_xt_)
                    pre["xt"].append((xt_, B_xt_))

            for sb in range(NSB):
                t0 = sb * 512
                if pre.get("posi") != sb:
                    prefetch_sb(sb)
                T.op("dve", lambda e: e.tensor_copy(out=posf[:], in_=posi[:]), reads=[B_posi], writes=[B_posf])
                for ti, (cc, off) in enumerate(((0, 0.0), (0, 0.25), (1, 0.0), (1, 0.25))):
                    v, B_v = f_r.next()
                    T.op("dve", lambda e: e.tensor_scalar(out=v, in0=posf[:], scalar1=col(c_inv + cc), scalar2=off,
                                                           op0=ALU.mult, op1=ALU.add), reads=[B_posf, B_cols], writes=[B_v])
                    k1, B_k1 = f_r.next()
                    T.op("dve", lambda e: e.tensor_scalar(out=k1, in0=v, scalar1=MAGIC, scalar2=None, op0=ALU.add),
                         reads=[B_v], writes=[B_k1])
                    T.op("dve", lambda e: e.tensor_scalar(out=k1, in0=k1, scalar1=-MAGIC, scalar2=None, op0=ALU.add),
                         reads=[B_k1], writes=[B_k1])
                    T.op("dve", lambda e: e.tensor_tensor(out=v, in0=v, in1=k1, op=ALU.subtract), reads=[B_v, B_k1], writes=[B_v])
                    T.op("act", lambda e: e.activation(out=tabs[:, ti, :], in_=v, func=AF.Sin, scale=2.0 * math.pi),
                         reads=[B_v], writes=[B_tab[ti]])
                for tt in range(4):
                    r0 = t0 + tt * 128
                    if tt < 2:
                        xt, B_xt = pre["xt"][tt]
                    else:
                        xt, B_xt = xt_r.next()
                        T.dma("sp", xt, x[r0:r0 + 128, :], writes=[B_xt], owner=B_xt)
                    ss, B_ss = st_r.next()
                    T.op("act", lambda e: e.activation(out=junk[:], in_=xt, func=AF.Square, accum_out=ss),
                         reads=[B_xt], writes=[B_junk, B_ss])
                    T.op("act", lambda e: e.activation(out=ss, in_=ss, func=AF.Sqrt, scale=1.0 / D, bias=col(c_eps)),
                         reads=[B_ss, B_cols], writes=[B_ss])
                    T.op("dve", lambda e: e.reciprocal(out=ss, in_=ss), reads=[B_ss], writes=[B_ss])
                    xn, B_xn = xn_r.next()
                    T.op("dve", lambda e: e.tensor_scalar(out=xn, in0=xt, scalar1=ss, scalar2=None, op0=ALU.mult),
                         reads=[B_xt, B_ss], writes=[B_xn])
                    bi, B_b = pt.next()
                    pT = ps[:, bi, :].bitcast(BF16).rearrange("p (c t) -> p c t", t=128)
                    T.mm([(lambda e, c=c: e.transpose(pT[:, c, :], xn[:, c * 128:(c + 1) * 128], IDENT)) for c in range(8)],
                         reads=[B_xn, B_cb], writes=[B_b])
                    T.op("dve", lambda e: e.tensor_tensor(out=hT[:, :, tt * 128:(tt + 1) * 128], in0=pT,
                                                           in1=cols[:, c_gA:c_gA + 8].unsqueeze(2).broadcast_to([128, 8, 128]),
                                                           op=ALU.mult), reads=[B_b, B_cols], writes=[B_hT])

                def proj(bi, B_b, wt, c0, ncols, rhs, nk, extra_reads):
                    T.mm([(lambda e, kc=kc: e.matmul(bank(bi, ncols), lhsT=wt[:, kc, c0:c0 + ncols], rhs=rhs[:, kc, :],
                                                     start=(kc == 0), stop=(kc == nk - 1))) for kc in range(nk)],
                         reads=[B_win] + extra_reads, writes=[B_b])

                jobs = []

                def add(front, back):
                    jobs.append((front, back))

                def lat_job(ncs, cbase, gbase, dstt, B_dst):
                    stt_ = {}

                    def front():
                        banks = []
                        for c in range(ncs):
                            bi, B_b = pa.next()
                            proj(bi, B_b, win, cbase + c * 128, 128, hT, 8, [B_hT])
                            banks.append((bi, B_b))
                        stt_["banks"] = banks
                        stt_["sqs"] = [square(bank(bi), B_b, 128) for (bi, B_b) in banks]

                    def back():
                        banks, sqs = stt_["banks"], stt_["sqs"]
                        si, B_s = px.next()
                        T.mm([(lambda e, c=c: e.matmul(bank(si), lhsT=ONES, rhs=sqs[c][0], start=(c == 0), stop=(c == ncs - 1)))
                              for c in range(ncs)], reads=[B_cb] + [q_[1] for q_ in sqs], writes=[B_s])
                        rs, B_rs = make_rstd(bank(si), B_s, 128 * ncs, 128)
                        for c in range(ncs):
                            bi, B_b = banks[c]
                            T.op("dve", lambda e, c=c, bi=bi: e.scalar_tensor_tensor(out=dstt[:, c, :], in0=bank(bi), scalar=col(gbase + c),
                                                                                      in1=rs, op0=ALU.mult, op1=ALU.mult),
                                 reads=[B_b, B_cols, B_rs], writes=[B_dst])
                    add(front, back)

                lat_job(3, C_CQ, c_gql, cqn, B_cqn)

                def kr_job():
                    stt_ = {}

                    def front():
                        bi, B_b = pa.next()
                        proj(bi, B_b, win, C_KR, 64, hT, 8, [B_hT])
                        stt_["b"] = (bi, B_b)
                        stt_["sq"] = square(bank(bi, 64), B_b, 64)

                    def back():
                        bi, B_b = stt_["b"]
                        sq, B_sq = stt_["sq"]
                        si, B_s = px.next()
                        T.mm([lambda e: e.matmul(bank(si, 64), lhsT=cb[0:64, 5, 0:64], rhs=sq[0:64], start=True, stop=True)],
                             reads=[B_sq, B_cb], writes=[B_s])
                        rs, B_rs = make_rstd(bank(si, 64), B_s, 64, 64)
                        rope_finish(64, bank(bi, 64), B_b, c_gkr, RGKR, COSM, SINM, B_tab[3], B_tab[2], rs, B_rs, s_kr[:, t0:t0 + 512])
                    add(front, back)

                kr_job()
                lat_job(2, C_CKV, c_gkvl, ckvn, B_ckvn)

                def da_job(typ, h):
                    stt_ = {}

                    def front():
                        bi, B_b = pa.next()
                        proj(bi, B_b, win, typ * 1024 + h * 128, 128, hT, 8, [B_hT])
                        stt_["b"] = (bi, B_b)
                        stt_["sq"] = square(bank(bi), B_b, 128)

                    def back():
                        bi, B_b = stt_["b"]
                        sq, B_sq = stt_["sq"]
                        si, B_s = px.next()
                        T.mm([lambda e: e.matmul(bank(si), lhsT=B64, rhs=sq, start=True, stop=True)], reads=[B_sq, B_cb], writes=[B_s])
                        rs, B_rs = make_rstd(bank(si), B_s, 64, 128)
                        dst = (s_qda, s_kda)[typ][h, :, t0:t0 + 512]
                        rope_finish(128, bank(bi), B_b, (c_gq, c_gk)[typ], (RGQ, RGK)[typ], COSD, SIND, B_tab[1], B_tab[0], rs, B_rs, dst)
                    add(front, back)

                for typ in range(2):
                    for h in range(8):
                        da_job(typ, h)

                def mq_job(h):
                    stt_ = {}

                    def front():
                        ai, B_a = pa.next()
                        proj(ai, B_a, wuq, h * 192, 128, cqn, 3, [B_cqn])
                        bi2, B_b2 = pa.next()
                        proj(bi2, B_b2, wuq, h * 192 + 128, 64, cqn, 3, [B_cqn])
                        stt_["a"] = (ai, B_a)
                        stt_["b"] = (bi2, B_b2)
                        stt_["sqa"] = square(bank(ai), B_a, 128)
                        stt_["sqb"] = square(bank(bi2, 64), B_b2, 64)

                    def back():
                        ai, B_a = stt_["a"]
                        bi2, B_b2 = stt_["b"]
                        sqa, B_sqa = stt_["sqa"]
                        sqb, B_sqb = stt_["sqb"]
                        si, B_s = px.next()
                        T.mm([lambda e: e.matmul(bank(si), lhsT=ONES, rhs=sqa, start=True, stop=False),
                              lambda e: e.matmul(bank(si), lhsT=cb[0:64, 5, :], rhs=sqb[0:64], start=False, stop=True)],
                             reads=[B_cb, B_sqa, B_sqb], writes=[B_s])
                        rs, B_rs = make_rstd(bank(si), B_s, 192, 128)
                        ob, B_ob = o_r.next()
                        T.op("dve", lambda e: e.scalar_tensor_tensor(out=ob, in0=bank(ai), scalar=col(c_gqn), in1=rs,
                                                                      op0=ALU.mult, op1=ALU.mult), reads=[B_a, B_cols, B_rs], writes=[B_ob])
                        store("sp", s_qn[h, :, t0:t0 + 512], ob, B_ob)
                        rope_finish(64, bank(bi2, 64), B_b2, c_gqr, RGMQ, COSM, SINM, B_tab[3], B_tab[2], rs, B_rs, s_qr[h, :, t0:t0 + 512])
                    add(front, back)

                for h in range(8):
                    mq_job(h)

                def kn_job(h):
                    stt_ = {}

                    def front():
                        bi, B_b = pa.next()
                        T.mm([(lambda e, c=c: e.matmul(bank(bi), lhsT=wkn[:, c, h, :], rhs=ckvn[:, c, :], start=(c == 0), stop=(c == 1)))
                              for c in range(2)], reads=[B_win, B_ckvn], writes=[B_b])
                        stt_["b"] = (bi, B_b)
                        stt_["sq"] = square(bank(bi), B_b, 128)

                    def back():
                        bi, B_b = stt_["b"]
                        sq, B_sq = stt_["sq"]
                        si, B_s = px.next()
                        T.mm([lambda e: e.matmul(bank(si), lhsT=ONES, rhs=sq, start=True, stop=True)], reads=[B_sq, B_cb], writes=[B_s])
                        rs, B_rs = make_rstd(bank(si), B_s, 128, 128)
                        ob, B_ob = o_r.next()
                        T.op("dve", lambda e: e.scalar_tensor_tensor(out=ob, in0=bank(bi), scalar=col(c_gkn), in1=rs,
                                                                      op0=ALU.mult, op1=ALU.mult), reads=[B_b, B_cols, B_rs], writes=[B_ob])
                        store("sp", s_kn[h, :, t0:t0 + 512], ob, B_ob)
                    add(front, back)

                for h in range(8):
                    kn_job(h)

                def v_job(tt, half, kind, ovh):
                    stt_ = {}

                    def front():
                        bi, B_b = pa.next()
                        if kind == 0:
                            T.mm([(lambda e, kc=kc: e.matmul(bank(bi), lhsT=hT[:, kc, tt * 128:(tt + 1) * 128],
                                                             rhs=win[:, kc, 2048 + half * 512:2048 + (half + 1) * 512],
                                                             start=(kc == 0), stop=(kc == 7))) for kc in range(8)],
                                 reads=[B_win, B_hT], writes=[B_b])
                        else:
                            T.mm([(lambda e, c=c: e.matmul(bank(bi), lhsT=ckvn[:, c, tt * 128:(tt + 1) * 128],
                                                           rhs=wv[:, c, half * 4:(half + 1) * 4, :].rearrange("p h d -> p (h d)"),
                                                           start=(c == 0), stop=(c == 1))) for c in range(2)],
                                 reads=[B_win, B_ckvn], writes=[B_b])
                        stt_["b"] = (bi, B_b)

                    def back():
                        bi, B_b = stt_["b"]
                        if half == 0:
                            ovh["ov"] = ov_r.next()
                        ov, B_ov = ovh["ov"]
                        T.op("act", lambda e: e.activation(out=ov[:, half * 512:(half + 1) * 512], in_=bank(bi), func=AF.Copy),
                             reads=[B_b], writes=[B_ov])
                        if half == 1:
                            dstt = (s_vda, s_vm)[kind]
                            store("sp", dstt[t0 + tt * 128:t0 + (tt + 1) * 128, :], ov, B_ov)
                    add(front, back)

                for kind in range(2):
                    for tt in range(4):
                        ovh = {}
                        for half in range(2):
                            v_job(tt, half, kind, ovh)

                def gate_job(c):
                    stt_ = {}

                    def front():
                        bi, B_b = pa.next()
                        proj(bi, B_b, win, C_GATE + c * 128, 128, hT, 8, [B_hT])
                        stt_["b"] = (bi, B_b)

                    def back():
                        bi, B_b = stt_["b"]
                        ob, B_ob = o_r.next()
                        T.op("act", lambda e: e.activation(out=ob, in_=bank(bi), func=AF.Sigmoid, bias=col(c_bg + c)),
                             reads=[B_b, B_cols], writes=[B_ob])
                        store("sp", s_gate[c, :, t0:t0 + 512], ob, B_ob)
                    add(front, back)

                for c in range(16):
                    gate_job(c)

                jobs[0][0]()
                for ji in range(len(jobs)):
                    if ji + 1 < len(jobs):
                        jobs[ji + 1][0]()
                    jobs[ji][1]()
                    if ji == 4 and sb + 1 < NSB:
                        prefetch_sb(sb + 1)
            T.barrier()

        with contextlib.ExitStack() as ph:
            mixT = sb_("mixT", [128, 8, S], BF16, ph)
            B_mix = Buf("mix", multi=True)
            with contextlib.ExitStack() as phb:
                kT_r = Ring([sb_(f"kT{i}", [128, S], BF16, phb)[:] for i in range(2)], "kT")
                vD_r = Ring([sb_(f"vD{i}", [128, NT, 128], BF16, phb)[:] for i in range(2)], "vD")
                kn_r = Ring([sb_(f"kn{i}", [128, S], BF16, phb)[:] for i in range(2)], "kn")
                vM_r = Ring([sb_(f"vM{i}", [128, NT, 128], BF16, phb)[:] for i in range(2)], "vM")
                krT = sb_("krT", [128, S], BF16, phb)
                B_kr = Buf("krT")
                evk = T.op("dve", lambda e: e.memset(krT[64:128, :], 0.0), writes=[B_kr])
                B_kr.par = True
                T.dma("sp", krT[0:64, :], s_kr, reads=[B_scr], writes=[B_kr], owner=B_kr)
                q_r = Ring([sb_(f"q{i}", [128, 4, 512], BF16, phb)[:] for i in range(2)], "q")
                for (qap_, B_qq) in q_r.items:
                    T.op("dve", lambda e: e.memset(qap_, 0.0), writes=[B_qq])
                    B_qq.par = True
                g_r = Ring([sb_(f"g{i}", [128, 2, 512], BF16, phb)[:] for i in range(2)], "g")
                p_r = Ring([sb_(f"p{i}", [128, 512], BF16, phb)[:] for i in range(8)], "p")
                f_r = Ring([sb_(f"fb{i}", [128, 512], F32, phb)[:] for i in range(6)], "fb")
                h_r = Ring([sb_(f"hb{i}", [128, 512], BF16, phb)[:] for i in range(2)], "hb")
                psS = Ring([0, 1, 2], "pS")
                psS.items = [(i, PB[i]) for i in (0, 1, 2, 6)]
                OBANK = {"m": (3, PB[3]), "d0": (4, PB[4]), "d1": (5, PB[5])}
                pz = Ring([0], "pz")
                pz.items = [(i, PB[i]) for i in (7,)]
                onesf = sb_("onesf", [128, 128], F32, phb)
                B_onesf = Buf("onesf")
                T.op("dve", lambda e: e.memset(onesf[:], 1.0), writes=[B_onesf])
                pacc_r = {k_: Ring([sb_(f"pacc{k_}{i}", [128, 512], F32, phb)[:] for i in range(3)], "pacc" + k_) for k_ in ("m", "d1")}
                oc_r = {k_: Ring([sb_(f"oc{k_}{i}", [128, 512], F32, phb)[:] for i in range(1)], "oc" + k_) for k_ in ("m", "d0", "d1")}
                ACCENG = {"m": "dve", "d0": "pe", "d1": "pool"}
                LOOK = 3

                def front(jb):
                    kb, c0, j = jb["kb"], jb["c0"], jb["j"]
                    si, B_s = psS.next()
                    fns = []
                    rd = [B_cb]
                    parts = jb["parts"]
                    for pi_, (kap, qap, rows, B_k, B_q) in enumerate(parts):
                        fns.append(lambda e, kap=kap, qap=qap, rows=rows, pi_=pi_: e.matmul(
                            bank(si, 128, c0, 512), lhsT=kap[0:rows, kb * 128:(kb + 1) * 128], rhs=qap[0:rows, c0:512],
                            start=(pi_ == 0), stop=(pi_ == len(parts) - 1 and j < 0)))
                        rd += [B_k, B_q]
                    if j >= 0:
                        fns.append(lambda e: e.matmul(bank(si, 128, c0, c0 + 128), lhsT=IDENT, rhs=NEGM, start=False, stop=True))
                    T.mm(fns, reads=rd, writes=[B_s])
                    pt_, B_p = p_r.next()
                    T.op("act", lambda e: e.activation(out=pt_[:, c0:512], in_=bank(si, 128, c0, 512), func=AF.Exp, scale=jb["scale"]),
                         reads=[B_s], writes=[B_p])
                    jb["pt"], jb["B_p"] = pt_, B_p

                def back(jb):
                    kb, c0, nkb, key = jb["kb"], jb["c0"], jb["nkb"], jb["key"]
                    pt_, B_p = jb["pt"], jb["B_p"]
                    oi, B_o = OBANK[key]
                    g = jb["grp"]
                    T.mm([lambda e: e.matmul(bank(oi, 128, c0, 512), lhsT=jb["vt"][:, kb, :], rhs=pt_[:, c0:512],
                                             start=(kb == 0), stop=(kb == nkb - 1))], reads=[jb["B_v"], B_p], writes=[B_o])
                    if key == "d0":
                        zb, B_zb = OBANK["m"]
                        T.mm([lambda e: e.matmul(bank(zb, 128, c0, 512), lhsT=ONES, rhs=pt_[:, c0:512],
                                                 start=(kb == 0), stop=(kb == nkb - 1))], reads=[B_cb, B_p], writes=[B_zb])
                    else:
                        if kb < 2:
                            g["pacc"][(key, kb)] = pacc_r[key].next()
                        pa_, B_pa = g["pacc"][(key, kb % 2)]
                        if kb < 2:
                            if c0 > 0:
                                T.op(ACCENG[key], lambda e: e.memset(pa_[:, 0:c0], 0.0), writes=[B_pa])
                            T.op(ACCENG[key], lambda e: e.tensor_copy(out=pa_[:, c0:512], in_=pt_[:, c0:512]), reads=[B_p, B_pa], writes=[B_pa])
                        else:
                            T.op(ACCENG[key], lambda e: e.tensor_tensor(out=pa_[:, c0:512], in0=pa_[:, c0:512], in1=pt_[:, c0:512], op=ALU.add),
                                 reads=[B_p, B_pa], writes=[B_pa])
                    if kb == nkb - 1:
                        oc, B_oc = oc_r[key].next()
                        T.op("act", lambda e: e.activation(out=oc, in_=bank(oi), func=AF.Copy), reads=[B_o], writes=[B_oc])
                        if key == "d0":
                            zi, B_z = OBANK["m"]
                        else:
                            zi, B_z = pz.next()
                            pa0, B_pa0 = g["pacc"][(key, 0)]
                            pa1, B_pa1 = g["pacc"][(key, 1)]
                            T.mm([lambda e: e.matmul(bank(zi), lhsT=onesf[:], rhs=pa0, start=True, stop=False),
                                  lambda e: e.matmul(bank(zi), lhsT=onesf[:], rhs=pa1, start=False, stop=True)],
                                 reads=[B_onesf, B_pa0, B_pa1], writes=[B_z])
                        rz, B_rz = f_r.next()
                        T.op("dve", lambda e: e.reciprocal(out=rz, in_=bank(zi)), reads=[B_z], writes=[B_rz])
                        g["fin"][key] = (oc, B_oc, rz, B_rz)
                        if key == "m":
                            fin_mla(g)
                        elif key == "d1":
                            fin_da(g)

                def fin_mla(g):
                    oc, B_oc, rz, B_rz = g["fin"]["m"]
                    gt, B_g = g["gt"], g["B_g"]
                    T.op("dve", lambda e: e.tensor_tensor(out=oc, in0=oc, in1=rz, op=ALU.mult), reads=[B_oc, B_rz], writes=[B_oc])
                    T.op("dve", lambda e: e.tensor_tensor(out=oc, in0=oc, in1=gt[:, 1, :], op=ALU.mult), reads=[B_oc, B_g], writes=[B_oc])

                def fin_da(g):
                    om, B_om = g["fin"]["m"][0], g["fin"]["m"][1]
                    o0, B_o0, r0, B_r0 = g["fin"]["d0"]
                    o1, B_o1, r1, B_r1 = g["fin"]["d1"]
                    gt, B_g = g["gt"], g["B_g"]
                    h, t0 = g["h"], g["t0"]
                    T.op("dve", lambda e: e.tensor_tensor(out=o0, in0=o0, in1=r0, op=ALU.mult), reads=[B_o0, B_r0], writes=[B_o0])
                    T.op("dve", lambda e: e.scalar_tensor_tensor(out=o1, in0=o1, scalar=col(c_neglam), in1=r1,
                                                                  op0=ALU.mult, op1=ALU.mult), reads=[B_o1, B_r1, B_cols], writes=[B_o1])
                    T.op("dve", lambda e: e.tensor_tensor(out=o0, in0=o0, in1=o1, op=ALU.add), reads=[B_o0, B_o1], writes=[B_o0])
                    sq, B_sq = h_r.next()
                    T.op("pool", lambda e: e.tensor_tensor(out=sq, in0=o0, in1=o0, op=ALU.mult), reads=[B_o0], writes=[B_sq])
                    zi, B_z = pz.next()
                    T.mm([lambda e: e.matmul(bank(zi), lhsT=ONES, rhs=sq, start=True, stop=True)], reads=[B_sq, B_cb], writes=[B_z])
                    sd, B_sd = f_r.next()
                    T.op("act", lambda e: e.activation(out=sd, in_=bank(zi), func=AF.Ln, scale=1.0 / 128, bias=col(c_eps)),
                         reads=[B_z, B_cols], writes=[B_sd])
                    T.op("act", lambda e: e.activation(out=sd, in_=sd, func=AF.Exp, scale=-0.5), reads=[B_sd], writes=[B_sd])
                    T.op("dve", lambda e: e.scalar_tensor_tensor(out=o0, in0=o0, scalar=col(c_subs), in1=sd, op0=ALU.mult, op1=ALU.mult),
                         reads=[B_o0, B_sd, B_cols], writes=[B_o0])
                    T.op("dve", lambda e: e.tensor_tensor(out=o0, in0=o0, in1=gt[:, 0, :], op=ALU.mult), reads=[B_o0, B_g], writes=[B_o0])
                    T.op("dve", lambda e: e.tensor_tensor(out=mixT[:, h, t0:t0 + 512], in0=o0, in1=om, op=ALU.add),
                         reads=[B_o0, B_om], writes=[B_mix])

                pending = []

                def push(jb):
                    front(jb)
                    pending.append(jb)
                    if len(pending) > LOOK:
                        back(pending.pop(0))

                for h in range(8):
                    kT, B_kT = kT_r.next()
                    T.dma("sp", kT, s_kda[h], reads=[B_scr], writes=[B_kT], owner=B_kT)
                    vD, B_vD = vD_r.next()
                    T.dma("sp", vD, s_vda[:, h * 128:(h + 1) * 128].rearrange("(n p) d -> p n d", p=128),
                          reads=[B_scr], writes=[B_vD], owner=B_vD)
                    kn, B_kn = kn_r.next()
                    T.dma("sp", kn, s_kn[h], reads=[B_scr], writes=[B_kn], owner=B_kn)
                    vM, B_vM = vM_r.next()
                    T.dma("sp", vM, s_vm[:, h * 128:(h + 1) * 128].rearrange("(n p) d -> p n d", p=128),
                          reads=[B_scr], writes=[B_vM], owner=B_vM)
                    for sbi in range(NSB):
                        t0 = sbi * 512
                        qt, B_q = q_r.next()
                        T.dma("sp", qt[0:64, 0, :], s_qda[h, 0:64, t0:t0 + 512], reads=[B_scr], writes=[B_q], owner=B_q)
                        T.dma("sp", qt[64:128, 3, :], s_qda[h, 64:128, t0:t0 + 512], reads=[B_scr], writes=[B_q], owner=B_q)
                        T.dma("sp", qt[:, 1, :], s_qn[h, :, t0:t0 + 512], reads=[B_scr], writes=[B_q], owner=B_q)
                        T.dma("sp", qt[0:64, 2, :], s_qr[h, :, t0:t0 + 512], reads=[B_scr], writes=[B_q], owner=B_q)
                        gt, B_g = g_r.next()
                        T.dma("sp", gt[:], s_gate.rearrange("(a c) p t -> c p a t", a=2)[h, :, :, t0:t0 + 512], reads=[B_scr], writes=[B_g], owner=B_g)
                        grp = {"pacc": {}, "fin": {}, "gt": gt, "B_g": B_g, "h": h, "t0": t0}
                        nkb = 4 * sbi + 4
                        for kb in range(nkb):
                            j = kb - 4 * sbi
                            push({"kb": kb, "j": j, "c0": max(j, 0) * 128, "nkb": nkb, "key": "m", "grp": grp, "scale": 192 ** -0.5,
                                  "vt": vM, "B_v": B_vM,
                                  "parts": [(kn, qt[:, 1, :], 128, B_kn, B_q), (krT[:], qt[:, 2, :], 128, B_kr, B_q)]})
                        for kb in range(nkb):
                            j = kb - 4 * sbi
                            for m in range(2):
                                push({"kb": kb, "j": j, "c0": max(j, 0) * 128, "nkb": nkb, "key": f"d{m}", "grp": grp, "scale": 0.125,
                                      "vt": vD, "B_v": B_vD,
                                      "parts": [(kT, qt[:, 3 * m, :], 128, B_kT, B_q)]})
                while pending:
                    back(pending.pop(0))
                T.barrier()

            B_mix.multi = False
            with contextlib.ExitStack() as phc:
                wo = sb_("wo", [128, 8, D], BF16, phc)
                B_wo = Buf("wo")
                T.dma("pool", wo[:], w_o.rearrange("(c p) n -> p c n", p=128), writes=[B_wo], owner=B_wo)
                wr = sb_("wr", [128, 8, 36], F32, phc)
                B_wr = Buf("wr", multi=True)
                T.dma("sp", wr[:, :, 0:4], w_group.rearrange("(c p) n -> p c n", p=128), writes=[B_wr], owner=B_wr)
                T.dma("sp", wr[:, :, 4:36], w_router.rearrange("(c p) n -> p c n", p=128), writes=[B_wr], owner=B_wr)
                br = sb_("br", [128, 36], F32, phc)
                T.dma("sp", br[:, 0:4], b_group.partition_broadcast(128), writes=[B_wr], owner=B_wr)
                T.dma("sp", br[:, 4:36], b_router.partition_broadcast(128), writes=[B_wr], owner=B_wr)
                gfr = sb_("gfr", [128, D], F32, phc)
                T.dma("sp", gfr[:], ffn_norm_g.partition_broadcast(128), writes=[B_wr], owner=B_wr)
                cm = sb_("cm", [128, 1 + NCH], F32, phc)
                T.dma("sp", cm[:], cmoe, writes=[B_wr], owner=B_wr)
                h2tok = sb_("h2tok", [128, NT, D], BF16, phc)
                B_h2 = Buf("h2tok", multi=True)
                oh_all = sb_("oh_all", [128, 2, NT, NE], F32, phc)
                p12 = sb_("p12", [128, 2, NT], F32, phc)
                B_plan = Buf("plan", multi=True)
                carry = sb_("carry", [128, NE], F32, phc)
                B_carry = Buf("carry")
                T.op("dve", lambda e: e.memset(carry[:], 0.0), writes=[B_carry])
                xt_r = Ring([sb_(f"cx{i}", [128, D], F32, phc)[:] for i in range(2)], "cx")
                x1_r = Ring([sb_(f"x1{i}", [128, D], F32, phc)[:] for i in range(2)], "x1")
                junk = sb_("junkc", [128, D], BF16, phc)
                B_junk = Buf("junkc")
                hf_r = Ring([sb_(f"hf{i}", [128, 8, 128], F32, phc)[:] for i in range(1)], "hf")
                sm = sb_("sm", [128, 2, 256], F32, phc)
                sm_r = Ring([sm[:, i, :] for i in range(2)], "sm")
                abf_r = Ring([sb_(f"abf{i}", [128, NE], BF16, phc)[:] for i in range(2)], "abf")
                pc = Ring([0, 1], "pc")
                pc.items = [((0, 1), PB[0]), ((2, 3), PB[2])]
                ptc = Ring([0], "ptc")
                ptc.items = [((4, 5), PB[4])]
                prt = (6, PB[6])
                prk = (7, PB[7])
                ctxs = {}

                def partA(t):
                    r0 = t * 128
                    (b0, b1), B_b = pc.next()
                    for half, bi in enumerate((b0, b1)):
                        T.mm([(lambda e, hh=hh: e.matmul(bank(bi), lhsT=mixT[:, hh, r0:r0 + 128], rhs=wo[:, hh, half * 512:(half + 1) * 512],
                                                         start=(hh == 0), stop=(hh == 7))) for hh in range(8)],
                             reads=[B_mix, B_wo], writes=[B_b])
                    xt, B_xt = xt_r.next()
                    T.dma("sp", xt, x[r0:r0 + 128, :], writes=[B_xt], owner=B_xt)
                    x1, B_x1 = x1_r.next()
                    T.op("dve", lambda e: e.tensor_tensor(out=x1.rearrange("p (a n) -> p a n", a=2), in0=ps[:, b0:b0 + 2, :],
                                                           in1=xt.rearrange("p (a n) -> p a n", a=2), op=ALU.add),
                         reads=[B_b, B_xt], writes=[B_x1])
                    T.dma("sp", out[r0:r0 + 128, :], x1, reads=[B_x1], writes=[B_out], owner=B_x1)
                    s_, B_s = sm_r.next()
                    T.op("act", lambda e: e.activation(out=junk[:], in_=x1, func=AF.Square, accum_out=s_[:, 0:1]),
                         reads=[B_x1], writes=[B_junk, B_s])
                    T.op("act", lambda e: e.activation(out=s_[:, 1:2], in_=s_[:, 0:1], func=AF.Sqrt, scale=1.0 / D, bias=col(c_eps)),
                         reads=[B_s, B_cols], writes=[B_s])
                    T.op("dve", lambda e: e.reciprocal(out=s_[:, 2:3], in_=s_[:, 1:2]), reads=[B_s], writes=[B_s])
                    T.op("dve", lambda e: e.tensor_scalar(out=xt, in0=x1, scalar1=s_[:, 2:3], scalar2=None, op0=ALU.mult),
                         reads=[B_x1, B_s], writes=[B_xt])
                    T.op("pool", lambda e: e.tensor_tensor(out=h2tok[:, t, :], in0=xt, in1=gfr[:], op=ALU.mult), reads=[B_xt, B_wr], writes=[B_h2])
                    (tb, _), B_tb = ptc.next()
                    pTf = ps[:, tb:tb + 2, :].rearrange("p a (c t) -> p (a c) t", t=128)
                    T.mm([(lambda e, c=c: e.transpose(pTf[:, c, :], xt[:, c * 128:(c + 1) * 128], identf[:])) for c in range(8)],
                         reads=[B_xt, B_cf], writes=[B_tb])
                    hf, B_hf = hf_r.next()
                    T.op("dve", lambda e: e.tensor_tensor(out=hf, in0=pTf, in1=cols[:, c_gF:c_gF + 8].unsqueeze(2).broadcast_to([128, 8, 128]),
                                                           op=ALU.mult), reads=[B_tb, B_cols], writes=[B_hf])
                    T.mm([(lambda e, c=c: e.matmul(bank(prt[0], 128, 0, 36), lhsT=hf[:, c, :], rhs=wr[:, c, :], start=(c == 0), stop=(c == 7)))
                          for c in range(8)], reads=[B_hf, B_wr], writes=[prt[1]])
                    def dv(fn):
                        T.op("dve", fn, reads=[B_s], writes=[B_s])
                    lg = s_[:, 4:40]
                    T.op("dve", lambda e: e.tensor_tensor(out=lg, in0=bank(prt[0], 128, 0, 36), in1=br[:], op=ALU.add),
                         reads=[prt[1], B_wr, B_s], writes=[B_s])
                    ctxs[t] = (s_, B_s)

                def partB(t):
                    s_, B_s = ctxs.pop(t)

                    def dv(fn):
                        T.op("dve", fn, reads=[B_s], writes=[B_s])
                    gmax = s_[:, 40:41]
                    dv(lambda e: e.tensor_reduce(out=gmax, in_=s_[:, 4:8], axis=AX.X, op=ALU.max))
                    ohg = s_[:, 44:48]
                    dv(lambda e: e.tensor_scalar(out=ohg, in0=s_[:, 4:8], scalar1=gmax, scalar2=None, op0=ALU.is_ge))
                    dv(lambda e: e.tensor_scalar(out=s_[:, 48:52], in0=s_[:, 4:8], scalar1=gmax, scalar2=None, op0=ALU.subtract))
                    T.op("act", lambda e: e.activation(out=s_[:, 48:52], in_=s_[:, 48:52], func=AF.Exp, accum_out=s_[:, 41:42]),
                         reads=[B_s], writes=[B_s])
                    dv(lambda e: e.reciprocal(out=s_[:, 42:43], in_=s_[:, 41:42]))
                    dv(lambda e: e.tensor_scalar(out=s_[:, 52:56], in0=ohg, scalar1=-1.0, scalar2=1.0e4, op0=ALU.add, op1=ALU.mult))
                    me = s_[:, 64:96]
                    dv(lambda e: e.tensor_tensor(out=me.rearrange("p (g k) -> p g k", k=8), in0=s_[:, 8:40].rearrange("p (g k) -> p g k", k=8),
                                                 in1=s_[:, 52:56].unsqueeze(2).broadcast_to([128, 4, 8]), op=ALU.add))
                    m1 = s_[:, 56:57]
                    dv(lambda e: e.tensor_reduce(out=m1, in_=me, axis=AX.X, op=ALU.max))
                    oh1 = oh_all[:, 0, t, :]
                    oh2 = oh_all[:, 1, t, :]
                    T.op("dve", lambda e: e.tensor_scalar(out=oh1, in0=me, scalar1=m1, scalar2=None, op0=ALU.is_ge), reads=[B_s], writes=[B_plan])
                    me2 = s_[:, 128:160]
                    T.op("dve", lambda e: e.scalar_tensor_tensor(out=me2, in0=oh1, scalar=-1.0e4, in1=me, op0=ALU.mult, op1=ALU.add),
                         reads=[B_s, B_plan], writes=[B_s])
                    m2 = s_[:, 57:58]
                    dv(lambda e: e.tensor_reduce(out=m2, in_=me2, axis=AX.X, op=ALU.max))
                    T.op("dve", lambda e: e.tensor_scalar(out=oh2, in0=me2, scalar1=m2, scalar2=None, op0=ALU.is_ge), reads=[B_s], writes=[B_plan])
                    dv(lambda e: e.tensor_tensor(out=s_[:, 58:59], in0=m1, in1=m2, op=ALU.subtract))
                    T.op("act", lambda e: e.activation(out=s_[:, 59:60], in_=s_[:, 58:59], func=AF.Sigmoid), reads=[B_s], writes=[B_s])
                    T.op("dve", lambda e: e.tensor_tensor(out=wt12[:, t, 0:1], in0=s_[:, 59:60], in1=s_[:, 42:43], op=ALU.mult), reads=[B_s], writes=[B_wt])
                    T.op("dve", lambda e: e.tensor_tensor(out=wt12[:, t, 1:2], in0=s_[:, 42:43], in1=wt12[:, t, 0:1], op=ALU.subtract),
                         reads=[B_s, B_wt], writes=[B_wt])
                    abf, B_abf = abf_r.next()
                    T.op("dve", lambda e: e.tensor_tensor(out=abf, in0=oh1, in1=oh2, op=ALU.add), reads=[B_plan], writes=[B_abf])
                    T.mm([lambda e: e.matmul(bank(prk[0], 128, 0, 32), lhsT=USTRICT, rhs=abf, start=True, stop=True)], reads=[B_abf, B_cb], writes=[prk[1]])
                    T.mm([lambda e: e.matmul(bank(prk[0], 128, 32, 64), lhsT=ONES, rhs=abf, start=True, stop=True)], reads=[B_abf, B_cb], writes=[prk[1]])
                    rk = s_[:, 160:192]
                    T.op("dve", lambda e: e.tensor_tensor(out=rk, in0=bank(prk[0], 128, 0, 32), in1=carry[:], op=ALU.add),
                         reads=[prk[1], B_carry, B_s], writes=[B_s])
                    T.op("dve", lambda e: e.tensor_tensor(out=carry[:], in0=bank(prk[0], 128, 32, 64), in1=carry[:], op=ALU.add),
                         reads=[prk[1], B_carry, B_s], writes=[B_carry])
                    for k_ in range(2):
                        T.op("dve", lambda e: e.tensor_tensor(out=s_[:, 192:224], in0=oh_all[:, k_, t, :], in1=rk, op=ALU.mult),
                             reads=[B_s, B_plan], writes=[B_s])
                        T.op("dve", lambda e: e.tensor_reduce(out=p12[:, k_, t:t + 1], in_=s_[:, 192:224], axis=AX.X, op=ALU.add),
                             reads=[B_s], writes=[B_plan])

                partA(0)
                for t in range(NT):
                    if t + 1 < NT:
                        partA(t + 1)
                    partB(t)
                B_plan.multi = False
                B_wt.multi = False
                g1 = sb_("gplan", [128, 8, NE], F32, phc)
                B_g1 = Buf("g1")
                def gp(fn, extra=()):
                    T.op("dve", fn, reads=[B_g1, B_carry, B_plan, B_wr] + list(extra), writes=[B_g1])
                gp(lambda e: e.tensor_scalar(out=g1[:, 0, :], in0=carry[:], scalar1=63.5, scalar2=1.0 / 128, op0=ALU.add, op1=ALU.mult))
                gp(lambda e: e.tensor_scalar(out=g1[:, 0, :], in0=g1[:, 0, :], scalar1=MAGIC, scalar2=None, op0=ALU.add))
                gp(lambda e: e.tensor_scalar(out=g1[:, 0, :], in0=g1[:, 0, :], scalar1=-MAGIC, scalar2=128.0, op0=ALU.add, op1=ALU.mult))
                gp(lambda e: e.tensor_copy(out=g1[:, 1, :], in_=g1[:, 0, :]))
                src_, dst_ = 1, 2
                for k_ in (1, 2, 4, 8, 16):
                    gp(lambda e: e.tensor_copy(out=g1[:, dst_, 0:k_], in_=g1[:, src_, 0:k_]))
                    gp(lambda e: e.tensor_tensor(out=g1[:, dst_, k_:NE], in0=g1[:, src_, k_:NE], in1=g1[:, src_, 0:NE - k_], op=ALU.add))
                    src_, dst_ = dst_, src_
                pends = g1[:, src_, :]
                pstart = g1[:, 3, :]
                gp(lambda e: e.tensor_tensor(out=pstart, in0=pends, in1=g1[:, 0, :], op=ALU.subtract))
                big = sb_("bigt", [128, max(NCH, NT) * NE], F32, phc)
                destf = sb_("destf", [128, 2, NT], F32, phc)
                for k_ in range(2):
                    gp(lambda e: e.tensor_tensor(out=big[:, 0:NT * NE].rearrange("p (t n) -> p t n", n=NE), in0=oh_all[:, k_, :, :],
                                                 in1=pstart.unsqueeze(1).broadcast_to([128, NT, NE]), op=ALU.mult))
                    gp(lambda e: e.tensor_reduce(out=destf[:, k_, :], in_=big[:, 0:NT * NE].rearrange("p (t n) -> p t n", n=NE), axis=AX.X, op=ALU.add))
                    gp(lambda e: e.tensor_tensor(out=destf[:, k_, :], in0=destf[:, k_, :], in1=p12[:, k_, :], op=ALU.add))
                    T.op("dve", lambda e: e.tensor_copy(out=dest[:, k_, :], in_=destf[:, k_, :]), reads=[B_g1], writes=[B_dest])
                gp(lambda e: e.tensor_tensor(out=big[:, 0:NCH * NE].rearrange("p (c n) -> p c n", n=NE),
                                             in0=pends.unsqueeze(1).broadcast_to([128, NCH, NE]),
                                             in1=cm[:, 1:1 + NCH].unsqueeze(2).broadcast_to([128, NCH, NE]), op=ALU.is_le))
                cef = sb_("cef", [128, 4, NCH], F32, phc)
                gp(lambda e: e.tensor_reduce(out=cef[:, 0, :], in_=big[:, 0:NCH * NE].rearrange("p (c n) -> p c n", n=NE), axis=AX.X, op=ALU.add))
                gp(lambda e: e.tensor_scalar(out=cef[:, 0, :], in0=cef[:, 0, :], scalar1=float(NE - 1), scalar2=None, op0=ALU.min))
                gp(lambda e: e.memset(cef[:, 1, :], 0.0))
                gp(lambda e: e.tensor_tensor(out=cef[:, 1, 1:NCH], in0=cef[:, 0, 1:NCH], in1=cef[:, 0, 0:NCH - 1], op=ALU.is_equal))
                gp(lambda e: e.tensor_scalar(out=cef[:, 2, :], in0=cef[:, 0, :], scalar1=256.0, scalar2=None, op0=ALU.mult))
                gp(lambda e: e.scalar_tensor_tensor(out=cef[:, 2, :], in0=cef[:, 1, :], scalar=16384.0, in1=cef[:, 2, :], op0=ALU.mult, op1=ALU.add))
                for hh in range(2):
                    T.op("dve", lambda e: e.tensor_scalar(out=idx2[:, hh, :], in0=cef[:, 2, :], scalar1=cm[:, 0:1], scalar2=float(hh), op0=ALU.add, op1=ALU.add),
                         reads=[B_g1, B_wr], writes=[B_dest])
                dreg = nc.gpsimd.to_reg(NCH * 128 - 1)
                B_h2.multi = False
                for t in range(NT):
                    for k_ in range(2):
                        T.idma(out=xs, out_offset=bass.IndirectOffsetOnAxis(ap=dest[:, k_, t:t + 1], axis=0), in_=h2tok[:, t, :], in_offset=None,
                               reads=[B_h2, B_dest], writes=[B_xs], owner=B_h2, bounds_check=dreg, oob_is_err=False)
                T.barrier()

        with contextlib.ExitStack() as phd:
            w1v = w1.rearrange("e (p h j) n -> (e p h) (j n)", p=128, h=2, j=4)
            w3v = w3.rearrange("e (p h j) n -> (e p h) (j n)", p=128, h=2, j=4)
            w2v = w2.rearrange("e (p h j) n -> (e p h) (j n)", p=128, h=2, j=2)
            wbt = sb_("wbt", [128, 3, 4096], BF16, phd)
            BW = [Buf(f"W{i}") for i in range(3)]
            T.op("dve", lambda e: e.memset(wbt[:], 0.0), writes=BW)
            for b_ in BW:
                b_.par = True
            xc_r = Ring([sb_(f"xc{i}", [128, D], BF16, phd)[:] for i in range(4)], "xc")
            xT_r = Ring([sb_(f"xT{i}", [128, 8, 128], BF16, phd)[:] for i in range(2)], "xT")
            sl_r = Ring([sb_(f"sl{i}", [128, 512], F32, phd)[:] for i in range(2)], "sl")
            G_r = Ring([sb_(f"G{i}", [128, 512], BF16, phd)[:] for i in range(2)], "G")
            GT_r = Ring([sb_(f"GT{i}", [128, 4, 128], BF16, phd)[:] for i in range(2)], "GT")
            yo_r = Ring([sb_(f"yo{i}", [128, D], F32, phd)[:] for i in range(2)], "yo")
            PXT = ((0, 1), PB[0])
            PH1 = (2, PB[2])
            PH3 = (3, PB[3])
            PGT = (4, PB[4])
            PY = ((5, 6), PB[5])
            st1 = {}

            xcs = {}

            def xload(cn):
                xc_, B_xc_ = xc_r.next()
                T.dma("sp", xc_, xs[cn * 128:(cn + 1) * 128, :], reads=[B_xs], writes=[B_xc_], owner=B_xc_)
                xcs[cn] = (xc_, B_xc_)

            xload(0)
            xload(1)

            def stage1(c):
                if c + 2 < NCH:
                    xload(c + 2)
                xc, B_xc = xcs.pop(c)
                wb = wbt
                (x0, x1b), B_px = PXT
                pxt = ps[:, x0:x0 + 2, :].rearrange("p a (c t) -> p (a c) t", t=128)
                xcv = xc.rearrange("t (p j) -> t j p", j=8)
                T.mm([(lambda e, jj=jj: e.matmul(pxt[:, jj, :], lhsT=xcv[:, jj, :], rhs=IDENT, start=True, stop=True)) for jj in range(8)],
                     reads=[B_xc, B_cb], writes=[B_px])
                xT, B_xT = xT_r.next()
                T.op("act", lambda e: e.activation(out=xT, in_=pxt, func=AF.Copy), reads=[B_px], writes=[B_xT])
                w1b = wb[:, 0, :].rearrange("p (j n) -> p j n", n=512)
                w3b = wb[:, 1, :].rearrange("p (j n) -> p j n", n=512)
                T.mm([(lambda e, jj=jj: e.matmul(bank(PH1[0]), lhsT=xT[:, jj, :], rhs=w1b[:, jj, :], start=(jj == 0), stop=(jj == 7))) for jj in range(8)],
                     reads=[B_xT, BW[0]], writes=[PH1[1]])
                T.mm([(lambda e, jj=jj: e.matmul(bank(PH3[0]), lhsT=xT[:, jj, :], rhs=w3b[:, jj, :], start=(jj == 0), stop=(jj == 7))) for jj in range(8)],
                     reads=[B_xT, BW[1]], writes=[PH3[1]])
                sl, B_sl = sl_r.next()
                T.op("act", lambda e: e.activation(out=sl, in_=bank(PH1[0]), func=AF.Silu), reads=[PH1[1]], writes=[B_sl])
                G, B_G = G_r.next()
                T.op("dve", lambda e: e.tensor_tensor(out=G, in0=bank(PH3[0]), in1=sl, op=ALU.mult), reads=[PH3[1], B_sl], writes=[B_G])
                st1[c] = (G, B_G, wb, None)

            breg = nc.gpsimd.to_reg(NE * 256 - 1)

            def gathers(c, which):
                for mi, wv_ in enumerate((w1v, w3v, w2v)):
                    if (mi == 2) != (which == 2):
                        continue
                    for hh in range(2):
                        T.idma(out=wbt[:, mi, hh * 2048:(hh + 1) * 2048], out_offset=None, in_=wv_,
                               in_offset=bass.IndirectOffsetOnAxis(ap=idx2[:, hh, c:c + 1], axis=0),
                               reads=[B_dest], writes=[BW[mi]], owner=BW[mi], bounds_check=breg, oob_is_err=False)

            def stage2(c):
                G, B_G, wb, B_w = st1.pop(c)
                Gv = G.rearrange("t (p j) -> t j p", j=4)
                pgt = ps[:, PGT[0], :].rearrange("p (c t) -> p c t", t=128)
                T.mm([(lambda e, jj=jj: e.matmul(pgt[:, jj, :], lhsT=Gv[:, jj, :], rhs=IDENT, start=True, stop=True)) for jj in range(4)],
                     reads=[B_G, B_cb], writes=[PGT[1]])
                GT, B_GT = GT_r.next()
                T.op("act", lambda e: e.activation(out=GT, in_=pgt, func=AF.Copy), reads=[PGT[1]], writes=[B_GT])
                w2b = wb[:, 2, :].rearrange("p (j n) -> p j n", n=1024)
                (y0, y1b), B_py = PY
                for half, bi in enumerate((y0, y1b)):
                    T.mm([(lambda e, jj=jj: e.matmul(bank(bi), lhsT=GT[:, jj, :], rhs=w2b[:, jj, half * 512:(half + 1) * 512], start=(jj == 0), stop=(jj == 3)))
                          for jj in range(4)], reads=[B_GT, BW[2]], writes=[B_py])
                yo, B_yo = yo_r.next()
                T.op("dve", lambda e: e.tensor_copy(out=yo.rearrange("p (a n) -> p a n", a=2), in_=ps[:, y0:y0 + 2, :]), reads=[B_py], writes=[B_yo])
                T.dma("sp", ys[c * 128:(c + 1) * 128, :], yo, reads=[B_yo], writes=[B_ys], owner=B_yo)

            gathers(0, 1)
            gathers(0, 2)
            for c in range(NCH + 1):
                if c < NCH:
                    stage1(c)
                if c + 1 < NCH:
                    gathers(c + 1, 1)
                if c >= 1:
                    stage2(c - 1)
                    if c < NCH:
                        gathers(c, 2)
            T.barrier()
            dreg2 = nc.gpsimd.to_reg(NCH * 128 - 1)
            y_r = Ring([sb_(f"yg{i}", [128, 2, D], F32, phd)[:] for i in range(2)], "yg")
            xo_r = Ring([sb_(f"xo{i}", [128, D], F32, phd)[:] for i in range(3)], "xo")
            exo = {}

            def eload(tn):
                xo_, B_xo_ = xo_r.next()
                T.dma("sp", xo_, out[tn * 128:(tn + 1) * 128, :], reads=[B_out], writes=[B_xo_], owner=B_xo_)
                exo[tn] = (xo_, B_xo_)

            eload(0)
            for t in range(NT):
                r0 = t * 128
                yg, B_yg = y_r.next()
                B_yg.par = True
                for k_ in range(2):
                    T.idma(out=yg[:, k_, :], out_offset=None, in_=ys, in_offset=bass.IndirectOffsetOnAxis(ap=dest[:, k_, t:t + 1], axis=0),
                           reads=[B_ys, B_dest], writes=[B_yg], owner=B_yg, bounds_check=dreg2, oob_is_err=False)
                if t + 1 < NT:
                    eload(t + 1)
                xo, B_xo = exo.pop(t)
                for k_ in range(2):
                    T.op("dve", lambda e: e.scalar_tensor_tensor(out=xo, in0=yg[:, k_, :], scalar=wt12[:, t, k_:k_ + 1], in1=xo, op0=ALU.mult, op1=ALU.add),
                         reads=[B_yg, B_xo, B_wt], writes=[B_xo])
                T.dma("sp", out[r0:r0 + 128, :], xo, reads=[B_xo], writes=[B_out], owner=B_xo)
            T.barrier()
    return nc


def _consts():
    cst = np.zeros((128, 7, 128), np.float32)
    cst[:, 0, :] = np.eye(128, dtype=np.float32)
    k = np.arange(128)[:, None]
    q = np.arange(128)[None, :]
    cst[:, 1, :] = np.where(k > q, NEG, 0.0)
    cst[:, 2, :] = ((k // 64) == (q // 64)).astype(np.float32)
    for m in range(2):
        for d in range(8):
            cst[m * 64 + d + 8, 3, m * 64 + d] = -1.0
            cst[m * 64 + d, 3, m * 64 + d + 8] = 1.0
    for d in range(32):
        cst[d + 32, 4, d] = -1.0
        cst[d, 4, d + 32] = 1.0
    cst[:, 5, :] = 1.0
    cst[:, 6, :] = (k < q).astype(np.float32)
    cinv = np.zeros((128, 2), np.float32)
    for p in range(128):
        d = p % 64
        if d < 16:
            cinv[p, 0] = THETA ** (-(2.0 * (d % 8)) / 16.0) / (2.0 * math.pi)
        if p < 64:
            cinv[p, 1] = THETA ** (-(2.0 * (p % 32)) / 64.0) / (2.0 * math.pi)
    return cst, cinv


_NC_CACHE = {}


def kernel(**inputs):
    x = np.asarray(inputs["x"])
    Bn, S, _ = x.shape
    if S not in _NC_CACHE:
        _NC_CACHE[S] = build(S)
    nc = _NC_CACHE[S]
    cst, cinv = _consts()
    shared = {}
    for k_, v in inputs.items():
        if k_ in ("x", "positions"):
            continue
        a = np.ascontiguousarray(np.asarray(v))
        shared[k_] = a.reshape(a.shape[1:]) if a.shape[0] == 1 else a
    shared["b_gate"] = shared["b_gate"].reshape(-1)
    shared["cst"] = cst
    shared["cinv"] = cinv
    nch = 2 * (S // 128) + NE
    cmoe = np.zeros((128, 1 + nch), np.float32)
    cmoe[:, 0] = 2.0 * np.arange(128)
    cmoe[:, 1:] = 128.0 * np.arange(nch)[None, :]
    shared["cmoe"] = cmoe
    positions = np.asarray(inputs["positions"]).astype(np.int32)
    in_maps = []
    for b in range(Bn):
        m = dict(shared)
        m["x"] = np.ascontiguousarray(x[b])
        m["pos"] = np.ascontiguousarray(positions[b])
        in_maps.append(m)
    res = run_bass_kernel_spmd(nc, in_maps, core_ids=list(range(Bn)))
    return np.stack([np.asarray(r["out"]) for r in res.results], axis=0).astype(np.float32)
```

```python
import math
import contextlib
import numpy as np
import concourse.bass as bass
import concourse.mybir as mybir
from concourse.bass_utils import run_bass_kernel_spmd

F32 = mybir.dt.float32
BF16 = mybir.dt.bfloat16
I32 = mybir.dt.int32
AF = mybir.ActivationFunctionType
ALU = mybir.AluOpType
AX = mybir.AxisListType

D = 1024
NCORES = 8
IN_COLS = 5824
C_CQ, C_CKV, C_KR, C_GATE = 3072, 3456, 3712, 3776
EPS = 1e-6
THETA = 500000.0
LAMBDA_INIT = 0.8 - 0.6 * math.exp(0.0)
NE = 32
DE = 512
MAGIC = 12582912.0
NEG = -30000.0


class Buf:
    def __init__(self, name, multi=False):
        self.name = name
        self.w = {}
        self.rs = {}
        self.multi = multi
        self.dsem = None
        self.dcnt = 0


class Tracker:
    def __init__(self, nc, stack):
        self.nc = nc
        self.stack = stack
        self.engs = {"pe": nc.tensor, "act": nc.scalar, "dve": nc.vector, "pool": nc.gpsimd, "sp": nc.sync}
        self.sem = {}
        self.cnt = {}
        self.nsem = 0
        self.waited = {}
        self.last = {}
        self.dbufs = []
        for k in self.engs:
            self._newsem(k)

    def _mksem(self, name):
        self.nsem += 1
        return self.stack.enter_context(self.nc.semaphore(f"{name}_{self.nsem}"))

    def _newsem(self, k):
        self.sem[k] = (self._mksem("e" + k), self.nsem)
        self.cnt[k] = 0

    def _wait(self, eng, ev):
        sem, sid, val, src, buf = ev
        if src == "dma":
            val = 16 * buf.dcnt
        elif src == eng and eng == "pe":
            return
        key = (eng, sid)
        if self.waited.get(key, 0) >= val:
            return
        self.waited[key] = val
        self.engs[eng].wait_ge(sem, val)

    def deps(self, eng, reads, writes):
        for b in reads:
            for ev in b.w.values():
                self._wait(eng, ev)
        for b in writes:
            if b.multi:
                continue
            for ev in b.w.values():
                if getattr(b, "par", False) and ev[3] == "dma":
                    continue
                self._wait(eng, ev)
            for ev in b.rs.values():
                self._wait(eng, ev)

    def _record(self, ev, key, reads, writes):
        for b in reads:
            b.rs[key] = ev
        for b in writes:
            if b.multi or getattr(b, "par", False):
                b.w[key] = ev
            else:
                b.w = {key: ev}
                b.rs = {}

    def op(self, eng, fn, reads=(), writes=()):
        self.deps(eng, reads, writes)
        if self.cnt[eng] >= 30000:
            self._newsem(eng)
        self.cnt[eng] += 1
        sem, sid = self.sem[eng]
        ev = (sem, sid, self.cnt[eng], eng, None)
        fn(self.engs[eng]).then_inc(sem, 1)
        self.last[eng] = ev
        self._record(ev, eng, reads, writes)
        return ev

    def mm(self, fns, reads=(), writes=()):
        self.deps("pe", reads, writes)
        for f in fns[:-1]:
            f(self.nc.tensor)
        return self.op("pe", fns[-1], reads, writes)

    def dma(self, q, out, in_, reads=(), writes=(), owner=None, **kw):
        self.deps(q, reads, writes)
        b = owner
        if b.dsem is None:
            b.dsem = (self._mksem("d"), self.nsem)
            self.dbufs.append(b)
        b.dcnt += 1
        sem, sid = b.dsem
        ev = (sem, sid, 16 * b.dcnt, "dma", b)
        self.engs[q].dma_start(out=out, in_=in_, **kw).then_inc(sem, 16)
        self._record(ev, ("dma", sid), reads, writes)
        return ev

    def idma(self, out, out_offset, in_, in_offset, reads=(), writes=(), owner=None, **kw):
        q = "pool"
        self.deps(q, reads, writes)
        b = owner
        if b.dsem is None:
            b.dsem = (self._mksem("d"), self.nsem)
            self.dbufs.append(b)
        b.dcnt += 1
        sem, sid = b.dsem
        ev = (sem, sid, 16 * b.dcnt, "dma", b)
        self.nc.gpsimd.indirect_dma_start(out=out, out_offset=out_offset, in_=in_, in_offset=in_offset, **kw).then_inc(sem, 16)
        self._record(ev, ("dma", sid), reads, writes)
        return ev

    def barrier(self):
        evs = list(self.last.values())
        for e in self.engs:
            for ev in evs:
                self._wait(e, ev)
            for b in self.dbufs:
                self._wait(e, (b.dsem[0], b.dsem[1], 0, "dma", b))


class Ring:
    def __init__(self, aps, name):
        self.items = [(ap, Buf(f"{name}{i}")) for i, ap in enumerate(aps)]
        self.i = 0

    def next(self):
        it = self.items[self.i % len(self.items)]
        self.i += 1
        return it


def build(S):
    NT = S // 128
    NSB = S // 512
    nc = bass.Bass("TRN2", target_bir_lowering=False)

    def din(name, shape, dt=F32):
        return nc.dram_tensor(name, shape, dt, kind="ExternalInput").ap()

    def dscr(name, shape, dt=BF16):
        return nc.dram_tensor(name, shape, dt, kind="Internal").ap()

    x = din("x", [S, D])
    pos = din("pos", [S], I32)
    attn_norm_g = din("attn_norm_g", [D])
    w_in = din("w_in", [D, IN_COLS])
    b_gate = din("b_gate", [2 * D])
    da_q_norm_g = din("da_q_norm_g", [64])
    da_k_norm_g = din("da_k_norm_g", [64])
    lq1 = din("da_lambda_q1", [64])
    lk1 = din("da_lambda_k1", [64])
    lq2 = din("da_lambda_q2", [64])
    lk2 = din("da_lambda_k2", [64])
    subln_g = din("da_subln_g", [128])
    q_lora_g = din("mla_q_lora_g", [384])
    w_uq = din("mla_w_uq", [384, 1536])
    kv_lora_g = din("mla_kv_lora_g", [256])
    w_ukv = din("mla_w_ukv", [256, 2048])
    q_norm_g = din("mla_q_norm_g", [192])
    kn_norm_g = din("mla_k_nope_norm_g", [128])
    kr_norm_g = din("mla_k_rope_norm_g", [64])
    w_o = din("w_o", [D, D])
    ffn_norm_g = din("ffn_norm_g", [D])
    w_group = din("w_group", [D, 4])
    b_group = din("b_group", [4])
    w_router = din("w_router", [D, 32])
    b_router = din("b_router", [32])
    w1 = din("w1", [NE, D, DE])
    w3 = din("w3", [NE, D, DE])
    w2 = din("w2", [NE, DE, D])
    cst = din("cst", [128, 7, 128])
    cinv = din("cinv", [128, 2])
    out = nc.dram_tensor("out", [S, D], F32, kind="ExternalOutput").ap()

    s_qda = dscr("s_qda", [8, 128, S])
    s_kda = dscr("s_kda", [8, 128, S])
    s_vda = dscr("s_vda", [S, D])
    s_qn = dscr("s_qn", [8, 128, S])
    s_qr = dscr("s_qr", [8, 64, S])
    s_kn = dscr("s_kn", [8, 128, S])
    s_kr = dscr("s_kr", [64, S])
    s_vm = dscr("s_vm", [S, D])
    s_gate = dscr("s_gate", [16, 128, S])
    NCH = 2 * NT + NE
    cmoe = din("cmoe", [128, 1 + NCH])
    xs = dscr("s_xs", [NCH * 128, D])
    ys = dscr("s_ys", [NCH * 128, D], F32)
    B_xs = Buf("xs", multi=True)
    B_ys = Buf("ys", multi=True)
    B_scr = Buf("scratch", multi=True)
    B_out = Buf("out", multi=True)

    with contextlib.ExitStack() as stack:
        T = Tracker(nc, stack)

        def sb_(name, shape, dt, st=None):
            return (st or stack).enter_context(nc.sbuf_tensor(name, shape, dt))

        ps = stack.enter_context(nc.psum_tensor("ps", [128, 8, 512], F32))
        PB = [Buf(f"psb{i}") for i in range(8)]

        cb = sb_("cb", [128, 7, 128], BF16)
        B_cb = Buf("cb")
        T.dma("pool", cb[:], cst, writes=[B_cb], owner=B_cb)
        identf = sb_("identf", [128, 128], F32)
        B_cf = Buf("cf")
        T.dma("sp", identf[:], cst[:, 0, :], writes=[B_cf], owner=B_cf)
        IDENT, NEGM, B64, RDA, RMLA, ONES, USTRICT = (cb[:, i, :] for i in range(7))
        wt12 = sb_("wt12", [128, NT, 2], F32)
        B_wt = Buf("wt12", multi=True)
        dest = sb_("dest", [128, 2, NT], I32)
        idx2 = sb_("idx2", [128, 2, NCH], I32)
        B_dest = Buf("dest", multi=True)
        cols = sb_("cols", [128, 64], F32)
        B_cols = Buf("cols")
        ev0 = T.op("dve", lambda e: e.memset(cols[:], 0.0), writes=[B_cols])
        T._wait("sp", ev0)
        ci = [0]

        def col_load(src, n, p0=0):
            c = ci[0]
            ci[0] += 1
            T.dma("sp", cols[p0:p0 + n, c:c + 1], src.rearrange("(p o) -> p o", o=1), writes=[B_cols], owner=B_cols)
            return c

        B_cols.multi = True
        c_inv = ci[0]
        ci[0] += 2
        T.dma("sp", cols[:, c_inv:c_inv + 2], cinv, writes=[B_cols], owner=B_cols)
        c_gA = ci[0]
        for c in range(8):
            col_load(attn_norm_g[c * 128:(c + 1) * 128], 128)
        c_gF = ci[0]
        for c in range(8):
            col_load(ffn_norm_g[c * 128:(c + 1) * 128], 128)
        c_bg = ci[0]
        for c in range(16):
            col_load(b_gate[c * 128:(c + 1) * 128], 128)
        c_gq = col_load(da_q_norm_g, 64)
        ci[0] -= 1
        col_load(da_q_norm_g, 64, 64)
        c_gk = col_load(da_k_norm_g, 64)
        ci[0] -= 1
        col_load(da_k_norm_g, 64, 64)
        c_sub = col_load(subln_g, 128)
        c_gql = ci[0]
        for c in range(3):
            col_load(q_lora_g[c * 128:(c + 1) * 128], 128)
        c_gkvl = ci[0]
        for c in range(2):
            col_load(kv_lora_g[c * 128:(c + 1) * 128], 128)
        c_gqn = col_load(q_norm_g[0:128], 128)
        c_gqr = col_load(q_norm_g[128:192], 64)
        c_gkn = col_load(kn_norm_g, 128)
        c_gkr = col_load(kr_norm_g, 64)
        c_eps = ci[0]
        ci[0] += 1
        c_lam = ci[0]
        ci[0] += 4
        B_cols.multi = False
        T.op("dve", lambda e: e.memset(cols[:, c_eps:c_eps + 1], EPS), reads=[B_cols], writes=[B_cols])

        def col(c, rows=128, p0=0):
            return cols[p0:p0 + rows, c:c + 1]

        lam_t = sb_("lam_t", [128, 4, 64], F32)
        B_lam = Buf("lam", multi=True)
        for i, src in enumerate((lq1, lk1, lq2, lk2)):
            T.dma("sp", lam_t[:, i, :], src.partition_broadcast(128), writes=[B_lam], owner=B_lam)
        B_lam.multi = False
        T.op("dve", lambda e: e.tensor_tensor(out=lam_t[:, 0, :], in0=lam_t[:, 0, :], in1=lam_t[:, 1, :], op=ALU.mult),
             reads=[B_lam], writes=[B_lam])
        T.op("dve", lambda e: e.tensor_tensor(out=lam_t[:, 2, :], in0=lam_t[:, 2, :], in1=lam_t[:, 3, :], op=ALU.mult),
             reads=[B_lam], writes=[B_lam])
        T.op("dve", lambda e: e.tensor_reduce(out=cols[:, c_lam:c_lam + 1], in_=lam_t[:, 0, :], axis=AX.X, op=ALU.add),
             reads=[B_lam], writes=[B_cols])
        T.op("dve", lambda e: e.tensor_reduce(out=cols[:, c_lam + 1:c_lam + 2], in_=lam_t[:, 2, :], axis=AX.X, op=ALU.add),
             reads=[B_lam, B_cols], writes=[B_cols])
        T.op("act", lambda e: e.activation(out=cols[:, c_lam:c_lam + 2], in_=cols[:, c_lam:c_lam + 2], func=AF.Exp),
             reads=[B_cols], writes=[B_cols])
        T.op("dve", lambda e: e.scalar_tensor_tensor(out=cols[:, c_lam + 2:c_lam + 3], in0=cols[:, c_lam + 1:c_lam + 2],
                                                      scalar=-LAMBDA_INIT, in1=cols[:, c_lam:c_lam + 1],
                                                      op0=ALU.add, op1=ALU.subtract), reads=[B_cols], writes=[B_cols])
        c_neglam = c_lam + 2
        T.op("dve", lambda e: e.tensor_scalar(out=cols[:, c_lam + 3:c_lam + 4], in0=cols[:, c_sub:c_sub + 1],
                                               scalar1=1.0 - LAMBDA_INIT, scalar2=None, op0=ALU.mult),
             reads=[B_cols], writes=[B_cols])
        c_subs = c_lam + 3
        rg = sb_("rg", [128, 4, 128], BF16)
        B_rg = Buf("rg")
        for i, (src, c) in enumerate(((RDA, c_gq), (RDA, c_gk), (RMLA, c_gqr), (RMLA, c_gkr))):
            T.op("dve", lambda e, i=i, src=src, c=c: e.tensor_scalar(out=rg[:, i, :], in0=src, scalar1=col(c), scalar2=None,
                                                                     op0=ALU.mult), reads=[B_cb, B_cols], writes=[B_rg])
        RGQ, RGK, RGMQ, RGKR = (rg[:, i, :] for i in range(4))
        CONST = [B_cb, B_cols, B_rg]
        zt = sb_("zt", [128, D], BF16)
        B_zt = Buf("zt")
        T.op("dve", lambda e: e.memset(zt[:], 0.0), writes=[B_zt])
        for c in range(NCH):
            T.dma("sp", xs[c * 128:(c + 1) * 128, :], zt[:], reads=[B_zt], writes=[B_xs], owner=B_zt)

        def bank(i, rows=128, c0=0, c1=512):
            return ps[0:rows, i, c0:c1]

        with contextlib.ExitStack() as ph:
            win = sb_("win", [128, 8, IN_COLS], BF16, ph)
            B_win = Buf("win", multi=True)
            w_in_v = w_in.rearrange("(c p) n -> p c n", p=128)
            for kc in range(8):
                for j in range(0, IN_COLS, 1456):
                    T.dma("pool", win[:, kc, j:j + 1456], w_in_v[:, kc, j:j + 1456], writes=[B_win], owner=B_win)
            wuq = sb_("wuq", [128, 3, 1536], BF16, ph)
            T.dma("pool", wuq[:], w_uq.rearrange("(c p) n -> p c n", p=128), writes=[B_win], owner=B_win)
            wkn = sb_("wkn", [128, 2, 8, 128], BF16, ph)
            wv = sb_("wv", [128, 2, 8, 128], BF16, ph)
            ukv_v = w_ukv.rearrange("(c p) (h t d) -> p c h t d", p=128, t=2, d=128)
            for c in range(2):
                T.dma("pool", wkn[:, c, :, :], ukv_v[:, c, :, 0, :], writes=[B_win], owner=B_win)
                T.dma("pool", wv[:, c, :, :], ukv_v[:, c, :, 1, :], writes=[B_win], owner=B_win)
            hT = sb_("hT", [128, 8, 512], BF16, ph)
            B_hT = Buf("hT")
            xt_r = Ring([sb_(f"xt{i}", [128, D], F32, ph)[:] for i in range(2)], "xt")
            junk = sb_("junk", [128, D], BF16, ph)
            B_junk = Buf("junk")
            xn_r = Ring([sb_(f"xn{i}", [128, D], BF16, ph)[:] for i in range(2)], "xn")
            st1 = sb_("st1", [128, 8], F32, ph)
            st_r = Ring([st1[:, i:i + 1] for i in range(8)], "st")
            posi = sb_("posi", [128, 512], I32, ph)
            B_posi = Buf("posi")
            posf = sb_("posf", [128, 512], F32, ph)
            B_posf = Buf("posf")
            tabs = sb_("tabs", [128, 4, 512], F32, ph)
            B_tab = [Buf(f"tab{i}") for i in range(4)]
            SIND, COSD, SINM, COSM = (tabs[:, i, :] for i in range(4))
            cqn = sb_("cqn", [128, 3, 512], BF16, ph)
            B_cqn = Buf("cqn")
            ckvn = sb_("ckvn", [128, 2, 512], BF16, ph)
            B_ckvn = Buf("ckvn")
            f_r = Ring([sb_(f"f{i}", [128, 512], F32, ph)[:] for i in range(8)], "f")
            h_r = Ring([sb_(f"h{i}", [128, 512], BF16, ph)[:] for i in range(8)], "h")
            o_r = Ring([sb_(f"o{i}", [128, 512], BF16, ph)[:] for i in range(4)], "o")
            ov_r = Ring([sb_(f"ov{i}", [128, D], BF16, ph)[:] for i in range(2)], "ov")
            pa = Ring([0, 1, 2], "pa")
            pa.items = [(i, PB[i]) for i in (0, 1, 2, 6)]
            px = Ring([3, 4, 5], "px")
            px.items = [(i, PB[i]) for i in (3, 4, 5)]
            pt = Ring([6, 7], "pt")
            pt.items = [(i, PB[i]) for i in (7,)]

            def make_rstd(ssq_ap, B_ssq, n, rows):
                sd, B_sd = f_r.next()
                T.op("act", lambda e: e.activation(out=sd[0:rows], in_=ssq_ap, func=AF.Sqrt, scale=1.0 / n,
                                                    bias=col(c_eps, rows)), reads=[B_ssq, B_cols], writes=[B_sd])
                rs, B_rs = f_r.next()
                T.op("dve", lambda e: e.reciprocal(out=rs[0:rows], in_=sd[0:rows]), reads=[B_sd], writes=[B_rs])
                return rs, B_rs

            def square(ps_ap, B_ps, rows):
                sq, B_sq = h_r.next()
                T.op("act", lambda e: e.activation(out=sq[0:rows], in_=ps_ap, func=AF.Square), reads=[B_ps], writes=[B_sq])
                return sq, B_sq

            def store(q, dst, src_ap, B_src):
                T.dma(q, dst, src_ap, reads=[B_src], writes=[B_scr], owner=B_src)

            def rope_finish(rows, ps_ap, B_ps, gc, rgmat, cos, sin, B_cos, B_sin, rs, B_rs, dst):
                cbf, B_cbf = h_r.next()
                T.op("act", lambda e: e.activation(out=cbf[0:rows], in_=ps_ap, func=AF.Copy), reads=[B_ps], writes=[B_cbf])
                bi, B_b = px.next()
                T.mm([lambda e: e.matmul(bank(bi, rows), lhsT=rgmat[0:rows, 0:rows], rhs=cbf[0:rows], start=True, stop=True)],
                     reads=[B_cbf, B_rg], writes=[B_b])
                t1, B_t1 = f_r.next()
                T.op("dve", lambda e: e.scalar_tensor_tensor(out=t1[0:rows], in0=ps_ap, scalar=col(gc, rows), in1=cos[0:rows],
                                                              op0=ALU.mult, op1=ALU.mult), reads=[B_ps, B_cols, B_cos], writes=[B_t1])
                t2, B_t2 = f_r.next()
                T.op("dve", lambda e: e.tensor_tensor(out=t2[0:rows], in0=bank(bi, rows), in1=sin[0:rows], op=ALU.mult),
                     reads=[B_b, B_sin], writes=[B_t2])
                T.op("pool", lambda e: e.tensor_tensor(out=t1[0:rows], in0=t1[0:rows], in1=t2[0:rows], op=ALU.add),
                     reads=[B_t2, B_t1], writes=[B_t1])
                ob, B_ob = o_r.next()
                T.op("pool", lambda e: e.tensor_tensor(out=ob[0:rows], in0=t1[0:rows], in1=rs[0:rows], op=ALU.mult),
                     reads=[B_t1, B_rs], writes=[B_ob])
                store("sp", dst, ob[0:rows], B_ob)

            pre = {}

            def prefetch_sb(sbn):
                tn = sbn * 512
                T.dma("sp", posi[:], pos[tn:tn + 512].partition_broadcast(128), writes=[B_posi], owner=B_posi)
                pre["posi"] = sbn
                pre["xt"] = []
                for tt_ in range(2):
                    xt_, B_xt_ = xt_r.next()
                    T.dma("sp", xt_, x[tn + tt_ * 128:tn + (tt_ + 1) * 128, :], writes=[B_xt_], owner=B_xt_)
                    pre["xt"].append((xt_, B_xt_))

            for sb in range(NSB):
                t0 = sb * 512
                if pre.get("posi") != sb:
                    prefetch_sb(sb)
                T.op("dve", lambda e: e.tensor_copy(out=posf[:], in_=posi[:]), reads=[B_posi], writes=[B_posf])
                for ti, (cc, off) in enumerate(((0, 0.0), (0, 0.25), (1, 0.0), (1, 0.25))):
                    v, B_v = f_r.next()
                    T.op("dve", lambda e: e.tensor_scalar(out=v, in0=posf[:], scalar1=col(c_inv + cc), scalar2=off,
                                                           op0=ALU.mult, op1=ALU.add), reads=[B_posf, B_cols], writes=[B_v])
                    k1, B_k1 = f_r.next()
                    T.op("dve", lambda e: e.tensor_scalar(out=k1, in0=v, scalar1=MAGIC, scalar2=None, op0=ALU.add),
                         reads=[B_v], writes=[B_k1])
                    T.op("dve", lambda e: e.tensor_scalar(out=k1, in0=k1, scalar1=-MAGIC, scalar2=None, op0=ALU.add),
                         reads=[B_k1], writes=[B_k1])
                    T.op("dve", lambda e: e.tensor_tensor(out=v, in0=v, in1=k1, op=ALU.subtract), reads=[B_v, B_k1], writes=[B_v])
                    T.op("act", lambda e: e.activation(out=tabs[:, ti, :], in_=v, func=AF.Sin, scale=2.0 * math.pi),
                         reads=[B_v], writes=[B_tab[ti]])
                for tt in range(4):
                    r0 = t0 + tt * 128
                    if tt < 2:
                        xt, B_xt = pre["xt"][tt]
                    else:
                        xt, B_xt = xt_r.next()
                        T.dma("sp", xt, x[r0:r0 + 128, :], writes=[B_xt], owner=B_xt)
                    ss, B_ss = st_r.next()
                    T.op("act", lambda e: e.activation(out=junk[:], in_=xt, func=AF.Square, accum_out=ss),
                         reads=[B_xt], writes=[B_junk, B_ss])
                    T.op("act", lambda e: e.activation(out=ss, in_=ss, func=AF.Sqrt, scale=1.0 / D, bias=col(c_eps)),
                         reads=[B_ss, B_cols], writes=[B_ss])
                    T.op("dve", lambda e: e.reciprocal(out=ss, in_=ss), reads=[B_ss], writes=[B_ss])
                    xn, B_xn = xn_r.next()
                    T.op("dve", lambda e: e.tensor_scalar(out=xn, in0=xt, scalar1=ss, scalar2=None, op0=ALU.mult),
                         reads=[B_xt, B_ss], writes=[B_xn])
                    bi, B_b = pt.next()
                    pT = ps[:, bi, :].bitcast(BF16).rearrange("p (c t) -> p c t", t=128)
                    T.mm([(lambda e, c=c: e.transpose(pT[:, c, :], xn[:, c * 128:(c + 1) * 128], IDENT)) for c in range(8)],
                         reads=[B_xn, B_cb], writes=[B_b])
                    T.op("dve", lambda e: e.tensor_tensor(out=hT[:, :, tt * 128:(tt + 1) * 128], in0=pT,
                                                           in1=cols[:, c_gA:c_gA + 8].unsqueeze(2).broadcast_to([128, 8, 128]),
                                                           op=ALU.mult), reads=[B_b, B_cols], writes=[B_hT])

                def proj(bi, B_b, wt, c0, ncols, rhs, nk, extra_reads):
                    T.mm([(lambda e, kc=kc: e.matmul(bank(bi, ncols), lhsT=wt[:, kc, c0:c0 + ncols], rhs=rhs[:, kc, :],
                                                     start=(kc == 0), stop=(kc == nk - 1))) for kc in range(nk)],
                         reads=[B_win] + extra_reads, writes=[B_b])

                jobs = []

                def add(front, back):
                    jobs.append((front, back))

                def lat_job(ncs, cbase, gbase, dstt, B_dst):
                    stt_ = {}

                    def front():
                        banks = []
                        for c in range(ncs):
                            bi, B_b = pa.next()
                            proj(bi, B_b, win, cbase + c * 128, 128, hT, 8, [B_hT])
                            banks.append((bi, B_b))
                        stt_["banks"] = banks
                        stt_["sqs"] = [square(bank(bi), B_b, 128) for (bi, B_b) in banks]

                    def back():
                        banks, sqs = stt_["banks"], stt_["sqs"]
                        si, B_s = px.next()
                        T.mm([(lambda e, c=c: e.matmul(bank(si), lhsT=ONES, rhs=sqs[c][0], start=(c == 0), stop=(c == ncs - 1)))
                              for c in range(ncs)], reads=[B_cb] + [q_[1] for q_ in sqs], writes=[B_s])
                        rs, B_rs = make_rstd(bank(si), B_s, 128 * ncs, 128)
                        for c in range(ncs):
                            bi, B_b = banks[c]
                            T.op("dve", lambda e, c=c, bi=bi: e.scalar_tensor_tensor(out=dstt[:, c, :], in0=bank(bi), scalar=col(gbase + c),
                                                                                      in1=rs, op0=ALU.mult, op1=ALU.mult),
                                 reads=[B_b, B_cols, B_rs], writes=[B_dst])
                    add(front, back)

                lat_job(3, C_CQ, c_gql, cqn, B_cqn)

                def kr_job():
                    stt_ = {}

                    def front():
                        bi, B_b = pa.next()
                        proj(bi, B_b, win, C_KR, 64, hT, 8, [B_hT])
                        stt_["b"] = (bi, B_b)
                        stt_["sq"] = square(bank(bi, 64), B_b, 64)

                    def back():
                        bi, B_b = stt_["b"]
                        sq, B_sq = stt_["sq"]
                        si, B_s = px.next()
                        T.mm([lambda e: e.matmul(bank(si, 64), lhsT=cb[0:64, 5, 0:64], rhs=sq[0:64], start=True, stop=True)],
                             reads=[B_sq, B_cb], writes=[B_s])
                        rs, B_rs = make_rstd(bank(si, 64), B_s, 64, 64)
                        rope_finish(64, bank(bi, 64), B_b, c_gkr, RGKR, COSM, SINM, B_tab[3], B_tab[2], rs, B_rs, s_kr[:, t0:t0 + 512])
                    add(front, back)

                kr_job()
                lat_job(2, C_CKV, c_gkvl, ckvn, B_ckvn)

                def da_job(typ, h):
                    stt_ = {}

                    def front():
                        bi, B_b = pa.next()
                        proj(bi, B_b, win, typ * 1024 + h * 128, 128, hT, 8, [B_hT])
                        stt_["b"] = (bi, B_b)
                        stt_["sq"] = square(bank(bi), B_b, 128)

                    def back():
                        bi, B_b = stt_["b"]
                        sq, B_sq = stt_["sq"]
                        si, B_s = px.next()
                        T.mm([lambda e: e.matmul(bank(si), lhsT=B64, rhs=sq, start=True, stop=True)], reads=[B_sq, B_cb], writes=[B_s])
                        rs, B_rs = make_rstd(bank(si), B_s, 64, 128)
                        dst = (s_qda, s_kda)[typ][h, :, t0:t0 + 512]
                        rope_finish(128, bank(bi), B_b, (c_gq, c_gk)[typ], (RGQ, RGK)[typ], COSD, SIND, B_tab[1], B_tab[0], rs, B_rs, dst)
                    add(front, back)

                for typ in range(2):
                    for h in range(8):
                        da_job(typ, h)

                def mq_job(h):
                    stt_ = {}

                    def front():
                        ai, B_a = pa.next()
                        proj(ai, B_a, wuq, h * 192, 128, cqn, 3, [B_cqn])
                        bi2, B_b2 = pa.next()
                        proj(bi2, B_b2, wuq, h * 192 + 128, 64, cqn, 3, [B_cqn])
                        stt_["a"] = (ai, B_a)
                        stt_["b"] = (bi2, B_b2)
                        stt_["sqa"] = square(bank(ai), B_a, 128)
                        stt_["sqb"] = square(bank(bi2, 64), B_b2, 64)

                    def back():
                        ai, B_a = stt_["a"]
                        bi2, B_b2 = stt_["b"]
                        sqa, B_sqa = stt_["sqa"]
                        sqb, B_sqb = stt_["sqb"]
                        si, B_s = px.next()
                        T.mm([lambda e: e.matmul(bank(si), lhsT=ONES, rhs=sqa, start=True, stop=False),
                              lambda e: e.matmul(bank(si), lhsT=cb[0:64, 5, :], rhs=sqb[0:64], start=False, stop=True)],
                             reads=[B_cb, B_sqa, B_sqb], writes=[B_s])
                        rs, B_rs = make_rstd(bank(si), B_s, 192, 128)
                        ob, B_ob = o_r.next()
                        T.op("dve", lambda e: e.scalar_tensor_tensor(out=ob, in0=bank(ai), scalar=col(c_gqn), in1=rs,
                                                                      op0=ALU.mult, op1=ALU.mult), reads=[B_a, B_cols, B_rs], writes=[B_ob])
                        store("sp", s_qn[h, :, t0:t0 + 512], ob, B_ob)
                        rope_finish(64, bank(bi2, 64), B_b2, c_gqr, RGMQ, COSM, SINM, B_tab[3], B_tab[2], rs, B_rs, s_qr[h, :, t0:t0 + 512])
                    add(front, back)

                for h in range(8):
                    mq_job(h)

                def kn_job(h):
                    stt_ = {}

                    def front():
                        bi, B_b = pa.next()
                        T.mm([(lambda e, c=c: e.matmul(bank(bi), lhsT=wkn[:, c, h, :], rhs=ckvn[:, c, :], start=(c == 0), stop=(c == 1)))
                              for c in range(2)], reads=[B_win, B_ckvn], writes=[B_b])
                        stt_["b"] = (bi, B_b)
                        stt_["sq"] = square(bank(bi), B_b, 128)

                    def back():
                        bi, B_b = stt_["b"]
                        sq, B_sq = stt_["sq"]
                        si, B_s = px.next()
                        T.mm([lambda e: e.matmul(bank(si), lhsT=ONES, rhs=sq, start=True, stop=True)], reads=[B_sq, B_cb], writes=[B_s])
                        rs, B_rs = make_rstd(bank(si), B_s, 128, 128)
                        ob, B_ob = o_r.next()
                        T.op("dve", lambda e: e.scalar_tensor_tensor(out=ob, in0=bank(bi), scalar=col(c_gkn), in1=rs,
                                                                      op0=ALU.mult, op1=ALU.mult), reads=[B_b, B_cols, B_rs], writes=[B_ob])
                        store("sp", s_kn[h, :, t0:t0 + 512], ob, B_ob)
                    add(front, back)

                for h in range(8):
                    kn_job(h)

                def v_job(tt, half, kind, ovh):
                    stt_ = {}

                    def front():
                        bi, B_b = pa.next()
                        if kind == 0:
                            T.mm([(lambda e, kc=kc: e.matmul(bank(bi), lhsT=hT[:, kc, tt * 128:(tt + 1) * 128],
                                                             rhs=win[:, kc, 2048 + half * 512:2048 + (half + 1) * 512],
                                                             start=(kc == 0), stop=(kc == 7))) for kc in range(8)],
                                 reads=[B_win, B_hT], writes=[B_b])
                        else:
                            T.mm([(lambda e, c=c: e.matmul(bank(bi), lhsT=ckvn[:, c, tt * 128:(tt + 1) * 128],
                                                           rhs=wv[:, c, half * 4:(half + 1) * 4, :].rearrange("p h d -> p (h d)"),
                                                           start=(c == 0), stop=(c == 1))) for c in range(2)],
                                 reads=[B_win, B_ckvn], writes=[B_b])
                        stt_["b"] = (bi, B_b)

                    def back():
                        bi, B_b = stt_["b"]
                        if half == 0:
                            ovh["ov"] = ov_r.next()
                        ov, B_ov = ovh["ov"]
                        T.op("act", lambda e: e.activation(out=ov[:, half * 512:(half + 1) * 512], in_=bank(bi), func=AF.Copy),
                             reads=[B_b], writes=[B_ov])
                        if half == 1:
                            dstt = (s_vda, s_vm)[kind]
                            store("sp", dstt[t0 + tt * 128:t0 + (tt + 1) * 128, :], ov, B_ov)
                    add(front, back)

                for kind in range(2):
                    for tt in range(4):
                        ovh = {}
                        for half in range(2):
                            v_job(tt, half, kind, ovh)

                def gate_job(c):
                    stt_ = {}

                    def front():
                        bi, B_b = pa.next()
                        proj(bi, B_b, win, C_GATE + c * 128, 128, hT, 8, [B_hT])
                        stt_["b"] = (bi, B_b)

                    def back():
                        bi, B_b = stt_["b"]
                        ob, B_ob = o_r.next()
                        T.op("act", lambda e: e.activation(out=ob, in_=bank(bi), func=AF.Sigmoid, bias=col(c_bg + c)),
                             reads=[B_b, B_cols], writes=[B_ob])
                        store("sp", s_gate[c, :, t0:t0 + 512], ob, B_ob)
                    add(front, back)

                for c in range(16):
                    gate_job(c)

                jobs[0][0]()
                for ji in range(len(jobs)):
                    if ji + 1 < len(jobs):
                        jobs[ji + 1][0]()
                    jobs[ji][1]()
                    if ji == 4 and sb + 1 < NSB:
                        prefetch_sb(sb + 1)
            T.barrier()

        with contextlib.ExitStack() as ph:
            mixT = sb_("mixT", [128, 8, S], BF16, ph)
            B_mix = Buf("mix", multi=True)
            with contextlib.ExitStack() as phb:
                kT_r = Ring([sb_(f"kT{i}", [128, S], BF16, phb)[:] for i in range(2)], "kT")
                vD_r = Ring([sb_(f"vD{i}", [128, NT, 128], BF16, phb)[:] for i in range(2)], "vD")
                kn_r = Ring([sb_(f"kn{i}", [128, S], BF16, phb)[:] for i in range(2)], "kn")
                vM_r = Ring([sb_(f"vM{i}", [128, NT, 128], BF16, phb)[:] for i in range(2)], "vM")
                krT = sb_("krT", [128, S], BF16, phb)
                B_kr = Buf("krT")
                evk = T.op("dve", lambda e: e.memset(krT[64:128, :], 0.0), writes=[B_kr])
                B_kr.par = True
                T.dma("sp", krT[0:64, :], s_kr, reads=[B_scr], writes=[B_kr], owner=B_kr)
                q_r = Ring([sb_(f"q{i}", [128, 4, 512], BF16, phb)[:] for i in range(2)], "q")
                for (qap_, B_qq) in q_r.items:
                    T.op("dve", lambda e: e.memset(qap_, 0.0), writes=[B_qq])
                    B_qq.par = True
                g_r = Ring([sb_(f"g{i}", [128, 2, 512], BF16, phb)[:] for i in range(2)], "g")
                p_r = Ring([sb_(f"p{i}", [128, 512], BF16, phb)[:] for i in range(8)], "p")
                f_r = Ring([sb_(f"fb{i}", [128, 512], F32, phb)[:] for i in range(6)], "fb")
                h_r = Ring([sb_(f"hb{i}", [128, 512], BF16, phb)[:] for i in range(2)], "hb")
                psS = Ring([0, 1, 2], "pS")
                psS.items = [(i, PB[i]) for i in (0, 1, 2, 6)]
                OBANK = {"m": (3, PB[3]), "d0": (4, PB[4]), "d1": (5, PB[5])}
                pz = Ring([0], "pz")
                pz.items = [(i, PB[i]) for i in (7,)]
                onesf = sb_("onesf", [128, 128], F32, phb)
                B_onesf = Buf("onesf")
                T.op("dve", lambda e: e.memset(onesf[:], 1.0), writes=[B_onesf])
                pacc_r = {k_: Ring([sb_(f"pacc{k_}{i}", [128, 512], F32, phb)[:] for i in range(3)], "pacc" + k_) for k_ in ("m", "d1")}
                oc_r = {k_: Ring([sb_(f"oc{k_}{i}", [128, 512], F32, phb)[:] for i in range(1)], "oc" + k_) for k_ in ("m", "d0", "d1")}
                ACCENG = {"m": "dve", "d0": "pe", "d1": "pool"}
                LOOK = 3

                def front(jb):
                    kb, c0, j = jb["kb"], jb["c0"], jb["j"]
                    si, B_s = psS.next()
                    fns = []
                    rd = [B_cb]
                    parts = jb["parts"]
                    for pi_, (kap, qap, rows, B_k, B_q) in enumerate(parts):
                        fns.append(lambda e, kap=kap, qap=qap, rows=rows, pi_=pi_: e.matmul(
                            bank(si, 128, c0, 512), lhsT=kap[0:rows, kb * 128:(kb + 1) * 128], rhs=qap[0:rows, c0:512],
                            start=(pi_ == 0), stop=(pi_ == len(parts) - 1 and j < 0)))
                        rd += [B_k, B_q]
                    if j >= 0:
                        fns.append(lambda e: e.matmul(bank(si, 128, c0, c0 + 128), lhsT=IDENT, rhs=NEGM, start=False, stop=True))
                    T.mm(fns, reads=rd, writes=[B_s])
                    pt_, B_p = p_r.next()
                    T.op("act", lambda e: e.activation(out=pt_[:, c0:512], in_=bank(si, 128, c0, 512), func=AF.Exp, scale=jb["scale"]),
                         reads=[B_s], writes=[B_p])
                    jb["pt"], jb["B_p"] = pt_, B_p

                def back(jb):
                    kb, c0, nkb, key = jb["kb"], jb["c0"], jb["nkb"], jb["key"]
                    pt_, B_p = jb["pt"], jb["B_p"]
                    oi, B_o = OBANK[key]
                    g = jb["grp"]
                    T.mm([lambda e: e.matmul(bank(oi, 128, c0, 512), lhsT=jb["vt"][:, kb, :], rhs=pt_[:, c0:512],
                                             start=(kb == 0), stop=(kb == nkb - 1))], reads=[jb["B_v"], B_p], writes=[B_o])
                    if key == "d0":
                        zb, B_zb = OBANK["m"]
                        T.mm([lambda e: e.matmul(bank(zb, 128, c0, 512), lhsT=ONES, rhs=pt_[:, c0:512],
                                                 start=(kb == 0), stop=(kb == nkb - 1))], reads=[B_cb, B_p], writes=[B_zb])
                    else:
                        if kb < 2:
                            g["pacc"][(key, kb)] = pacc_r[key].next()
                        pa_, B_pa = g["pacc"][(key, kb % 2)]
                        if kb < 2:
                            if c0 > 0:
                                T.op(ACCENG[key], lambda e: e.memset(pa_[:, 0:c0], 0.0), writes=[B_pa])
                            T.op(ACCENG[key], lambda e: e.tensor_copy(out=pa_[:, c0:512], in_=pt_[:, c0:512]), reads=[B_p, B_pa], writes=[B_pa])
                        else:
                            T.op(ACCENG[key], lambda e: e.tensor_tensor(out=pa_[:, c0:512], in0=pa_[:, c0:512], in1=pt_[:, c0:512], op=ALU.add),
                                 reads=[B_p, B_pa], writes=[B_pa])
                    if kb == nkb - 1:
                        oc, B_oc = oc_r[key].next()
                        T.op("act", lambda e: e.activation(out=oc, in_=bank(oi), func=AF.Copy), reads=[B_o], writes=[B_oc])
                        if key == "d0":
                            zi, B_z = OBANK["m"]
                        else:
                            zi, B_z = pz.next()
                            pa0, B_pa0 = g["pacc"][(key, 0)]
                            pa1, B_pa1 = g["pacc"][(key, 1)]
                            T.mm([lambda e: e.matmul(bank(zi), lhsT=onesf[:], rhs=pa0, start=True, stop=False),
                                  lambda e: e.matmul(bank(zi), lhsT=onesf[:], rhs=pa1, start=False, stop=True)],
                                 reads=[B_onesf, B_pa0, B_pa1], writes=[B_z])
                        rz, B_rz = f_r.next()
                        T.op("dve", lambda e: e.reciprocal(out=rz, in_=bank(zi)), reads=[B_z], writes=[B_rz])
                        g["fin"][key] = (oc, B_oc, rz, B_rz)
                        if key == "m":
                            fin_mla(g)
                        elif key == "d1":
                            fin_da(g)

                def fin_mla(g):
                    oc, B_oc, rz, B_rz = g["fin"]["m"]
                    gt, B_g = g["gt"], g["B_g"]
                    T.op("dve", lambda e: e.tensor_tensor(out=oc, in0=oc, in1=rz, op=ALU.mult), reads=[B_oc, B_rz], writes=[B_oc])
                    T.op("dve", lambda e: e.tensor_tensor(out=oc, in0=oc, in1=gt[:, 1, :], op=ALU.mult), reads=[B_oc, B_g], writes=[B_oc])

                def fin_da(g):
                    om, B_om = g["fin"]["m"][0], g["fin"]["m"][1]
                    o0, B_o0, r0, B_r0 = g["fin"]["d0"]
                    o1, B_o1, r1, B_r1 = g["fin"]["d1"]
                    gt, B_g = g["gt"], g["B_g"]
                    h, t0 = g["h"], g["t0"]
                    T.op("dve", lambda e: e.tensor_tensor(out=o0, in0=o0, in1=r0, op=ALU.mult), reads=[B_o0, B_r0], writes=[B_o0])
                    T.op("dve", lambda e: e.scalar_tensor_tensor(out=o1, in0=o1, scalar=col(c_neglam), in1=r1,
                                                                  op0=ALU.mult, op1=ALU.mult), reads=[B_o1, B_r1, B_cols], writes=[B_o1])
                    T.op("dve", lambda e: e.tensor_tensor(out=o0, in0=o0, in1=o1, op=ALU.add), reads=[B_o0, B_o1], writes=[B_o0])
                    sq, B_sq = h_r.next()
                    T.op("pool", lambda e: e.tensor_tensor(out=sq, in0=o0, in1=o0, op=ALU.mult), reads=[B_o0], writes=[B_sq])
                    zi, B_z = pz.next()
                    T.mm([lambda e: e.matmul(bank(zi), lhsT=ONES, rhs=sq, start=True, stop=True)], reads=[B_sq, B_cb], writes=[B_z])
                    sd, B_sd = f_r.next()
                    T.op("act", lambda e: e.activation(out=sd, in_=bank(zi), func=AF.Ln, scale=1.0 / 128, bias=col(c_eps)),
                         reads=[B_z, B_cols], writes=[B_sd])
                    T.op("act", lambda e: e.activation(out=sd, in_=sd, func=AF.Exp, scale=-0.5), reads=[B_sd], writes=[B_sd])
                    T.op("dve", lambda e: e.scalar_tensor_tensor(out=o0, in0=o0, scalar=col(c_subs), in1=sd, op0=ALU.mult, op1=ALU.mult),
                         reads=[B_o0, B_sd, B_cols], writes=[B_o0])
                    T.op("dve", lambda e: e.tensor_tensor(out=o0, in0=o0, in1=gt[:, 0, :], op=ALU.mult), reads=[B_o0, B_g], writes=[B_o0])
                    T.op("dve", lambda e: e.tensor_tensor(out=mixT[:, h, t0:t0 + 512], in0=o0, in1=om, op=ALU.add),
                         reads=[B_o0, B_om], writes=[B_mix])

                pending = []

                def push(jb):
                    front(jb)
                    pending.append(jb)
                    if len(pending) > LOOK:
                        back(pending.pop(0))

                for h in range(8):
                    kT, B_kT = kT_r.next()
                    T.dma("sp", kT, s_kda[h], reads=[B_scr], writes=[B_kT], owner=B_kT)
                    vD, B_vD = vD_r.next()
                    T.dma("sp", vD, s_vda[:, h * 128:(h + 1) * 128].rearrange("(n p) d -> p n d", p=128),
                          reads=[B_scr], writes=[B_vD], owner=B_vD)
                    kn, B_kn = kn_r.next()
                    T.dma("sp", kn, s_kn[h], reads=[B_scr], writes=[B_kn], owner=B_kn)
                    vM, B_vM = vM_r.next()
                    T.dma("sp", vM, s_vm[:, h * 128:(h + 1) * 128].rearrange("(n p) d -> p n d", p=128),
                          reads=[B_scr], writes=[B_vM], owner=B_vM)
                    for sbi in range(NSB):
                        t0 = sbi * 512
                        qt, B_q = q_r.next()
                        T.dma("sp", qt[0:64, 0, :], s_qda[h, 0:64, t0:t0 + 512], reads=[B_scr], writes=[B_q], owner=B_q)
                        T.dma("sp", qt[64:128, 3, :], s_qda[h, 64:128, t0:t0 + 512], reads=[B_scr], writes=[B_q], owner=B_q)
                        T.dma("sp", qt[:, 1, :], s_qn[h, :, t0:t0 + 512], reads=[B_scr], writes=[B_q], owner=B_q)
                        T.dma("sp", qt[0:64, 2, :], s_qr[h, :, t0:t0 + 512], reads=[B_scr], writes=[B_q], owner=B_q)
                        gt, B_g = g_r.next()
                        T.dma("sp", gt[:], s_gate.rearrange("(a c) p t -> c p a t", a=2)[h, :, :, t0:t0 + 512], reads=[B_scr], writes=[B_g], owner=B_g)
                        grp = {"pacc": {}, "fin": {}, "gt": gt, "B_g": B_g, "h": h, "t0": t0}
                        nkb = 4 * sbi + 4
                        for kb in range(nkb):
                            j = kb - 4 * sbi
                            push({"kb": kb, "j": j, "c0": max(j, 0) * 128, "nkb": nkb, "key": "m", "grp": grp, "scale": 192 ** -0.5,
                                  "vt": vM, "B_v": B_vM,
                                  "parts": [(kn, qt[:, 1, :], 128, B_kn, B_q), (krT[:], qt[:, 2, :], 128, B_kr, B_q)]})
                        for kb in range(nkb):
                            j = kb - 4 * sbi
                            for m in range(2):
                                push({"kb": kb, "j": j, "c0": max(j, 0) * 128, "nkb": nkb, "key": f"d{m}", "grp": grp, "scale": 0.125,
                                      "vt": vD, "B_v": B_vD,
                                      "parts": [(kT, qt[:, 3 * m, :], 128, B_kT, B_q)]})
                while pending:
                    back(pending.pop(0))
                T.barrier()

            B_mix.multi = False
            with contextlib.ExitStack() as phc:
                wo = sb_("wo", [128, 8, D], BF16, phc)
                B_wo = Buf("wo")
                T.dma("pool", wo[:], w_o.rearrange("(c p) n -> p c n", p=128), writes=[B_wo], owner=B_wo)
                wr = sb_("wr", [128, 8, 36], F32, phc)
                B_wr = Buf("wr", multi=True)
                T.dma("sp", wr[:, :, 0:4], w_group.rearrange("(c p) n -> p c n", p=128), writes=[B_wr], owner=B_wr)
                T.dma("sp", wr[:, :, 4:36], w_router.rearrange("(c p) n -> p c n", p=128), writes=[B_wr], owner=B_wr)
                br = sb_("br", [128, 36], F32, phc)
                T.dma("sp", br[:, 0:4], b_group.partition_broadcast(128), writes=[B_wr], owner=B_wr)
                T.dma("sp", br[:, 4:36], b_router.partition_broadcast(128), writes=[B_wr], owner=B_wr)
                gfr = sb_("gfr", [128, D], F32, phc)
                T.dma("sp", gfr[:], ffn_norm_g.partition_broadcast(128), writes=[B_wr], owner=B_wr)
                cm = sb_("cm", [128, 1 + NCH], F32, phc)
                T.dma("sp", cm[:], cmoe, writes=[B_wr], owner=B_wr)
                h2tok = sb_("h2tok", [128, NT, D], BF16, phc)
                B_h2 = Buf("h2tok", multi=True)
                oh_all = sb_("oh_all", [128, 2, NT, NE], F32, phc)
                p12 = sb_("p12", [128, 2, NT], F32, phc)
                B_plan = Buf("plan", multi=True)
                carry = sb_("carry", [128, NE], F32, phc)
                B_carry = Buf("carry")
                T.op("dve", lambda e: e.memset(carry[:], 0.0), writes=[B_carry])
                xt_r = Ring([sb_(f"cx{i}", [128, D], F32, phc)[:] for i in range(2)], "cx")
                x1_r = Ring([sb_(f"x1{i}", [128, D], F32, phc)[:] for i in range(2)], "x1")
                junk = sb_("junkc", [128, D], BF16, phc)
                B_junk = Buf("junkc")
                hf_r = Ring([sb_(f"hf{i}", [128, 8, 128], F32, phc)[:] for i in range(1)], "hf")
                sm = sb_("sm", [128, 2, 256], F32, phc)
                sm_r = Ring([sm[:, i, :] for i in range(2)], "sm")
                abf_r = Ring([sb_(f"abf{i}", [128, NE], BF16, phc)[:] for i in range(2)], "abf")
                pc = Ring([0, 1], "pc")
                pc.items = [((0, 1), PB[0]), ((2, 3), PB[2])]
                ptc = Ring([0], "ptc")
                ptc.items = [((4, 5), PB[4])]
                prt = (6, PB[6])
                prk = (7, PB[7])
                ctxs = {}

                def partA(t):
                    r0 = t * 128
                    (b0, b1), B_b = pc.next()
                    for half, bi in enumerate((b0, b1)):
                        T.mm([(lambda e, hh=hh: e.matmul(bank(bi), lhsT=mixT[:, hh, r0:r0 + 128], rhs=wo[:, hh, half * 512:(half + 1) * 512],
                                                         start=(hh == 0), stop=(hh == 7))) for hh in range(8)],
                             reads=[B_mix, B_wo], writes=[B_b])
                    xt, B_xt = xt_r.next()
                    T.dma("sp", xt, x[r0:r0 + 128, :], writes=[B_xt], owner=B_xt)
                    x1, B_x1 = x1_r.next()
                    T.op("dve", lambda e: e.tensor_tensor(out=x1.rearrange("p (a n) -> p a n", a=2), in0=ps[:, b0:b0 + 2, :],
                                                           in1=xt.rearrange("p (a n) -> p a n", a=2), op=ALU.add),
                         reads=[B_b, B_xt], writes=[B_x1])
                    T.dma("sp", out[r0:r0 + 128, :], x1, reads=[B_x1], writes=[B_out], owner=B_x1)
                    s_, B_s = sm_r.next()
                    T.op("act", lambda e: e.activation(out=junk[:], in_=x1, func=AF.Square, accum_out=s_[:, 0:1]),
                         reads=[B_x1], writes=[B_junk, B_s])
                    T.op("act", lambda e: e.activation(out=s_[:, 1:2], in_=s_[:, 0:1], func=AF.Sqrt, scale=1.0 / D, bias=col(c_eps)),
                         reads=[B_s, B_cols], writes=[B_s])
                    T.op("dve", lambda e: e.reciprocal(out=s_[:, 2:3], in_=s_[:, 1:2]), reads=[B_s], writes=[B_s])
                    T.op("dve", lambda e: e.tensor_scalar(out=xt, in0=x1, scalar1=s_[:, 2:3], scalar2=None, op0=ALU.mult),
                         reads=[B_x1, B_s], writes=[B_xt])
                    T.op("pool", lambda e: e.tensor_tensor(out=h2tok[:, t, :], in0=xt, in1=gfr[:], op=ALU.mult), reads=[B_xt, B_wr], writes=[B_h2])
                    (tb, _), B_tb = ptc.next()
                    pTf = ps[:, tb:tb + 2, :].rearrange("p a (c t) -> p (a c) t", t=128)
                    T.mm([(lambda e, c=c: e.transpose(pTf[:, c, :], xt[:, c * 128:(c + 1) * 128], identf[:])) for c in range(8)],
                         reads=[B_xt, B_cf], writes=[B_tb])
                    yield
                    hf, B_hf = hf_r.next()
                    T.op("dve", lambda e: e.tensor_tensor(out=hf, in0=pTf, in1=cols[:, c_gF:c_gF + 8].unsqueeze(2).broadcast_to([128, 8, 128]),
                                                           op=ALU.mult), reads=[B_tb, B_cols], writes=[B_hf])
                    T.mm([(lambda e, c=c: e.matmul(bank(prt[0], 128, 0, 36), lhsT=hf[:, c, :], rhs=wr[:, c, :], start=(c == 0), stop=(c == 7)))
                          for c in range(8)], reads=[B_hf, B_wr], writes=[prt[1]])
                    def dv(fn):
                        T.op("dve", fn, reads=[B_s], writes=[B_s])
                    yield
                    lg = s_[:, 4:40]
                    T.op("dve", lambda e: e.tensor_tensor(out=lg, in0=bank(prt[0], 128, 0, 36), in1=br[:], op=ALU.add),
                         reads=[prt[1], B_wr, B_s], writes=[B_s])
                    ctxs[t] = (s_, B_s)

                def partB(t):
                    s_, B_s = ctxs.pop(t)

                    def dv(fn):
                        T.op("dve", fn, reads=[B_s], writes=[B_s])
                    gmax = s_[:, 40:41]
                    dv(lambda e: e.tensor_reduce(out=gmax, in_=s_[:, 4:8], axis=AX.X, op=ALU.max))
                    ohg = s_[:, 44:48]
                    dv(lambda e: e.tensor_scalar(out=ohg, in0=s_[:, 4:8], scalar1=gmax, scalar2=None, op0=ALU.is_ge))
                    dv(lambda e: e.tensor_scalar(out=s_[:, 48:52], in0=s_[:, 4:8], scalar1=gmax, scalar2=None, op0=ALU.subtract))
                    T.op("act", lambda e: e.activation(out=s_[:, 48:52], in_=s_[:, 48:52], func=AF.Exp, accum_out=s_[:, 41:42]),
                         reads=[B_s], writes=[B_s])
                    dv(lambda e: e.reciprocal(out=s_[:, 42:43], in_=s_[:, 41:42]))
                    dv(lambda e: e.tensor_scalar(out=s_[:, 52:56], in0=ohg, scalar1=-1.0, scalar2=1.0e4, op0=ALU.add, op1=ALU.mult))
                    me = s_[:, 64:96]
                    dv(lambda e: e.tensor_tensor(out=me.rearrange("p (g k) -> p g k", k=8), in0=s_[:, 8:40].rearrange("p (g k) -> p g k", k=8),
                                                 in1=s_[:, 52:56].unsqueeze(2).broadcast_to([128, 4, 8]), op=ALU.add))
                    m1 = s_[:, 56:57]
                    dv(lambda e: e.tensor_reduce(out=m1, in_=me, axis=AX.X, op=ALU.max))
                    oh1 = oh_all[:, 0, t, :]
                    oh2 = oh_all[:, 1, t, :]
                    T.op("dve", lambda e: e.tensor_scalar(out=oh1, in0=me, scalar1=m1, scalar2=None, op0=ALU.is_ge), reads=[B_s], writes=[B_plan])
                    yield
                    me2 = s_[:, 128:160]
                    T.op("dve", lambda e: e.scalar_tensor_tensor(out=me2, in0=oh1, scalar=-1.0e4, in1=me, op0=ALU.mult, op1=ALU.add),
                         reads=[B_s, B_plan], writes=[B_s])
                    m2 = s_[:, 57:58]
                    dv(lambda e: e.tensor_reduce(out=m2, in_=me2, axis=AX.X, op=ALU.max))
                    T.op("dve", lambda e: e.tensor_scalar(out=oh2, in0=me2, scalar1=m2, scalar2=None, op0=ALU.is_ge), reads=[B_s], writes=[B_plan])
                    dv(lambda e: e.tensor_tensor(out=s_[:, 58:59], in0=m1, in1=m2, op=ALU.subtract))
                    T.op("act", lambda e: e.activation(out=s_[:, 59:60], in_=s_[:, 58:59], func=AF.Sigmoid), reads=[B_s], writes=[B_s])
                    T.op("dve", lambda e: e.tensor_tensor(out=wt12[:, t, 0:1], in0=s_[:, 59:60], in1=s_[:, 42:43], op=ALU.mult), reads=[B_s], writes=[B_wt])
                    T.op("dve", lambda e: e.tensor_tensor(out=wt12[:, t, 1:2], in0=s_[:, 42:43], in1=wt12[:, t, 0:1], op=ALU.subtract),
                         reads=[B_s, B_wt], writes=[B_wt])
                    abf, B_abf = abf_r.next()
                    T.op("dve", lambda e: e.tensor_tensor(out=abf, in0=oh1, in1=oh2, op=ALU.add), reads=[B_plan], writes=[B_abf])
                    yield
                    T.mm([lambda e: e.matmul(bank(prk[0], 128, 0, 32), lhsT=USTRICT, rhs=abf, start=True, stop=True)], reads=[B_abf, B_cb], writes=[prk[1]])
                    T.mm([lambda e: e.matmul(bank(prk[0], 128, 32, 64), lhsT=ONES, rhs=abf, start=True, stop=True)], reads=[B_abf, B_cb], writes=[prk[1]])
                    rk = s_[:, 160:192]
                    T.op("dve", lambda e: e.tensor_tensor(out=rk, in0=bank(prk[0], 128, 0, 32), in1=carry[:], op=ALU.add),
                         reads=[prk[1], B_carry, B_s], writes=[B_s])
                    T.op("dve", lambda e: e.tensor_tensor(out=carry[:], in0=bank(prk[0], 128, 32, 64), in1=carry[:], op=ALU.add),
                         reads=[prk[1], B_carry, B_s], writes=[B_carry])
                    for k_ in range(2):
                        T.op("dve", lambda e: e.tensor_tensor(out=s_[:, 192:224], in0=oh_all[:, k_, t, :], in1=rk, op=ALU.mult),
                             reads=[B_s, B_plan], writes=[B_s])
                        T.op("dve", lambda e: e.tensor_reduce(out=p12[:, k_, t:t + 1], in_=s_[:, 192:224], axis=AX.X, op=ALU.add),
                             reads=[B_s], writes=[B_plan])

                def run(g):
                    try:
                        next(g)
                        return True
                    except StopIteration:
                        return False

                gA = partA(0)
                while run(gA):
                    pass
                gB_prev = None
                for t in range(NT):
                    gA = partA(t + 1) if t + 1 < NT else None
                    gB = partB(t)
                    if gA is not None:
                        run(gA)
                    run(gB)
                    if gA is not None:
                        run(gA)
                    run(gB)
                    if gA is not None:
                        while run(gA):
                            pass
                    if gB_prev is not None:
                        while run(gB_prev):
                            pass
                    gB_prev = gB
                while run(gB_prev):
                    pass
                B_plan.multi = False
                B_wt.multi = False
                g1 = sb_("gplan", [128, 8, NE], F32, phc)
                B_g1 = Buf("g1")
                def gp(fn, extra=()):
                    T.op("dve", fn, reads=[B_g1, B_carry, B_plan, B_wr] + list(extra), writes=[B_g1])
                gp(lambda e: e.tensor_scalar(out=g1[:, 0, :], in0=carry[:], scalar1=63.5, scalar2=1.0 / 128, op0=ALU.add, op1=ALU.mult))
                gp(lambda e: e.tensor_scalar(out=g1[:, 0, :], in0=g1[:, 0, :], scalar1=MAGIC, scalar2=None, op0=ALU.add))
                gp(lambda e: e.tensor_scalar(out=g1[:, 0, :], in0=g1[:, 0, :], scalar1=-MAGIC, scalar2=128.0, op0=ALU.add, op1=ALU.mult))
                gp(lambda e: e.tensor_copy(out=g1[:, 1, :], in_=g1[:, 0, :]))
                src_, dst_ = 1, 2
                for k_ in (1, 2, 4, 8, 16):
                    gp(lambda e: e.tensor_copy(out=g1[:, dst_, 0:k_], in_=g1[:, src_, 0:k_]))
                    gp(lambda e: e.tensor_tensor(out=g1[:, dst_, k_:NE], in0=g1[:, src_, k_:NE], in1=g1[:, src_, 0:NE - k_], op=ALU.add))
                    src_, dst_ = dst_, src_
                pends = g1[:, src_, :]
                pstart = g1[:, 3, :]
                gp(lambda e: e.tensor_tensor(out=pstart, in0=pends, in1=g1[:, 0, :], op=ALU.subtract))
                big = sb_("bigt", [128, max(NCH, NT) * NE], F32, phc)
                destf = sb_("destf", [128, 2, NT], F32, phc)
                for k_ in range(2):
                    gp(lambda e: e.tensor_tensor(out=big[:, 0:NT * NE].rearrange("p (t n) -> p t n", n=NE), in0=oh_all[:, k_, :, :],
                                                 in1=pstart.unsqueeze(1).broadcast_to([128, NT, NE]), op=ALU.mult))
                    gp(lambda e: e.tensor_reduce(out=destf[:, k_, :], in_=big[:, 0:NT * NE].rearrange("p (t n) -> p t n", n=NE), axis=AX.X, op=ALU.add))
                    gp(lambda e: e.tensor_tensor(out=destf[:, k_, :], in0=destf[:, k_, :], in1=p12[:, k_, :], op=ALU.add))
                    T.op("dve", lambda e: e.tensor_copy(out=dest[:, k_, :], in_=destf[:, k_, :]), reads=[B_g1], writes=[B_dest])
                gp(lambda e: e.tensor_tensor(out=big[:, 0:NCH * NE].rearrange("p (c n) -> p c n", n=NE),
                                             in0=pends.unsqueeze(1).broadcast_to([128, NCH, NE]),
                                             in1=cm[:, 1:1 + NCH].unsqueeze(2).broadcast_to([128, NCH, NE]), op=ALU.is_le))
                cef = sb_("cef", [128, 4, NCH], F32, phc)
                gp(lambda e: e.tensor_reduce(out=cef[:, 0, :], in_=big[:, 0:NCH * NE].rearrange("p (c n) -> p c n", n=NE), axis=AX.X, op=ALU.add))
                gp(lambda e: e.tensor_scalar(out=cef[:, 0, :], in0=cef[:, 0, :], scalar1=float(NE - 1), scalar2=None, op0=ALU.min))
                gp(lambda e: e.memset(cef[:, 1, :], 0.0))
                gp(lambda e: e.tensor_tensor(out=cef[:, 1, 1:NCH], in0=cef[:, 0, 1:NCH], in1=cef[:, 0, 0:NCH - 1], op=ALU.is_equal))
                gp(lambda e: e.tensor_scalar(out=cef[:, 2, :], in0=cef[:, 0, :], scalar1=256.0, scalar2=None, op0=ALU.mult))
                gp(lambda e: e.scalar_tensor_tensor(out=cef[:, 2, :], in0=cef[:, 1, :], scalar=16384.0, in1=cef[:, 2, :], op0=ALU.mult, op1=ALU.add))
                for hh in range(2):
                    T.op("dve", lambda e: e.tensor_scalar(out=idx2[:, hh, :], in0=cef[:, 2, :], scalar1=cm[:, 0:1], scalar2=float(hh), op0=ALU.add, op1=ALU.add),
                         reads=[B_g1, B_wr], writes=[B_dest])
                dreg = nc.gpsimd.to_reg(NCH * 128 - 1)
                B_h2.multi = False
                for t in range(NT):
                    for k_ in range(2):
                        T.idma(out=xs, out_offset=bass.IndirectOffsetOnAxis(ap=dest[:, k_, t:t + 1], axis=0), in_=h2tok[:, t, :], in_offset=None,
                               reads=[B_h2, B_dest], writes=[B_xs], owner=B_h2, bounds_check=dreg, oob_is_err=False)
                T.barrier()

        with contextlib.ExitStack() as phd:
            w1v = w1.rearrange("e (p h j) n -> (e p h) (j n)", p=128, h=2, j=4)
            w3v = w3.rearrange("e (p h j) n -> (e p h) (j n)", p=128, h=2, j=4)
            w2v = w2.rearrange("e (p h j) n -> (e p h) (j n)", p=128, h=2, j=2)
            wbt = sb_("wbt", [128, 3, 4096], BF16, phd)
            BW = [Buf(f"W{i}") for i in range(3)]
            T.op("dve", lambda e: e.memset(wbt[:], 0.0), writes=BW)
            for b_ in BW:
                b_.par = True
            xc_r = Ring([sb_(f"xc{i}", [128, D], BF16, phd)[:] for i in range(4)], "xc")
            xT_r = Ring([sb_(f"xT{i}", [128, 8, 128], BF16, phd)[:] for i in range(2)], "xT")
            sl_r = Ring([sb_(f"sl{i}", [128, 512], F32, phd)[:] for i in range(2)], "sl")
            G_r = Ring([sb_(f"G{i}", [128, 512], BF16, phd)[:] for i in range(2)], "G")
            GT_r = Ring([sb_(f"GT{i}", [128, 4, 128], BF16, phd)[:] for i in range(2)], "GT")
            yo_r = Ring([sb_(f"yo{i}", [128, D], F32, phd)[:] for i in range(2)], "yo")
            PXT = ((0, 1), PB[0])
            PH1 = (2, PB[2])
            PH3 = (3, PB[3])
            PGT = (4, PB[4])
            PY = ((5, 6), PB[5])
            st1 = {}

            xcs = {}

            def xload(cn):
                xc_, B_xc_ = xc_r.next()
                T.dma("sp", xc_, xs[cn * 128:(cn + 1) * 128, :], reads=[B_xs], writes=[B_xc_], owner=B_xc_)
                xcs[cn] = (xc_, B_xc_)

            xload(0)
            xload(1)

            def stage1(c):
                if c + 2 < NCH:
                    xload(c + 2)
                xc, B_xc = xcs.pop(c)
                wb = wbt
                (x0, x1b), B_px = PXT
                pxt = ps[:, x0:x0 + 2, :].rearrange("p a (c t) -> p (a c) t", t=128)
                xcv = xc.rearrange("t (p j) -> t j p", j=8)
                T.mm([(lambda e, jj=jj: e.matmul(pxt[:, jj, :], lhsT=xcv[:, jj, :], rhs=IDENT, start=True, stop=True)) for jj in range(8)],
                     reads=[B_xc, B_cb], writes=[B_px])
                xT, B_xT = xT_r.next()
                T.op("act", lambda e: e.activation(out=xT, in_=pxt, func=AF.Copy), reads=[B_px], writes=[B_xT])
                w1b = wb[:, 0, :].rearrange("p (j n) -> p j n", n=512)
                w3b = wb[:, 1, :].rearrange("p (j n) -> p j n", n=512)
                T.mm([(lambda e, jj=jj: e.matmul(bank(PH1[0]), lhsT=xT[:, jj, :], rhs=w1b[:, jj, :], start=(jj == 0), stop=(jj == 7))) for jj in range(8)],
                     reads=[B_xT, BW[0]], writes=[PH1[1]])
                T.mm([(lambda e, jj=jj: e.matmul(bank(PH3[0]), lhsT=xT[:, jj, :], rhs=w3b[:, jj, :], start=(jj == 0), stop=(jj == 7))) for jj in range(8)],
                     reads=[B_xT, BW[1]], writes=[PH3[1]])
                sl, B_sl = sl_r.next()
                T.op("act", lambda e: e.activation(out=sl, in_=bank(PH1[0]), func=AF.Silu), reads=[PH1[1]], writes=[B_sl])
                G, B_G = G_r.next()
                T.op("dve", lambda e: e.tensor_tensor(out=G, in0=bank(PH3[0]), in1=sl, op=ALU.mult), reads=[PH3[1], B_sl], writes=[B_G])
                st1[c] = (G, B_G, wb, None)

            breg = nc.gpsimd.to_reg(NE * 256 - 1)

            def gathers(c, which):
                for mi, wv_ in enumerate((w1v, w3v, w2v)):
                    if (mi == 2) != (which == 2):
                        continue
                    for hh in range(2):
                        T.idma(out=wbt[:, mi, hh * 2048:(hh + 1) * 2048], out_offset=None, in_=wv_,
                               in_offset=bass.IndirectOffsetOnAxis(ap=idx2[:, hh, c:c + 1], axis=0),
                               reads=[B_dest], writes=[BW[mi]], owner=BW[mi], bounds_check=breg, oob_is_err=False)

            def stage2(c):
                G, B_G, wb, B_w = st1.pop(c)
                Gv = G.rearrange("t (p j) -> t j p", j=4)
                pgt = ps[:, PGT[0], :].rearrange("p (c t) -> p c t", t=128)
                T.mm([(lambda e, jj=jj: e.matmul(pgt[:, jj, :], lhsT=Gv[:, jj, :], rhs=IDENT, start=True, stop=True)) for jj in range(4)],
                     reads=[B_G, B_cb], writes=[PGT[1]])
                GT, B_GT = GT_r.next()
                T.op("act", lambda e: e.activation(out=GT, in_=pgt, func=AF.Copy), reads=[PGT[1]], writes=[B_GT])
                w2b = wb[:, 2, :].rearrange("p (j n) -> p j n", n=1024)
                (y0, y1b), B_py = PY
                for half, bi in enumerate((y0, y1b)):
                    T.mm([(lambda e, jj=jj: e.matmul(bank(bi), lhsT=GT[:, jj, :], rhs=w2b[:, jj, half * 512:(half + 1) * 512], start=(jj == 0), stop=(jj == 3)))
                          for jj in range(4)], reads=[B_GT, BW[2]], writes=[B_py])
                yo, B_yo = yo_r.next()
                T.op("dve", lambda e: e.tensor_copy(out=yo.rearrange("p (a n) -> p a n", a=2), in_=ps[:, y0:y0 + 2, :]), reads=[B_py], writes=[B_yo])
                T.dma("sp", ys[c * 128:(c + 1) * 128, :], yo, reads=[B_yo], writes=[B_ys], owner=B_yo)

            gathers(0, 1)
            gathers(0, 2)
            for c in range(NCH + 1):
                if c < NCH:
                    stage1(c)
                if c + 1 < NCH:
                    gathers(c + 1, 1)
                if c >= 1:
                    stage2(c - 1)
                    if c < NCH:
                        gathers(c, 2)
            T.barrier()
            dreg2 = nc.gpsimd.to_reg(NCH * 128 - 1)
            y_r = Ring([sb_(f"yg{i}", [128, 2, D], F32, phd)[:] for i in range(2)], "yg")
            xo_r = Ring([sb_(f"xo{i}", [128, D], F32, phd)[:] for i in range(3)], "xo")
            exo = {}

            def eload(tn):
                xo_, B_xo_ = xo_r.next()
                T.dma("sp", xo_, out[tn * 128:(tn + 1) * 128, :], reads=[B_out], writes=[B_xo_], owner=B_xo_)
                exo[tn] = (xo_, B_xo_)

            eload(0)
            for t in range(NT):
                r0 = t * 128
                yg, B_yg = y_r.next()
                B_yg.par = True
                for k_ in range(2):
                    T.idma(out=yg[:, k_, :], out_offset=None, in_=ys, in_offset=bass.IndirectOffsetOnAxis(ap=dest[:, k_, t:t + 1], axis=0),
                           reads=[B_ys, B_dest], writes=[B_yg], owner=B_yg, bounds_check=dreg2, oob_is_err=False)
                if t + 1 < NT:
                    eload(t + 1)
                xo, B_xo = exo.pop(t)
                for k_ in range(2):
                    T.op("dve", lambda e: e.scalar_tensor_tensor(out=xo, in0=yg[:, k_, :], scalar=wt12[:, t, k_:k_ + 1], in1=xo, op0=ALU.mult, op1=ALU.add),
                         reads=[B_yg, B_xo, B_wt], writes=[B_xo])
                T.dma("sp", out[r0:r0 + 128, :], xo, reads=[B_xo], writes=[B_out], owner=B_xo)
            T.barrier()
    return nc


def _consts():
    cst = np.zeros((128, 7, 128), np.float32)
    cst[:, 0, :] = np.eye(128, dtype=np.float32)
    k = np.arange(128)[:, None]
    q = np.arange(128)[None, :]
    cst[:, 1, :] = np.where(k > q, NEG, 0.0)
    cst[:, 2, :] = ((k // 64) == (q // 64)).astype(np.float32)
    for m in range(2):
        for d in range(8):
            cst[m * 64 + d + 8, 3, m * 64 + d] = -1.0
            cst[m * 64 + d, 3, m * 64 + d + 8] = 1.0
    for d in range(32):
        cst[d + 32, 4, d] = -1.0
        cst[d, 4, d + 32] = 1.0
    cst[:, 5, :] = 1.0
    cst[:, 6, :] = (k < q).astype(np.float32)
    cinv = np.zeros((128, 2), np.float32)
    for p in range(128):
        d = p % 64
        if d < 16:
            cinv[p, 0] = THETA ** (-(2.0 * (d % 8)) / 16.0) / (2.0 * math.pi)
        if p < 64:
            cinv[p, 1] = THETA ** (-(2.0 * (p % 32)) / 64.0) / (2.0 * math.pi)
    return cst, cinv


_NC_CACHE = {}


def kernel(**inputs):
    x = np.asarray(inputs["x"])
    Bn, S, _ = x.shape
    if S not in _NC_CACHE:
        _NC_CACHE[S] = build(S)
    nc = _NC_CACHE[S]
    cst, cinv = _consts()
    shared = {}
    for k_, v in inputs.items():
        if k_ in ("x", "positions"):
            continue
        a = np.ascontiguousarray(np.asarray(v))
        shared[k_] = a.reshape(a.shape[1:]) if a.shape[0] == 1 else a
    shared["b_gate"] = shared["b_gate"].reshape(-1)
    shared["cst"] = cst
    shared["cinv"] = cinv
    nch = 2 * (S // 128) + NE
    cmoe = np.zeros((128, 1 + nch), np.float32)
    cmoe[:, 0] = 2.0 * np.arange(128)
    cmoe[:, 1:] = 128.0 * np.arange(nch)[None, :]
    shared["cmoe"] = cmoe
    positions = np.asarray(inputs["positions"]).astype(np.int32)
    in_maps = []
    for b in range(Bn):
        m = dict(shared)
        m["x"] = np.ascontiguousarray(x[b])
        m["pos"] = np.ascontiguousarray(positions[b])
        in_maps.append(m)
    res = run_bass_kernel_spmd(nc, in_maps, core_ids=list(range(Bn)))
    return np.stack([np.asarray(r["out"]) for r in res.results], axis=0).astype(np.float32)
```

```python
import math
import contextlib
import numpy as np
import concourse.bass as bass
import concourse.mybir as mybir
from concourse.bass_utils import run_bass_kernel_spmd

F32 = mybir.dt.float32
BF16 = mybir.dt.bfloat16
I32 = mybir.dt.int32
AF = mybir.ActivationFunctionType
ALU = mybir.AluOpType
AX = mybir.AxisListType

D = 1024
NCORES = 8
IN_COLS = 5824
C_CQ, C_CKV, C_KR, C_GATE = 3072, 3456, 3712, 3776
EPS = 1e-6
THETA = 500000.0
LAMBDA_INIT = 0.8 - 0.6 * math.exp(0.0)
NE = 32
DE = 512
MAGIC = 12582912.0
NEG = -30000.0


class Buf:
    def __init__(self, name, multi=False):
        self.name = name
        self.w = {}
        self.rs = {}
        self.multi = multi
        self.dsem = None
        self.dcnt = 0


class Tracker:
    def __init__(self, nc, stack):
        self.nc = nc
        self.stack = stack
        self.engs = {"pe": nc.tensor, "act": nc.scalar, "dve": nc.vector, "pool": nc.gpsimd, "sp": nc.sync}
        self.sem = {}
        self.cnt = {}
        self.nsem = 0
        self.waited = {}
        self.last = {}
        self.dbufs = []
        for k in self.engs:
            self._newsem(k)

    def _mksem(self, name):
        self.nsem += 1
        return self.stack.enter_context(self.nc.semaphore(f"{name}_{self.nsem}"))

    def _newsem(self, k):
        self.sem[k] = (self._mksem("e" + k), self.nsem)
        self.cnt[k] = 0

    def _wait(self, eng, ev):
        sem, sid, val, src, buf = ev
        if src == "dma":
            val = 16 * buf.dcnt
        elif src == eng and eng == "pe":
            return
        key = (eng, sid)
        if self.waited.get(key, 0) >= val:
            return
        self.waited[key] = val
        self.engs[eng].wait_ge(sem, val)

    def deps(self, eng, reads, writes):
        for b in reads:
            for ev in b.w.values():
                self._wait(eng, ev)
        for b in writes:
            if b.multi:
                continue
            for ev in b.w.values():
                if getattr(b, "par", False) and ev[3] == "dma":
                    continue
                self._wait(eng, ev)
            for ev in b.rs.values():
                self._wait(eng, ev)

    def _record(self, ev, key, reads, writes):
        for b in reads:
            b.rs[key] = ev
        for b in writes:
            if b.multi or getattr(b, "par", False):
                b.w[key] = ev
            else:
                b.w = {key: ev}
                b.rs = {}

    def op(self, eng, fn, reads=(), writes=()):
        self.deps(eng, reads, writes)
        if self.cnt[eng] >= 30000:
            self._newsem(eng)
        self.cnt[eng] += 1
        sem, sid = self.sem[eng]
        ev = (sem, sid, self.cnt[eng], eng, None)
        fn(self.engs[eng]).then_inc(sem, 1)
        self.last[eng] = ev
        self._record(ev, eng, reads, writes)
        return ev

    def mm(self, fns, reads=(), writes=()):
        self.deps("pe", reads, writes)
        for f in fns[:-1]:
            f(self.nc.tensor)
        return self.op("pe", fns[-1], reads, writes)

    def dma(self, q, out, in_, reads=(), writes=(), owner=None, **kw):
        self.deps(q, reads, writes)
        b = owner
        if b.dsem is None:
            b.dsem = (self._mksem("d"), self.nsem)
            self.dbufs.append(b)
        b.dcnt += 1
        sem, sid = b.dsem
        ev = (sem, sid, 16 * b.dcnt, "dma", b)
        self.engs[q].dma_start(out=out, in_=in_, **kw).then_inc(sem, 16)
        self._record(ev, ("dma", sid), reads, writes)
        return ev

    def idma(self, out, out_offset, in_, in_offset, reads=(), writes=(), owner=None, **kw):
        q = "pool"
        self.deps(q, reads, writes)
        b = owner
        if b.dsem is None:
            b.dsem = (self._mksem("d"), self.nsem)
            self.dbufs.append(b)
        b.dcnt += 1
        sem, sid = b.dsem
        ev = (sem, sid, 16 * b.dcnt, "dma", b)
        self.nc.gpsimd.indirect_dma_start(out=out, out_offset=out_offset, in_=in_, in_offset=in_offset, **kw).then_inc(sem, 16)
        self._record(ev, ("dma", sid), reads, writes)
        return ev

    def barrier(self):
        evs = list(self.last.values())
        for e in self.engs:
            for ev in evs:
                self._wait(e, ev)
            for b in self.dbufs:
                self._wait(e, (b.dsem[0], b.dsem[1], 0, "dma", b))


class Ring:
    def __init__(self, aps, name):
        self.items = [(ap, Buf(f"{name}{i}")) for i, ap in enumerate(aps)]
        self.i = 0

    def next(self):
        it = self.items[self.i % len(self.items)]
        self.i += 1
        return it


def build(S):
    NT = S // 128
    NSB = S // 512
    nc = bass.Bass("TRN2", target_bir_lowering=False)

    def din(name, shape, dt=F32):
        return nc.dram_tensor(name, shape, dt, kind="ExternalInput").ap()

    def dscr(name, shape, dt=BF16):
        return nc.dram_tensor(name, shape, dt, kind="Internal").ap()

    x = din("x", [S, D])
    pos = din("pos", [S], I32)
    attn_norm_g = din("attn_norm_g", [D])
    w_in = din("w_in", [D, IN_COLS])
    b_gate = din("b_gate", [2 * D])
    da_q_norm_g = din("da_q_norm_g", [64])
    da_k_norm_g = din("da_k_norm_g", [64])
    lq1 = din("da_lambda_q1", [64])
    lk1 = din("da_lambda_k1", [64])
    lq2 = din("da_lambda_q2", [64])
    lk2 = din("da_lambda_k2", [64])
    subln_g = din("da_subln_g", [128])
    q_lora_g = din("mla_q_lora_g", [384])
    w_uq = din("mla_w_uq", [384, 1536])
    kv_lora_g = din("mla_kv_lora_g", [256])
    w_ukv = din("mla_w_ukv", [256, 2048])
    q_norm_g = din("mla_q_norm_g", [192])
    kn_norm_g = din("mla_k_nope_norm_g", [128])
    kr_norm_g = din("mla_k_rope_norm_g", [64])
    w_o = din("w_o", [D, D])
    ffn_norm_g = din("ffn_norm_g", [D])
    w_group = din("w_group", [D, 4])
    b_group = din("b_group", [4])
    w_router = din("w_router", [D, 32])
    b_router = din("b_router", [32])
    w1 = din("w1", [NE, D, DE])
    w3 = din("w3", [NE, D, DE])
    w2 = din("w2", [NE, DE, D])
    cst = din("cst", [128, 7, 128])
    cinv = din("cinv", [128, 2])
    out = nc.dram_tensor("out", [S, D], F32, kind="ExternalOutput").ap()

    s_qda = dscr("s_qda", [8, 128, S])
    s_kda = dscr("s_kda", [8, 128, S])
    s_vda = dscr("s_vda", [S, D])
    s_qn = dscr("s_qn", [8, 128, S])
    s_qr = dscr("s_qr", [8, 64, S])
    s_kn = dscr("s_kn", [8, 128, S])
    s_kr = dscr("s_kr", [64, S])
    s_vm = dscr("s_vm", [S, D])
    s_gate = dscr("s_gate", [16, 128, S])
    NCH = 2 * NT + NE
    cmoe = din("cmoe", [128, 1 + NCH])
    xs = dscr("s_xs", [NCH * 128, D])
    ys = dscr("s_ys", [NCH * 128, D], F32)
    B_xs = Buf("xs", multi=True)
    B_ys = Buf("ys", multi=True)
    B_scr = Buf("scratch", multi=True)
    B_out = Buf("out", multi=True)

    with contextlib.ExitStack() as stack:
        T = Tracker(nc, stack)

        def sb_(name, shape, dt, st=None):
            return (st or stack).enter_context(nc.sbuf_tensor(name, shape, dt))

        ps = stack.enter_context(nc.psum_tensor("ps", [128, 8, 512], F32))
        PB = [Buf(f"psb{i}") for i in range(8)]

        cb = sb_("cb", [128, 7, 128], BF16)
        B_cb = Buf("cb")
        T.dma("pool", cb[:], cst, writes=[B_cb], owner=B_cb)
        identf = sb_("identf", [128, 128], F32)
        B_cf = Buf("cf")
        T.dma("sp", identf[:], cst[:, 0, :], writes=[B_cf], owner=B_cf)
        IDENT, NEGM, B64, RDA, RMLA, ONES, USTRICT = (cb[:, i, :] for i in range(7))
        wt12 = sb_("wt12", [128, NT, 2], F32)
        B_wt = Buf("wt12", multi=True)
        dest = sb_("dest", [128, 2, NT], I32)
        idx2 = sb_("idx2", [128, 2, NCH], I32)
        B_dest = Buf("dest", multi=True)
        cols = sb_("cols", [128, 64], F32)
        B_cols = Buf("cols")
        ev0 = T.op("dve", lambda e: e.memset(cols[:], 0.0), writes=[B_cols])
        T._wait("sp", ev0)
        ci = [0]

        def col_load(src, n, p0=0):
            c = ci[0]
            ci[0] += 1
            T.dma("sp", cols[p0:p0 + n, c:c + 1], src.rearrange("(p o) -> p o", o=1), writes=[B_cols], owner=B_cols)
            return c

        B_cols.multi = True
        c_inv = ci[0]
        ci[0] += 2
        T.dma("sp", cols[:, c_inv:c_inv + 2], cinv, writes=[B_cols], owner=B_cols)
        c_gA = ci[0]
        for c in range(8):
            col_load(attn_norm_g[c * 128:(c + 1) * 128], 128)
        c_gF = ci[0]
        for c in range(8):
            col_load(ffn_norm_g[c * 128:(c + 1) * 128], 128)
        c_bg = ci[0]
        for c in range(16):
            col_load(b_gate[c * 128:(c + 1) * 128], 128)
        c_gq = col_load(da_q_norm_g, 64)
        ci[0] -= 1
        col_load(da_q_norm_g, 64, 64)
        c_gk = col_load(da_k_norm_g, 64)
        ci[0] -= 1
        col_load(da_k_norm_g, 64, 64)
        c_sub = col_load(subln_g, 128)
        c_gql = ci[0]
        for c in range(3):
            col_load(q_lora_g[c * 128:(c + 1) * 128], 128)
        c_gkvl = ci[0]
        for c in range(2):
            col_load(kv_lora_g[c * 128:(c + 1) * 128], 128)
        c_gqn = col_load(q_norm_g[0:128], 128)
        c_gqr = col_load(q_norm_g[128:192], 64)
        c_gkn = col_load(kn_norm_g, 128)
        c_gkr = col_load(kr_norm_g, 64)
        c_eps = ci[0]
        ci[0] += 1
        c_lam = ci[0]
        ci[0] += 4
        B_cols.multi = False
        T.op("dve", lambda e: e.memset(cols[:, c_eps:c_eps + 1], EPS), reads=[B_cols], writes=[B_cols])

        def col(c, rows=128, p0=0):
            return cols[p0:p0 + rows, c:c + 1]

        lam_t = sb_("lam_t", [128, 4, 64], F32)
        B_lam = Buf("lam", multi=True)
        for i, src in enumerate((lq1, lk1, lq2, lk2)):
            T.dma("sp", lam_t[:, i, :], src.partition_broadcast(128), writes=[B_lam], owner=B_lam)
        B_lam.multi = False
        T.op("dve", lambda e: e.tensor_tensor(out=lam_t[:, 0, :], in0=lam_t[:, 0, :], in1=lam_t[:, 1, :], op=ALU.mult),
             reads=[B_lam], writes=[B_lam])
        T.op("dve", lambda e: e.tensor_tensor(out=lam_t[:, 2, :], in0=lam_t[:, 2, :], in1=lam_t[:, 3, :], op=ALU.mult),
             reads=[B_lam], writes=[B_lam])
        T.op("dve", lambda e: e.tensor_reduce(out=cols[:, c_lam:c_lam + 1], in_=lam_t[:, 0, :], axis=AX.X, op=ALU.add),
             reads=[B_lam], writes=[B_cols])
        T.op("dve", lambda e: e.tensor_reduce(out=cols[:, c_lam + 1:c_lam + 2], in_=lam_t[:, 2, :], axis=AX.X, op=ALU.add),
             reads=[B_lam, B_cols], writes=[B_cols])
        T.op("act", lambda e: e.activation(out=cols[:, c_lam:c_lam + 2], in_=cols[:, c_lam:c_lam + 2], func=AF.Exp),
             reads=[B_cols], writes=[B_cols])
        T.op("dve", lambda e: e.scalar_tensor_tensor(out=cols[:, c_lam + 2:c_lam + 3], in0=cols[:, c_lam + 1:c_lam + 2],
                                                      scalar=-LAMBDA_INIT, in1=cols[:, c_lam:c_lam + 1],
                                                      op0=ALU.add, op1=ALU.subtract), reads=[B_cols], writes=[B_cols])
        c_neglam = c_lam + 2
        T.op("dve", lambda e: e.tensor_scalar(out=cols[:, c_lam + 3:c_lam + 4], in0=cols[:, c_sub:c_sub + 1],
                                               scalar1=1.0 - LAMBDA_INIT, scalar2=None, op0=ALU.mult),
             reads=[B_cols], writes=[B_cols])
        c_subs = c_lam + 3
        rg = sb_("rg", [128, 4, 128], BF16)
        B_rg = Buf("rg")
        for i, (src, c) in enumerate(((RDA, c_gq), (RDA, c_gk), (RMLA, c_gqr), (RMLA, c_gkr))):
            T.op("dve", lambda e, i=i, src=src, c=c: e.tensor_scalar(out=rg[:, i, :], in0=src, scalar1=col(c), scalar2=None,
                                                                     op0=ALU.mult), reads=[B_cb, B_cols], writes=[B_rg])
        RGQ, RGK, RGMQ, RGKR = (rg[:, i, :] for i in range(4))
        CONST = [B_cb, B_cols, B_rg]
        zt = sb_("zt", [128, D], BF16)
        B_zt = Buf("zt")
        T.op("dve", lambda e: e.memset(zt[:], 0.0), writes=[B_zt])
        for c in range(NCH):
            T.dma("sp", xs[c * 128:(c + 1) * 128, :], zt[:], reads=[B_zt], writes=[B_xs], owner=B_zt)

        def bank(i, rows=128, c0=0, c1=512):
            return ps[0:rows, i, c0:c1]

        with contextlib.ExitStack() as ph:
            win = sb_("win", [128, 8, IN_COLS], BF16, ph)
            B_win = Buf("win", multi=True)
            w_in_v = w_in.rearrange("(c p) n -> p c n", p=128)
            for kc in range(8):
                for j in range(0, IN_COLS, 1456):
                    T.dma("pool", win[:, kc, j:j + 1456], w_in_v[:, kc, j:j + 1456], writes=[B_win], owner=B_win)
            wuq = sb_("wuq", [128, 3, 1536], BF16, ph)
            T.dma("pool", wuq[:], w_uq.rearrange("(c p) n -> p c n", p=128), writes=[B_win], owner=B_win)
            wkn = sb_("wkn", [128, 2, 8, 128], BF16, ph)
            wv = sb_("wv", [128, 2, 8, 128], BF16, ph)
            ukv_v = w_ukv.rearrange("(c p) (h t d) -> p c h t d", p=128, t=2, d=128)
            for c in range(2):
                T.dma("pool", wkn[:, c, :, :], ukv_v[:, c, :, 0, :], writes=[B_win], owner=B_win)
                T.dma("pool", wv[:, c, :, :], ukv_v[:, c, :, 1, :], writes=[B_win], owner=B_win)
            hT = sb_("hT", [128, 8, 512], BF16, ph)
            B_hT = Buf("hT")
            xt_r = Ring([sb_(f"xt{i}", [128, D], F32, ph)[:] for i in range(2)], "xt")
            junk = sb_("junk", [128, D], BF16, ph)
            B_junk = Buf("junk")
            xn_r = Ring([sb_(f"xn{i}", [128, D], BF16, ph)[:] for i in range(2)], "xn")
            st1 = sb_("st1", [128, 8], F32, ph)
            st_r = Ring([st1[:, i:i + 1] for i in range(8)], "st")
            posi = sb_("posi", [128, 512], I32, ph)
            B_posi = Buf("posi")
            posf = sb_("posf", [128, 512], F32, ph)
            B_posf = Buf("posf")
            tabs = sb_("tabs", [128, 4, 512], F32, ph)
            B_tab = [Buf(f"tab{i}") for i in range(4)]
            SIND, COSD, SINM, COSM = (tabs[:, i, :] for i in range(4))
            cqn = sb_("cqn", [128, 3, 512], BF16, ph)
            B_cqn = Buf("cqn")
            ckvn = sb_("ckvn", [128, 2, 512], BF16, ph)
            B_ckvn = Buf("ckvn")
            f_r = Ring([sb_(f"f{i}", [128, 512], F32, ph)[:] for i in range(8)], "f")
            h_r = Ring([sb_(f"h{i}", [128, 512], BF16, ph)[:] for i in range(8)], "h")
            o_r = Ring([sb_(f"o{i}", [128, 512], BF16, ph)[:] for i in range(4)], "o")
            ov_r = Ring([sb_(f"ov{i}", [128, D], BF16, ph)[:] for i in range(2)], "ov")
            pa = Ring([0, 1, 2], "pa")
            pa.items = [(i, PB[i]) for i in (0, 1, 2, 6)]
            px = Ring([3, 4, 5], "px")
            px.items = [(i, PB[i]) for i in (3, 4, 5)]
            pt = Ring([6, 7], "pt")
            pt.items = [(i, PB[i]) for i in (7,)]

            def make_rstd(ssq_ap, B_ssq, n, rows):
                sd, B_sd = f_r.next()
                T.op("act", lambda e: e.activation(out=sd[0:rows], in_=ssq_ap, func=AF.Sqrt, scale=1.0 / n,
                                                    bias=col(c_eps, rows)), reads=[B_ssq, B_cols], writes=[B_sd])
                rs, B_rs = f_r.next()
                T.op("dve", lambda e: e.reciprocal(out=rs[0:rows], in_=sd[0:rows]), reads=[B_sd], writes=[B_rs])
                return rs, B_rs

            def square(ps_ap, B_ps, rows):
                sq, B_sq = h_r.next()
                T.op("act", lambda e: e.activation(out=sq[0:rows], in_=ps_ap, func=AF.Square), reads=[B_ps], writes=[B_sq])
                return sq, B_sq

            def store(q, dst, src_ap, B_src):
                T.dma(q, dst, src_ap, reads=[B_src], writes=[B_scr], owner=B_src)

            def rope_finish(rows, ps_ap, B_ps, gc, rgmat, cos, sin, B_cos, B_sin, rs, B_rs, dst):
                cbf, B_cbf = h_r.next()
                T.op("act", lambda e: e.activation(out=cbf[0:rows], in_=ps_ap, func=AF.Copy), reads=[B_ps], writes=[B_cbf])
                bi, B_b = px.next()
                T.mm([lambda e: e.matmul(bank(bi, rows), lhsT=rgmat[0:rows, 0:rows], rhs=cbf[0:rows], start=True, stop=True)],
                     reads=[B_cbf, B_rg], writes=[B_b])
                t1, B_t1 = f_r.next()
                T.op("dve", lambda e: e.scalar_tensor_tensor(out=t1[0:rows], in0=ps_ap, scalar=col(gc, rows), in1=cos[0:rows],
                                                              op0=ALU.mult, op1=ALU.mult), reads=[B_ps, B_cols, B_cos], writes=[B_t1])
                t2, B_t2 = f_r.next()
                T.op("dve", lambda e: e.tensor_tensor(out=t2[0:rows], in0=bank(bi, rows), in1=sin[0:rows], op=ALU.mult),
                     reads=[B_b, B_sin], writes=[B_t2])
                T.op("pool", lambda e: e.tensor_tensor(out=t1[0:rows], in0=t1[0:rows], in1=t2[0:rows], op=ALU.add),
                     reads=[B_t2, B_t1], writes=[B_t1])
                ob, B_ob = o_r.next()
                T.op("pool", lambda e: e.tensor_tensor(out=ob[0:rows], in0=t1[0:rows], in1=rs[0:rows], op=ALU.mult),
                     reads=[B_t1, B_rs], writes=[B_ob])
                store("sp", dst, ob[0:rows], B_ob)

            pre = {}

            def prefetch_sb(sbn):
                tn = sbn * 512
                T.dma("sp", posi[:], pos[tn:tn + 512].partition_broadcast(128), writes=[B_posi], owner=B_posi)
                pre["posi"] = sbn
                pre["xt"] = []
                for tt_ in range(2):
                    xt_, B_xt_ = xt_r.next()
                    T.dma("sp", xt_, x[tn + tt_ * 128:tn + (tt_ + 1) * 128, :], writes=[B_xt_], owner=B_xt_)
                    pre["xt"].append((xt_, B_xt_))

            for sb in range(NSB):
                t0 = sb * 512
                if pre.get("posi") != sb:
                    prefetch_sb(sb)
                T.op("dve", lambda e: e.tensor_copy(out=posf[:], in_=posi[:]), reads=[B_posi], writes=[B_posf])
                for ti, (cc, off) in enumerate(((0, 0.0), (0, 0.25), (1, 0.0), (1, 0.25))):
                    v, B_v = f_r.next()
                    T.op("dve", lambda e: e.tensor_scalar(out=v, in0=posf[:], scalar1=col(c_inv + cc), scalar2=off,
                                                           op0=ALU.mult, op1=ALU.add), reads=[B_posf, B_cols], writes=[B_v])
                    k1, B_k1 = f_r.next()
                    T.op("dve", lambda e: e.tensor_scalar(out=k1, in0=v, scalar1=MAGIC, scalar2=None, op0=ALU.add),
                         reads=[B_v], writes=[B_k1])
                    T.op("dve", lambda e: e.tensor_scalar(out=k1, in0=k1, scalar1=-MAGIC, scalar2=None, op0=ALU.add),
                         reads=[B_k1], writes=[B_k1])
                    T.op("dve", lambda e: e.tensor_tensor(out=v, in0=v, in1=k1, op=ALU.subtract), reads=[B_v, B_k1], writes=[B_v])
                    T.op("act", lambda e: e.activation(out=tabs[:, ti, :], in_=v, func=AF.Sin, scale=2.0 * math.pi),
                         reads=[B_v], writes=[B_tab[ti]])
                for tt in range(4):
                    r0 = t0 + tt * 128
                    if tt < 2:
                        xt, B_xt = pre["xt"][tt]
                    else:
                        xt, B_xt = xt_r.next()
                        T.dma("sp", xt, x[r0:r0 + 128, :], writes=[B_xt], owner=B_xt)
                    ss, B_ss = st_r.next()
                    T.op("act", lambda e: e.activation(out=junk[:], in_=xt, func=AF.Square, accum_out=ss),
                         reads=[B_xt], writes=[B_junk, B_ss])
                    T.op("act", lambda e: e.activation(out=ss, in_=ss, func=AF.Sqrt, scale=1.0 / D, bias=col(c_eps)),
                         reads=[B_ss, B_cols], writes=[B_ss])
                    T.op("dve", lambda e: e.reciprocal(out=ss, in_=ss), reads=[B_ss], writes=[B_ss])
                    xn, B_xn = xn_r.next()
                    T.op("dve", lambda e: e.tensor_scalar(out=xn, in0=xt, scalar1=ss, scalar2=None, op0=ALU.mult),
                         reads=[B_xt, B_ss], writes=[B_xn])
                    bi, B_b = pt.next()
                    pT = ps[:, bi, :].bitcast(BF16).rearrange("p (c t) -> p c t", t=128)
                    T.mm([(lambda e, c=c: e.transpose(pT[:, c, :], xn[:, c * 128:(c + 1) * 128], IDENT)) for c in range(8)],
                         reads=[B_xn, B_cb], writes=[B_b])
                    T.op("dve", lambda e: e.tensor_tensor(out=hT[:, :, tt * 128:(tt + 1) * 128], in0=pT,
                                                           in1=cols[:, c_gA:c_gA + 8].unsqueeze(2).broadcast_to([128, 8, 128]),
                                                           op=ALU.mult), reads=[B_b, B_cols], writes=[B_hT])

                def proj(bi, B_b, wt, c0, ncols, rhs, nk, extra_reads):
                    T.mm([(lambda e, kc=kc: e.matmul(bank(bi, ncols), lhsT=wt[:, kc, c0:c0 + ncols], rhs=rhs[:, kc, :],
                                                     start=(kc == 0), stop=(kc == nk - 1))) for kc in range(nk)],
                         reads=[B_win] + extra_reads, writes=[B_b])

                jobs = []

                def add(front, back):
                    jobs.append((front, back))

                def lat_job(ncs, cbase, gbase, dstt, B_dst):
                    stt_ = {}

                    def front():
                        banks = []
                        for c in range(ncs):
                            bi, B_b = pa.next()
                            proj(bi, B_b, win, cbase + c * 128, 128, hT, 8, [B_hT])
                            banks.append((bi, B_b))
                        stt_["banks"] = banks
                        stt_["sqs"] = [square(bank(bi), B_b, 128) for (bi, B_b) in banks]

                    def back():
                        banks, sqs = stt_["banks"], stt_["sqs"]
                        si, B_s = px.next()
                        T.mm([(lambda e, c=c: e.matmul(bank(si), lhsT=ONES, rhs=sqs[c][0], start=(c == 0), stop=(c == ncs - 1)))
                              for c in range(ncs)], reads=[B_cb] + [q_[1] for q_ in sqs], writes=[B_s])
                        rs, B_rs = make_rstd(bank(si), B_s, 128 * ncs, 128)
                        for c in range(ncs):
                            bi, B_b = banks[c]
                            T.op("dve", lambda e, c=c, bi=bi: e.scalar_tensor_tensor(out=dstt[:, c, :], in0=bank(bi), scalar=col(gbase + c),
                                                                                      in1=rs, op0=ALU.mult, op1=ALU.mult),
                                 reads=[B_b, B_cols, B_rs], writes=[B_dst])
                    add(front, back)

                lat_job(3, C_CQ, c_gql, cqn, B_cqn)

                def kr_job():
                    stt_ = {}

                    def front():
                        bi, B_b = pa.next()
                        proj(bi, B_b, win, C_KR, 64, hT, 8, [B_hT])
                        stt_["b"] = (bi, B_b)
                        stt_["sq"] = square(bank(bi, 64), B_b, 64)

                    def back():
                        bi, B_b = stt_["b"]
                        sq, B_sq = stt_["sq"]
                        si, B_s = px.next()
                        T.mm([lambda e: e.matmul(bank(si, 64), lhsT=cb[0:64, 5, 0:64], rhs=sq[0:64], start=True, stop=True)],
                             reads=[B_sq, B_cb], writes=[B_s])
                        rs, B_rs = make_rstd(bank(si, 64), B_s, 64, 64)
                        rope_finish(64, bank(bi, 64), B_b, c_gkr, RGKR, COSM, SINM, B_tab[3], B_tab[2], rs, B_rs, s_kr[:, t0:t0 + 512])
                    add(front, back)

                kr_job()
                lat_job(2, C_CKV, c_gkvl, ckvn, B_ckvn)

                def da_job(typ, h):
                    stt_ = {}

                    def front():
                        bi, B_b = pa.next()
                        proj(bi, B_b, win, typ * 1024 + h * 128, 128, hT, 8, [B_hT])
                        stt_["b"] = (bi, B_b)
                        stt_["sq"] = square(bank(bi), B_b, 128)

                    def back():
                        bi, B_b = stt_["b"]
                        sq, B_sq = stt_["sq"]
                        si, B_s = px.next()
                        T.mm([lambda e: e.matmul(bank(si), lhsT=B64, rhs=sq, start=True, stop=True)], reads=[B_sq, B_cb], writes=[B_s])
                        rs, B_rs = make_rstd(bank(si), B_s, 64, 128)
                        dst = (s_qda, s_kda)[typ][h, :, t0:t0 + 512]
                        rope_finish(128, bank(bi), B_b, (c_gq, c_gk)[typ], (RGQ, RGK)[typ], COSD, SIND, B_tab[1], B_tab[0], rs, B_rs, dst)
                    add(front, back)

                for typ in range(2):
                    for h in range(8):
                        da_job(typ, h)

                def mq_job(h):
                    stt_ = {}

                    def front():
                        ai, B_a = pa.next()
                        proj(ai, B_a, wuq, h * 192, 128, cqn, 3, [B_cqn])
                        bi2, B_b2 = pa.next()
                        proj(bi2, B_b2, wuq, h * 192 + 128, 64, cqn, 3, [B_cqn])
                        stt_["a"] = (ai, B_a)
                        stt_["b"] = (bi2, B_b2)
                        stt_["sqa"] = square(bank(ai), B_a, 128)
                        stt_["sqb"] = square(bank(bi2, 64), B_b2, 64)

                    def back():
                        ai, B_a = stt_["a"]
                        bi2, B_b2 = stt_["b"]
                        sqa, B_sqa = stt_["sqa"]
                        sqb, B_sqb = stt_["sqb"]
                        si, B_s = px.next()
                        T.mm([lambda e: e.matmul(bank(si), lhsT=ONES, rhs=sqa, start=True, stop=False),
                              lambda e: e.matmul(bank(si), lhsT=cb[0:64, 5, :], rhs=sqb[0:64], start=False, stop=True)],
                             reads=[B_cb, B_sqa, B_sqb], writes=[B_s])
                        rs, B_rs = make_rstd(bank(si), B_s, 192, 128)
                        ob, B_ob = o_r.next()
                        T.op("dve", lambda e: e.scalar_tensor_tensor(out=ob, in0=bank(ai), scalar=col(c_gqn), in1=rs,
                                                                      op0=ALU.mult, op1=ALU.mult), reads=[B_a, B_cols, B_rs], writes=[B_ob])
                        store("sp", s_qn[h, :, t0:t0 + 512], ob, B_ob)
                        rope_finish(64, bank(bi2, 64), B_b2, c_gqr, RGMQ, COSM, SINM, B_tab[3], B_tab[2], rs, B_rs, s_qr[h, :, t0:t0 + 512])
                    add(front, back)

                for h in range(8):
                    mq_job(h)

                def kn_job(h):
                    stt_ = {}

                    def front():
                        bi, B_b = pa.next()
                        T.mm([(lambda e, c=c: e.matmul(bank(bi), lhsT=wkn[:, c, h, :], rhs=ckvn[:, c, :], start=(c == 0), stop=(c == 1)))
                              for c in range(2)], reads=[B_win, B_ckvn], writes=[B_b])
                        stt_["b"] = (bi, B_b)
                        stt_["sq"] = square(bank(bi), B_b, 128)

                    def back():
                        bi, B_b = stt_["b"]
                        sq, B_sq = stt_["sq"]
                        si, B_s = px.next()
                        T.mm([lambda e: e.matmul(bank(si), lhsT=ONES, rhs=sq, start=True, stop=True)], reads=[B_sq, B_cb], writes=[B_s])
                        rs, B_rs = make_rstd(bank(si), B_s, 128, 128)
                        ob, B_ob = o_r.next()
                        T.op("dve", lambda e: e.scalar_tensor_tensor(out=ob, in0=bank(bi), scalar=col(c_gkn), in1=rs,
                                                                      op0=ALU.mult, op1=ALU.mult), reads=[B_b, B_cols, B_rs], writes=[B_ob])
                        store("sp", s_kn[h, :, t0:t0 + 512], ob, B_ob)
                    add(front, back)

                for h in range(8):
                    kn_job(h)

                def v_job(tt, half, kind, ovh):
                    stt_ = {}

                    def front():
                        bi, B_b = pa.next()
                        if kind == 0:
                            T.mm([(lambda e, kc=kc: e.matmul(bank(bi), lhsT=hT[:, kc, tt * 128:(tt + 1) * 128],
                                                             rhs=win[:, kc, 2048 + half * 512:2048 + (half + 1) * 512],
                                                             start=(kc == 0), stop=(kc == 7))) for kc in range(8)],
                                 reads=[B_win, B_hT], writes=[B_b])
                        else:
                            T.mm([(lambda e, c=c: e.matmul(bank(bi), lhsT=ckvn[:, c, tt * 128:(tt + 1) * 128],
                                                           rhs=wv[:, c, half * 4:(half + 1) * 4, :].rearrange("p h d -> p (h d)"),
                                                           start=(c == 0), stop=(c == 1))) for c in range(2)],
                                 reads=[B_win, B_ckvn], writes=[B_b])
                        stt_["b"] = (bi, B_b)

                    def back():
                        bi, B_b = stt_["b"]
                        if half == 0:
                            ovh["ov"] = ov_r.next()
                        ov, B_ov = ovh["ov"]
                        T.op("act", lambda e: e.activation(out=ov[:, half * 512:(half + 1) * 512], in_=bank(bi), func=AF.Copy),
                             reads=[B_b], writes=[B_ov])
                        if half == 1:
                            dstt = (s_vda, s_vm)[kind]
                            store("sp", dstt[t0 + tt * 128:t0 + (tt + 1) * 128, :], ov, B_ov)
                    add(front, back)

                for kind in range(2):
                    for tt in range(4):
                        ovh = {}
                        for half in range(2):
                            v_job(tt, half, kind, ovh)

                def gate_job(c):
                    stt_ = {}

                    def front():
                        bi, B_b = pa.next()
                        proj(bi, B_b, win, C_GATE + c * 128, 128, hT, 8, [B_hT])
                        stt_["b"] = (bi, B_b)

                    def back():
                        bi, B_b = stt_["b"]
                        ob, B_ob = o_r.next()
                        T.op("act", lambda e: e.activation(out=ob, in_=bank(bi), func=AF.Sigmoid, bias=col(c_bg + c)),
                             reads=[B_b, B_cols], writes=[B_ob])
                        store("sp", s_gate[c, :, t0:t0 + 512], ob, B_ob)
                    add(front, back)

                for c in range(16):
                    gate_job(c)

                jobs[0][0]()
                for ji in range(len(jobs)):
                    if ji + 1 < len(jobs):
                        jobs[ji + 1][0]()
                    jobs[ji][1]()
                    if ji == 4 and sb + 1 < NSB:
                        prefetch_sb(sb + 1)
            T.barrier()

        with contextlib.ExitStack() as ph:
            mixT = sb_("mixT", [128, 8, S], BF16, ph)
            B_mix = Buf("mix", multi=True)
            with contextlib.ExitStack() as phb:
                kT_r = Ring([sb_(f"kT{i}", [128, S], BF16, phb)[:] for i in range(2)], "kT")
                vD_r = Ring([sb_(f"vD{i}", [128, NT, 128], BF16, phb)[:] for i in range(2)], "vD")
                kn_r = Ring([sb_(f"kn{i}", [128, S], BF16, phb)[:] for i in range(2)], "kn")
                vM_r = Ring([sb_(f"vM{i}", [128, NT, 128], BF16, phb)[:] for i in range(2)], "vM")
                krT = sb_("krT", [128, S], BF16, phb)
                B_kr = Buf("krT")
                evk = T.op("dve", lambda e: e.memset(krT[64:128, :], 0.0), writes=[B_kr])
                B_kr.par = True
                T.dma("sp", krT[0:64, :], s_kr, reads=[B_scr], writes=[B_kr], owner=B_kr)
                q_r = Ring([sb_(f"q{i}", [128, 4, 512], BF16, phb)[:] for i in range(2)], "q")
                for (qap_, B_qq) in q_r.items:
                    T.op("dve", lambda e: e.memset(qap_, 0.0), writes=[B_qq])
                    B_qq.par = True
                g_r = Ring([sb_(f"g{i}", [128, 2, 512], BF16, phb)[:] for i in range(2)], "g")
                p_r = Ring([sb_(f"p{i}", [128, 512], BF16, phb)[:] for i in range(8)], "p")
                f_r = Ring([sb_(f"fb{i}", [128, 512], F32, phb)[:] for i in range(6)], "fb")
                h_r = Ring([sb_(f"hb{i}", [128, 512], BF16, phb)[:] for i in range(2)], "hb")
                psS = Ring([0, 1, 2], "pS")
                psS.items = [(i, PB[i]) for i in (0, 1, 2, 6)]
                OBANK = {"m": (3, PB[3]), "d0": (4, PB[4]), "d1": (5, PB[5])}
                pz = Ring([0], "pz")
                pz.items = [(i, PB[i]) for i in (7,)]
                onesf = sb_("onesf", [128, 128], F32, phb)
                B_onesf = Buf("onesf")
                T.op("dve", lambda e: e.memset(onesf[:], 1.0), writes=[B_onesf])
                pacc_r = {k_: Ring([sb_(f"pacc{k_}{i}", [128, 512], F32, phb)[:] for i in range(3)], "pacc" + k_) for k_ in ("m", "d1")}
                oc_r = {k_: Ring([sb_(f"oc{k_}{i}", [128, 512], F32, phb)[:] for i in range(1)], "oc" + k_) for k_ in ("m", "d0", "d1")}
                ACCENG = {"m": "dve", "d0": "pe", "d1": "pool"}
                LOOK = 3

                def front(jb):
                    kb, c0, j = jb["kb"], jb["c0"], jb["j"]
                    si, B_s = psS.next()
                    fns = []
                    rd = [B_cb]
                    parts = jb["parts"]
                    for pi_, (kap, qap, rows, B_k, B_q) in enumerate(parts):
                        fns.append(lambda e, kap=kap, qap=qap, rows=rows, pi_=pi_: e.matmul(
                            bank(si, 128, c0, 512), lhsT=kap[0:rows, kb * 128:(kb + 1) * 128], rhs=qap[0:rows, c0:512],
                            start=(pi_ == 0), stop=(pi_ == len(parts) - 1 and j < 0)))
                        rd += [B_k, B_q]
                    if j >= 0:
                        fns.append(lambda e: e.matmul(bank(si, 128, c0, c0 + 128), lhsT=IDENT, rhs=NEGM, start=False, stop=True))
                    T.mm(fns, reads=rd, writes=[B_s])
                    pt_, B_p = p_r.next()
                    T.op("act", lambda e: e.activation(out=pt_[:, c0:512], in_=bank(si, 128, c0, 512), func=AF.Exp, scale=jb["scale"]),
                         reads=[B_s], writes=[B_p])
                    jb["pt"], jb["B_p"] = pt_, B_p

                def back(jb):
                    kb, c0, nkb, key = jb["kb"], jb["c0"], jb["nkb"], jb["key"]
                    pt_, B_p = jb["pt"], jb["B_p"]
                    oi, B_o = OBANK[key]
                    g = jb["grp"]
                    T.mm([lambda e: e.matmul(bank(oi, 128, c0, 512), lhsT=jb["vt"][:, kb, :], rhs=pt_[:, c0:512],
                                             start=(kb == 0), stop=(kb == nkb - 1))], reads=[jb["B_v"], B_p], writes=[B_o])
                    if key == "d0":
                        zb, B_zb = OBANK["m"]
                        T.mm([lambda e: e.matmul(bank(zb, 128, c0, 512), lhsT=ONES, rhs=pt_[:, c0:512],
                                                 start=(kb == 0), stop=(kb == nkb - 1))], reads=[B_cb, B_p], writes=[B_zb])
                    else:
                        if kb < 2:
                            g["pacc"][(key, kb)] = pacc_r[key].next()
                        pa_, B_pa = g["pacc"][(key, kb % 2)]
                        if kb < 2:
                            if c0 > 0:
                                T.op(ACCENG[key], lambda e: e.memset(pa_[:, 0:c0], 0.0), writes=[B_pa])
                            T.op(ACCENG[key], lambda e: e.tensor_copy(out=pa_[:, c0:512], in_=pt_[:, c0:512]), reads=[B_p, B_pa], writes=[B_pa])
                        else:
                            T.op(ACCENG[key], lambda e: e.tensor_tensor(out=pa_[:, c0:512], in0=pa_[:, c0:512], in1=pt_[:, c0:512], op=ALU.add),
                                 reads=[B_p, B_pa], writes=[B_pa])
                    if kb == nkb - 1:
                        oc, B_oc = oc_r[key].next()
                        T.op("act", lambda e: e.activation(out=oc, in_=bank(oi), func=AF.Copy), reads=[B_o], writes=[B_oc])
                        if key == "d0":
                            zi, B_z = OBANK["m"]
                        else:
                            zi, B_z = pz.next()
                            pa0, B_pa0 = g["pacc"][(key, 0)]
                            pa1, B_pa1 = g["pacc"][(key, 1)]
                            T.mm([lambda e: e.matmul(bank(zi), lhsT=onesf[:], rhs=pa0, start=True, stop=False),
                                  lambda e: e.matmul(bank(zi), lhsT=onesf[:], rhs=pa1, start=False, stop=True)],
                                 reads=[B_onesf, B_pa0, B_pa1], writes=[B_z])
                        rz, B_rz = f_r.next()
                        T.op("dve", lambda e: e.reciprocal(out=rz, in_=bank(zi)), reads=[B_z], writes=[B_rz])
                        g["fin"][key] = (oc, B_oc, rz, B_rz)
                        if key == "m":
                            fin_mla(g)
                        elif key == "d1":
                            fin_da(g)

                def fin_mla(g):
                    oc, B_oc, rz, B_rz = g["fin"]["m"]
                    gt, B_g = g["gt"], g["B_g"]
                    T.op("dve", lambda e: e.tensor_tensor(out=oc, in0=oc, in1=rz, op=ALU.mult), reads=[B_oc, B_rz], writes=[B_oc])
                    T.op("dve", lambda e: e.tensor_tensor(out=oc, in0=oc, in1=gt[:, 1, :], op=ALU.mult), reads=[B_oc, B_g], writes=[B_oc])

                def fin_da(g):
                    om, B_om = g["fin"]["m"][0], g["fin"]["m"][1]
                    o0, B_o0, r0, B_r0 = g["fin"]["d0"]
                    o1, B_o1, r1, B_r1 = g["fin"]["d1"]
                    gt, B_g = g["gt"], g["B_g"]
                    h, t0 = g["h"], g["t0"]
                    T.op("dve", lambda e: e.tensor_tensor(out=o0, in0=o0, in1=r0, op=ALU.mult), reads=[B_o0, B_r0], writes=[B_o0])
                    T.op("dve", lambda e: e.scalar_tensor_tensor(out=o1, in0=o1, scalar=col(c_neglam), in1=r1,
                                                                  op0=ALU.mult, op1=ALU.mult), reads=[B_o1, B_r1, B_cols], writes=[B_o1])
                    T.op("dve", lambda e: e.tensor_tensor(out=o0, in0=o0, in1=o1, op=ALU.add), reads=[B_o0, B_o1], writes=[B_o0])
                    sq, B_sq = h_r.next()
                    T.op("pool", lambda e: e.tensor_tensor(out=sq, in0=o0, in1=o0, op=ALU.mult), reads=[B_o0], writes=[B_sq])
                    zi, B_z = pz.next()
                    T.mm([lambda e: e.matmul(bank(zi), lhsT=ONES, rhs=sq, start=True, stop=True)], reads=[B_sq, B_cb], writes=[B_z])
                    sd, B_sd = f_r.next()
                    T.op("act", lambda e: e.activation(out=sd, in_=bank(zi), func=AF.Ln, scale=1.0 / 128, bias=col(c_eps)),
                         reads=[B_z, B_cols], writes=[B_sd])
                    T.op("act", lambda e: e.activation(out=sd, in_=sd, func=AF.Exp, scale=-0.5), reads=[B_sd], writes=[B_sd])
                    T.op("dve", lambda e: e.scalar_tensor_tensor(out=o0, in0=o0, scalar=col(c_subs), in1=sd, op0=ALU.mult, op1=ALU.mult),
                         reads=[B_o0, B_sd, B_cols], writes=[B_o0])
                    T.op("dve", lambda e: e.tensor_tensor(out=o0, in0=o0, in1=gt[:, 0, :], op=ALU.mult), reads=[B_o0, B_g], writes=[B_o0])
                    T.op("dve", lambda e: e.tensor_tensor(out=mixT[:, h, t0:t0 + 512], in0=o0, in1=om, op=ALU.add),
                         reads=[B_o0, B_om], writes=[B_mix])

                pending = []

                def push(jb):
                    front(jb)
                    pending.append(jb)
                    if len(pending) > LOOK:
                        back(pending.pop(0))

                for h in range(8):
                    kT, B_kT = kT_r.next()
                    T.dma("sp", kT, s_kda[h], reads=[B_scr], writes=[B_kT], owner=B_kT)
                    vD, B_vD = vD_r.next()
                    T.dma("sp", vD, s_vda[:, h * 128:(h + 1) * 128].rearrange("(n p) d -> p n d", p=128),
                          reads=[B_scr], writes=[B_vD], owner=B_vD)
                    kn, B_kn = kn_r.next()
                    T.dma("sp", kn, s_kn[h], reads=[B_scr], writes=[B_kn], owner=B_kn)
                    vM, B_vM = vM_r.next()
                    T.dma("sp", vM, s_vm[:, h * 128:(h + 1) * 128].rearrange("(n p) d -> p n d", p=128),
                          reads=[B_scr], writes=[B_vM], owner=B_vM)
                    for sbi in range(NSB):
                        t0 = sbi * 512
                        qt, B_q = q_r.next()
                        T.dma("sp", qt[0:64, 0, :], s_qda[h, 0:64, t0:t0 + 512], reads=[B_scr], writes=[B_q], owner=B_q)
                        T.dma("sp", qt[64:128, 3, :], s_qda[h, 64:128, t0:t0 + 512], reads=[B_scr], writes=[B_q], owner=B_q)
                        T.dma("sp", qt[:, 1, :], s_qn[h, :, t0:t0 + 512], reads=[B_scr], writes=[B_q], owner=B_q)
                        T.dma("sp", qt[0:64, 2, :], s_qr[h, :, t0:t0 + 512], reads=[B_scr], writes=[B_q], owner=B_q)
                        gt, B_g = g_r.next()
                        T.dma("sp", gt[:], s_gate.rearrange("(a c) p t -> c p a t", a=2)[h, :, :, t0:t0 + 512], reads=[B_scr], writes=[B_g], owner=B_g)
                        grp = {"pacc": {}, "fin": {}, "gt": gt, "B_g": B_g, "h": h, "t0": t0}
                        nkb = 4 * sbi + 4
                        for kb in range(nkb):
                            j = kb - 4 * sbi
                            push({"kb": kb, "j": j, "c0": max(j, 0) * 128, "nkb": nkb, "key": "m", "grp": grp, "scale": 192 ** -0.5,
                                  "vt": vM, "B_v": B_vM,
                                  "parts": [(kn, qt[:, 1, :], 128, B_kn, B_q), (krT[:], qt[:, 2, :], 128, B_kr, B_q)]})
                        for kb in range(nkb):
                            j = kb - 4 * sbi
                            for m in range(2):
                                push({"kb": kb, "j": j, "c0": max(j, 0) * 128, "nkb": nkb, "key": f"d{m}", "grp": grp, "scale": 0.125,
                                      "vt": vD, "B_v": B_vD,
                                      "parts": [(kT, qt[:, 3 * m, :], 128, B_kT, B_q)]})
                while pending:
                    back(pending.pop(0))
                T.barrier()

            B_mix.multi = False
            with contextlib.ExitStack() as phc:
                wo = sb_("wo", [128, 8, D], BF16, phc)
                B_wo = Buf("wo")
                T.dma("pool", wo[:], w_o.rearrange("(c p) n -> p c n", p=128), writes=[B_wo], owner=B_wo)
                wr = sb_("wr", [128, 8, 36], F32, phc)
                B_wr = Buf("wr", multi=True)
                T.dma("sp", wr[:, :, 0:4], w_group.rearrange("(c p) n -> p c n", p=128), writes=[B_wr], owner=B_wr)
                T.dma("sp", wr[:, :, 4:36], w_router.rearrange("(c p) n -> p c n", p=128), writes=[B_wr], owner=B_wr)
                br = sb_("br", [128, 36], F32, phc)
                T.dma("sp", br[:, 0:4], b_group.partition_broadcast(128), writes=[B_wr], owner=B_wr)
                T.dma("sp", br[:, 4:36], b_router.partition_broadcast(128), writes=[B_wr], owner=B_wr)
                gfr = sb_("gfr", [128, D], F32, phc)
                T.dma("sp", gfr[:], ffn_norm_g.partition_broadcast(128), writes=[B_wr], owner=B_wr)
                cm = sb_("cm", [128, 1 + NCH], F32, phc)
                T.dma("sp", cm[:], cmoe, writes=[B_wr], owner=B_wr)
                h2tok = sb_("h2tok", [128, NT, D], BF16, phc)
                B_h2 = Buf("h2tok", multi=True)
                oh_all = sb_("oh_all", [128, 2, NT, NE], F32, phc)
                p12 = sb_("p12", [128, 2, NT], F32, phc)
                B_plan = Buf("plan", multi=True)
                carry = sb_("carry", [128, NE], F32, phc)
                B_carry = Buf("carry")
                T.op("dve", lambda e: e.memset(carry[:], 0.0), writes=[B_carry])
                xt_r = Ring([sb_(f"cx{i}", [128, D], F32, phc)[:] for i in range(2)], "cx")
                x1_r = Ring([sb_(f"x1{i}", [128, D], F32, phc)[:] for i in range(2)], "x1")
                junk = sb_("junkc", [128, D], BF16, phc)
                B_junk = Buf("junkc")
                hf_r = Ring([sb_(f"hf{i}", [128, 8, 128], F32, phc)[:] for i in range(1)], "hf")
                sm = sb_("sm", [128, 2, 256], F32, phc)
                sm_r = Ring([sm[:, i, :] for i in range(2)], "sm")
                abf_r = Ring([sb_(f"abf{i}", [128, NE], BF16, phc)[:] for i in range(2)], "abf")
                pc = Ring([0, 1], "pc")
                pc.items = [((0, 1), PB[0]), ((2, 3), PB[2])]
                ptc = Ring([0], "ptc")
                ptc.items = [((4, 5), PB[4])]
                prt = (6, PB[6])
                prk = (7, PB[7])
                ctxs = {}

                def partA(t):
                    r0 = t * 128
                    (b0, b1), B_b = pc.next()
                    for half, bi in enumerate((b0, b1)):
                        T.mm([(lambda e, hh=hh: e.matmul(bank(bi), lhsT=mixT[:, hh, r0:r0 + 128], rhs=wo[:, hh, half * 512:(half + 1) * 512],
                                                         start=(hh == 0), stop=(hh == 7))) for hh in range(8)],
                             reads=[B_mix, B_wo], writes=[B_b])
                    xt, B_xt = xt_r.next()
                    T.dma("sp", xt, x[r0:r0 + 128, :], writes=[B_xt], owner=B_xt)
                    x1, B_x1 = x1_r.next()
                    T.op("dve", lambda e: e.tensor_tensor(out=x1.rearrange("p (a n) -> p a n", a=2), in0=ps[:, b0:b0 + 2, :],
                                                           in1=xt.rearrange("p (a n) -> p a n", a=2), op=ALU.add),
                         reads=[B_b, B_xt], writes=[B_x1])
                    T.dma("sp", out[r0:r0 + 128, :], x1, reads=[B_x1], writes=[B_out], owner=B_x1)
                    s_, B_s = sm_r.next()
                    T.op("act", lambda e: e.activation(out=junk[:], in_=x1, func=AF.Square, accum_out=s_[:, 0:1]),
                         reads=[B_x1], writes=[B_junk, B_s])
                    T.op("act", lambda e: e.activation(out=s_[:, 1:2], in_=s_[:, 0:1], func=AF.Sqrt, scale=1.0 / D, bias=col(c_eps)),
                         reads=[B_s, B_cols], writes=[B_s])
                    T.op("dve", lambda e: e.reciprocal(out=s_[:, 2:3], in_=s_[:, 1:2]), reads=[B_s], writes=[B_s])
                    T.op("dve", lambda e: e.tensor_scalar(out=xt, in0=x1, scalar1=s_[:, 2:3], scalar2=None, op0=ALU.mult),
                         reads=[B_x1, B_s], writes=[B_xt])
                    T.op("pool", lambda e: e.tensor_tensor(out=h2tok[:, t, :], in0=xt, in1=gfr[:], op=ALU.mult), reads=[B_xt, B_wr], writes=[B_h2])
                    (tb, _), B_tb = ptc.next()
                    pTf = ps[:, tb:tb + 2, :].rearrange("p a (c t) -> p (a c) t", t=128)
                    T.mm([(lambda e, c=c: e.transpose(pTf[:, c, :], xt[:, c * 128:(c + 1) * 128], identf[:])) for c in range(8)],
                         reads=[B_xt, B_cf], writes=[B_tb])
                    yield
                    hf, B_hf = hf_r.next()
                    T.op("dve", lambda e: e.tensor_tensor(out=hf, in0=pTf, in1=cols[:, c_gF:c_gF + 8].unsqueeze(2).broadcast_to([128, 8, 128]),
                                                           op=ALU.mult), reads=[B_tb, B_cols], writes=[B_hf])
                    T.mm([(lambda e, c=c: e.matmul(bank(prt[0], 128, 0, 36), lhsT=hf[:, c, :], rhs=wr[:, c, :], start=(c == 0), stop=(c == 7)))
                          for c in range(8)], reads=[B_hf, B_wr], writes=[prt[1]])
                    def dv(fn):
                        T.op("dve", fn, reads=[B_s], writes=[B_s])
                    yield
                    lg = s_[:, 4:40]
                    T.op("dve", lambda e: e.tensor_tensor(out=lg, in0=bank(prt[0], 128, 0, 36), in1=br[:], op=ALU.add),
                         reads=[prt[1], B_wr, B_s], writes=[B_s])
                    ctxs[t] = (s_, B_s)

                def partB(t):
                    s_, B_s = ctxs.pop(t)

                    def dv(fn):
                        T.op("dve", fn, reads=[B_s], writes=[B_s])
                    gmax = s_[:, 40:41]
                    dv(lambda e: e.tensor_reduce(out=gmax, in_=s_[:, 4:8], axis=AX.X, op=ALU.max))
                    ohg = s_[:, 44:48]
                    dv(lambda e: e.tensor_scalar(out=ohg, in0=s_[:, 4:8], scalar1=gmax, scalar2=None, op0=ALU.is_ge))
                    dv(lambda e: e.tensor_scalar(out=s_[:, 48:52], in0=s_[:, 4:8], scalar1=gmax, scalar2=None, op0=ALU.subtract))
                    T.op("act", lambda e: e.activation(out=s_[:, 48:52], in_=s_[:, 48:52], func=AF.Exp, accum_out=s_[:, 41:42]),
                         reads=[B_s], writes=[B_s])
                    dv(lambda e: e.reciprocal(out=s_[:, 42:43], in_=s_[:, 41:42]))
                    dv(lambda e: e.tensor_scalar(out=s_[:, 52:56], in0=ohg, scalar1=-1.0, scalar2=1.0e4, op0=ALU.add, op1=ALU.mult))
                    me = s_[:, 64:96]
                    dv(lambda e: e.tensor_tensor(out=me.rearrange("p (g k) -> p g k", k=8), in0=s_[:, 8:40].rearrange("p (g k) -> p g k", k=8),
                                                 in1=s_[:, 52:56].unsqueeze(2).broadcast_to([128, 4, 8]), op=ALU.add))
                    m1 = s_[:, 56:57]
                    dv(lambda e: e.tensor_reduce(out=m1, in_=me, axis=AX.X, op=ALU.max))
                    oh1 = oh_all[:, 0, t, :]
                    oh2 = oh_all[:, 1, t, :]
                    T.op("dve", lambda e: e.tensor_scalar(out=oh1, in0=me, scalar1=m1, scalar2=None, op0=ALU.is_ge), reads=[B_s], writes=[B_plan])
                    yield
                    me2 = s_[:, 128:160]
                    T.op("dve", lambda e: e.scalar_tensor_tensor(out=me2, in0=oh1, scalar=-1.0e4, in1=me, op0=ALU.mult, op1=ALU.add),
                         reads=[B_s, B_plan], writes=[B_s])
                    m2 = s_[:, 57:58]
                    dv(lambda e: e.tensor_reduce(out=m2, in_=me2, axis=AX.X, op=ALU.max))
                    T.op("dve", lambda e: e.tensor_scalar(out=oh2, in0=me2, scalar1=m2, scalar2=None, op0=ALU.is_ge), reads=[B_s], writes=[B_plan])
                    dv(lambda e: e.tensor_tensor(out=s_[:, 58:59], in0=m1, in1=m2, op=ALU.subtract))
                    T.op("act", lambda e: e.activation(out=s_[:, 59:60], in_=s_[:, 58:59], func=AF.Sigmoid), reads=[B_s], writes=[B_s])
                    T.op("dve", lambda e: e.tensor_tensor(out=wt12[:, t, 0:1], in0=s_[:, 59:60], in1=s_[:, 42:43], op=ALU.mult), reads=[B_s], writes=[B_wt])
                    T.op("dve", lambda e: e.tensor_tensor(out=wt12[:, t, 1:2], in0=s_[:, 42:43], in1=wt12[:, t, 0:1], op=ALU.subtract),
                         reads=[B_s, B_wt], writes=[B_wt])
                    abf, B_abf = abf_r.next()
                    T.op("dve", lambda e: e.tensor_tensor(out=abf, in0=oh1, in1=oh2, op=ALU.add), reads=[B_plan], writes=[B_abf])
                    yield
                    T.mm([lambda e: e.matmul(bank(prk[0], 128, 0, 32), lhsT=USTRICT, rhs=abf, start=True, stop=True)], reads=[B_abf, B_cb], writes=[prk[1]])
                    T.mm([lambda e: e.matmul(bank(prk[0], 128, 32, 64), lhsT=ONES, rhs=abf, start=True, stop=True)], reads=[B_abf, B_cb], writes=[prk[1]])
                    rk = s_[:, 160:192]
                    T.op("dve", lambda e: e.tensor_tensor(out=rk, in0=bank(prk[0], 128, 0, 32), in1=carry[:], op=ALU.add),
                         reads=[prk[1], B_carry, B_s], writes=[B_s])
                    T.op("dve", lambda e: e.tensor_tensor(out=carry[:], in0=bank(prk[0], 128, 32, 64), in1=carry[:], op=ALU.add),
                         reads=[prk[1], B_carry, B_s], writes=[B_carry])
                    for k_ in range(2):
                        T.op("dve", lambda e: e.tensor_tensor(out=s_[:, 192:224], in0=oh_all[:, k_, t, :], in1=rk, op=ALU.mult),
                             reads=[B_s, B_plan], writes=[B_s])
                        T.op("dve", lambda e: e.tensor_reduce(out=p12[:, k_, t:t + 1], in_=s_[:, 192:224], axis=AX.X, op=ALU.add),
                             reads=[B_s], writes=[B_plan])

                def run(g):
                    try:
                        next(g)
                        return True
                    except StopIteration:
                        return False

                gA = partA(0)
                while run(gA):
                    pass
                gB_prev = None
                for t in range(NT):
                    gA = partA(t + 1) if t + 1 < NT else None
                    gB = partB(t)
                    if gA is not None:
                        run(gA)
                    run(gB)
                    if gA is not None:
                        run(gA)
                    run(gB)
                    if gA is not None:
                        while run(gA):
                            pass
                    if gB_prev is not None:
                        while run(gB_prev):
                            pass
                    gB_prev = gB
                while run(gB_prev):
                    pass
                B_plan.multi = False
                B_wt.multi = False
                g1 = sb_("gplan", [128, 8, NE], F32, phc)
                B_g1 = Buf("g1")
                def gp(fn, extra=()):
                    T.op("dve", fn, reads=[B_g1, B_carry, B_plan, B_wr] + list(extra), writes=[B_g1])
                gp(lambda e: e.tensor_scalar(out=g1[:, 0, :], in0=carry[:], scalar1=63.5, scalar2=1.0 / 128, op0=ALU.add, op1=ALU.mult))
                gp(lambda e: e.tensor_scalar(out=g1[:, 0, :], in0=g1[:, 0, :], scalar1=MAGIC, scalar2=None, op0=ALU.add))
                gp(lambda e: e.tensor_scalar(out=g1[:, 0, :], in0=g1[:, 0, :], scalar1=-MAGIC, scalar2=128.0, op0=ALU.add, op1=ALU.mult))
                gp(lambda e: e.tensor_copy(out=g1[:, 1, :], in_=g1[:, 0, :]))
                src_, dst_ = 1, 2
                for k_ in (1, 2, 4, 8, 16):
                    gp(lambda e: e.tensor_copy(out=g1[:, dst_, 0:k_], in_=g1[:, src_, 0:k_]))
                    gp(lambda e: e.tensor_tensor(out=g1[:, dst_, k_:NE], in0=g1[:, src_, k_:NE], in1=g1[:, src_, 0:NE - k_], op=ALU.add))
                    src_, dst_ = dst_, src_
                pends = g1[:, src_, :]
                pstart = g1[:, 3, :]
                gp(lambda e: e.tensor_tensor(out=pstart, in0=pends, in1=g1[:, 0, :], op=ALU.subtract))
                big = sb_("bigt", [128, max(NCH, NT) * NE], F32, phc)
                destf = sb_("destf", [128, 2, NT], F32, phc)
                for k_ in range(2):
                    gp(lambda e: e.tensor_tensor(out=big[:, 0:NT * NE].rearrange("p (t n) -> p t n", n=NE), in0=oh_all[:, k_, :, :],
                                                 in1=pstart.unsqueeze(1).broadcast_to([128, NT, NE]), op=ALU.mult))
                    gp(lambda e: e.tensor_reduce(out=destf[:, k_, :], in_=big[:, 0:NT * NE].rearrange("p (t n) -> p t n", n=NE), axis=AX.X, op=ALU.add))
                    gp(lambda e: e.tensor_tensor(out=destf[:, k_, :], in0=destf[:, k_, :], in1=p12[:, k_, :], op=ALU.add))
                    T.op("dve", lambda e: e.tensor_copy(out=dest[:, k_, :], in_=destf[:, k_, :]), reads=[B_g1], writes=[B_dest])
                gp(lambda e: e.tensor_tensor(out=big[:, 0:NCH * NE].rearrange("p (c n) -> p c n", n=NE),
                                             in0=pends.unsqueeze(1).broadcast_to([128, NCH, NE]),
                                             in1=cm[:, 1:1 + NCH].unsqueeze(2).broadcast_to([128, NCH, NE]), op=ALU.is_le))
                cef = sb_("cef", [128, 4, NCH], F32, phc)
                gp(lambda e: e.tensor_reduce(out=cef[:, 0, :], in_=big[:, 0:NCH * NE].rearrange("p (c n) -> p c n", n=NE), axis=AX.X, op=ALU.add))
                gp(lambda e: e.tensor_scalar(out=cef[:, 0, :], in0=cef[:, 0, :], scalar1=float(NE - 1), scalar2=None, op0=ALU.min))
                gp(lambda e: e.memset(cef[:, 1, :], 0.0))
                gp(lambda e: e.tensor_tensor(out=cef[:, 1, 1:NCH], in0=cef[:, 0, 1:NCH], in1=cef[:, 0, 0:NCH - 1], op=ALU.is_equal))
                gp(lambda e: e.memset(cef[:, 1, NCH // 2:NCH // 2 + 1], 0.0))
                gp(lambda e: e.tensor_scalar(out=cef[:, 2, :], in0=cef[:, 0, :], scalar1=256.0, scalar2=None, op0=ALU.mult))
                gp(lambda e: e.scalar_tensor_tensor(out=cef[:, 2, :], in0=cef[:, 1, :], scalar=16384.0, in1=cef[:, 2, :], op0=ALU.mult, op1=ALU.add))
                for hh in range(2):
                    T.op("dve", lambda e: e.tensor_scalar(out=idx2[:, hh, :], in0=cef[:, 2, :], scalar1=cm[:, 0:1], scalar2=float(hh), op0=ALU.add, op1=ALU.add),
                         reads=[B_g1, B_wr], writes=[B_dest])
                dreg = nc.gpsimd.to_reg(NCH * 128 - 1)
                B_h2.multi = False
                for t in range(NT):
                    for k_ in range(2):
                        T.idma(out=xs, out_offset=bass.IndirectOffsetOnAxis(ap=dest[:, k_, t:t + 1], axis=0), in_=h2tok[:, t, :], in_offset=None,
                               reads=[B_h2, B_dest], writes=[B_xs], owner=B_h2, bounds_check=dreg, oob_is_err=False)
                T.barrier()

        with contextlib.ExitStack() as phd:
            w1v = w1.rearrange("e (p h j) n -> (e p h) (j n)", p=128, h=2, j=4)
            w3v = w3.rearrange("e (p h j) n -> (e p h) (j n)", p=128, h=2, j=4)
            w2v = w2.rearrange("e (p h j) n -> (e p h) (j n)", p=128, h=2, j=2)
            HCH = NCH // 2
            wbts = [sb_(f"wbt{i}", [128, 3, 4096], BF16, phd) for i in range(2)]
            BWS = [[Buf(f"W{j}_{i}") for i in range(3)] for j in range(2)]
            for j_ in range(2):
                T.op("dve", lambda e: e.memset(wbts[j_][:], 0.0), writes=BWS[j_])
                for b_ in BWS[j_]:
                    b_.par = True
            seq = []
            for i_ in range(HCH):
                seq += [i_, HCH + i_]

            def strm(c):
                return 0 if c < HCH else 1
            xc_r = Ring([sb_(f"xc{i}", [128, D], BF16, phd)[:] for i in range(4)], "xc")
            xT_r = Ring([sb_(f"xT{i}", [128, 8, 128], BF16, phd)[:] for i in range(2)], "xT")
            sl_r = Ring([sb_(f"sl{i}", [128, 512], F32, phd)[:] for i in range(2)], "sl")
            G_r = Ring([sb_(f"G{i}", [128, 512], BF16, phd)[:] for i in range(2)], "G")
            GT_r = Ring([sb_(f"GT{i}", [128, 4, 128], BF16, phd)[:] for i in range(2)], "GT")
            yo_r = Ring([sb_(f"yo{i}", [128, D], F32, phd)[:] for i in range(2)], "yo")
            PXT = ((0, 1), PB[0])
            PH1 = (2, PB[2])
            PH3 = (3, PB[3])
            PGT = (4, PB[4])
            PY = ((5, 6), PB[5])
            st1 = {}

            xcs = {}

            def xload(cn):
                xc_, B_xc_ = xc_r.next()
                T.dma("sp", xc_, xs[cn * 128:(cn + 1) * 128, :], reads=[B_xs], writes=[B_xc_], owner=B_xc_)
                xcs[cn] = (xc_, B_xc_)

            xload(seq[0])
            xload(seq[1])

            def stage1(c, nxt2):
                if nxt2 is not None:
                    xload(nxt2)
                xc, B_xc = xcs.pop(c)
                wb = wbts[strm(c)]
                BW = BWS[strm(c)]
                (x0, x1b), B_px = PXT
                pxt = ps[:, x0:x0 + 2, :].rearrange("p a (c t) -> p (a c) t", t=128)
                xcv = xc.rearrange("t (p j) -> t j p", j=8)
                T.mm([(lambda e, jj=jj: e.matmul(pxt[:, jj, :], lhsT=xcv[:, jj, :], rhs=IDENT, start=True, stop=True)) for jj in range(8)],
                     reads=[B_xc, B_cb], writes=[B_px])
                xT, B_xT = xT_r.next()
                T.op("act", lambda e: e.activation(out=xT, in_=pxt, func=AF.Copy), reads=[B_px], writes=[B_xT])
                w1b = wb[:, 0, :].rearrange("p (j n) -> p j n", n=512)
                w3b = wb[:, 1, :].rearrange("p (j n) -> p j n", n=512)
                T.mm([(lambda e, jj=jj: e.matmul(bank(PH1[0]), lhsT=xT[:, jj, :], rhs=w1b[:, jj, :], start=(jj == 0), stop=(jj == 7))) for jj in range(8)],
                     reads=[B_xT, BW[0]], writes=[PH1[1]])
                T.mm([(lambda e, jj=jj: e.matmul(bank(PH3[0]), lhsT=xT[:, jj, :], rhs=w3b[:, jj, :], start=(jj == 0), stop=(jj == 7))) for jj in range(8)],
                     reads=[B_xT, BW[1]], writes=[PH3[1]])
                sl, B_sl = sl_r.next()
                T.op("act", lambda e: e.activation(out=sl, in_=bank(PH1[0]), func=AF.Silu), reads=[PH1[1]], writes=[B_sl])
                G, B_G = G_r.next()
                T.op("dve", lambda e: e.tensor_tensor(out=G, in0=bank(PH3[0]), in1=sl, op=ALU.mult), reads=[PH3[1], B_sl], writes=[B_G])
                st1[c] = (G, B_G, wb, None)

            breg = nc.gpsimd.to_reg(NE * 256 - 1)

            def gathers(c, which):
                wbt = wbts[strm(c)]
                BW = BWS[strm(c)]
                for mi, wv_ in enumerate((w1v, w3v, w2v)):
                    if (mi == 2) != (which == 2):
                        continue
                    for hh in range(2):
                        T.idma(out=wbt[:, mi, hh * 2048:(hh + 1) * 2048], out_offset=None, in_=wv_,
                               in_offset=bass.IndirectOffsetOnAxis(ap=idx2[:, hh, c:c + 1], axis=0),
                               reads=[B_dest], writes=[BW[mi]], owner=BW[mi], bounds_check=breg, oob_is_err=False)

            def stage2(c):
                G, B_G, wb, B_w = st1.pop(c)
                BW = BWS[strm(c)]
                Gv = G.rearrange("t (p j) -> t j p", j=4)
                pgt = ps[:, PGT[0], :].rearrange("p (c t) -> p c t", t=128)
                T.mm([(lambda e, jj=jj: e.matmul(pgt[:, jj, :], lhsT=Gv[:, jj, :], rhs=IDENT, start=True, stop=True)) for jj in range(4)],
                     reads=[B_G, B_cb], writes=[PGT[1]])
                GT, B_GT = GT_r.next()
                T.op("act", lambda e: e.activation(out=GT, in_=pgt, func=AF.Copy), reads=[PGT[1]], writes=[B_GT])
                w2b = wb[:, 2, :].rearrange("p (j n) -> p j n", n=1024)
                (y0, y1b), B_py = PY
                for half, bi in enumerate((y0, y1b)):
                    T.mm([(lambda e, jj=jj: e.matmul(bank(bi), lhsT=GT[:, jj, :], rhs=w2b[:, jj, half * 512:(half + 1) * 512], start=(jj == 0), stop=(jj == 3)))
                          for jj in range(4)], reads=[B_GT, BW[2]], writes=[B_py])
                yo, B_yo = yo_r.next()
                T.op("dve", lambda e: e.tensor_copy(out=yo.rearrange("p (a n) -> p a n", a=2), in_=ps[:, y0:y0 + 2, :]), reads=[B_py], writes=[B_yo])
                T.dma("sp", ys[c * 128:(c + 1) * 128, :], yo, reads=[B_yo], writes=[B_ys], owner=B_yo)

            for c0_ in (0, HCH):
                gathers(c0_, 1)
                gathers(c0_, 2)

            def nxt(c):
                return c + 1 if (c + 1) % HCH != 0 else None

            for i_ in range(NCH + 1):
                if i_ < NCH:
                    c = seq[i_]
                    stage1(c, seq[i_ + 2] if i_ + 2 < NCH else None)
                    if nxt(c) is not None:
                        gathers(nxt(c), 1)
                if i_ >= 1:
                    p_ = seq[i_ - 1]
                    stage2(p_)
                    if nxt(p_) is not None:
                        gathers(nxt(p_), 2)
            T.barrier()
            dreg2 = nc.gpsimd.to_reg(NCH * 128 - 1)
            y_r = Ring([sb_(f"yg{i}", [128, 2, D], F32, phd)[:] for i in range(2)], "yg")
            xo_r = Ring([sb_(f"xo{i}", [128, D], F32, phd)[:] for i in range(3)], "xo")
            exo = {}

            def eload(tn):
                xo_, B_xo_ = xo_r.next()
                T.dma("sp", xo_, out[tn * 128:(tn + 1) * 128, :], reads=[B_out], writes=[B_xo_], owner=B_xo_)
                exo[tn] = (xo_, B_xo_)

            eload(0)
            for t in range(NT):
                r0 = t * 128
                yg, B_yg = y_r.next()
                B_yg.par = True
                for k_ in range(2):
                    T.idma(out=yg[:, k_, :], out_offset=None, in_=ys, in_offset=bass.IndirectOffsetOnAxis(ap=dest[:, k_, t:t + 1], axis=0),
                           reads=[B_ys, B_dest], writes=[B_yg], owner=B_yg, bounds_check=dreg2, oob_is_err=False)
                if t + 1 < NT:
                    eload(t + 1)
                xo, B_xo = exo.pop(t)
                for k_ in range(2):
                    T.op("dve", lambda e: e.scalar_tensor_tensor(out=xo, in0=yg[:, k_, :], scalar=wt12[:, t, k_:k_ + 1], in1=xo, op0=ALU.mult, op1=ALU.add),
                         reads=[B_yg, B_xo, B_wt], writes=[B_xo])
                T.dma("sp", out[r0:r0 + 128, :], xo, reads=[B_xo], writes=[B_out], owner=B_xo)
            T.barrier()
    return nc


def _consts():
    cst = np.zeros((128, 7, 128), np.float32)
    cst[:, 0, :] = np.eye(128, dtype=np.float32)
    k = np.arange(128)[:, None]
    q = np.arange(128)[None, :]
    cst[:, 1, :] = np.where(k > q, NEG, 0.0)
    cst[:, 2, :] = ((k // 64) == (q // 64)).astype(np.float32)
    for m in range(2):
        for d in range(8):
            cst[m * 64 + d + 8, 3, m * 64 + d] = -1.0
            cst[m * 64 + d, 3, m * 64 + d + 8] = 1.0
    for d in range(32):
        cst[d + 32, 4, d] = -1.0
        cst[d, 4, d + 32] = 1.0
    cst[:, 5, :] = 1.0
    cst[:, 6, :] = (k < q).astype(np.float32)
    cinv = np.zeros((128, 2), np.float32)
    for p in range(128):
        d = p % 64
        if d < 16:
            cinv[p, 0] = THETA ** (-(2.0 * (d % 8)) / 16.0) / (2.0 * math.pi)
        if p < 64:
            cinv[p, 1] = THETA ** (-(2.0 * (p % 32)) / 64.0) / (2.0 * math.pi)
    return cst, cinv


_NC_CACHE = {}


def kernel(**inputs):
    x = np.asarray(inputs["x"])
    Bn, S, _ = x.shape
    if S not in _NC_CACHE:
        _NC_CACHE[S] = build(S)
    nc = _NC_CACHE[S]
    cst, cinv = _consts()
    shared = {}
    for k_, v in inputs.items():
        if k_ in ("x", "positions"):
            continue
        a = np.ascontiguousarray(np.asarray(v))
        shared[k_] = a.reshape(a.shape[1:]) if a.shape[0] == 1 else a
    shared["b_gate"] = shared["b_gate"].reshape(-1)
    shared["cst"] = cst
    shared["cinv"] = cinv
    nch = 2 * (S // 128) + NE
    cmoe = np.zeros((128, 1 + nch), np.float32)
    cmoe[:, 0] = 2.0 * np.arange(128)
    cmoe[:, 1:] = 128.0 * np.arange(nch)[None, :]
    shared["cmoe"] = cmoe
    positions = np.asarray(inputs["positions"]).astype(np.int32)
    in_maps = []
    for b in range(Bn):
        m = dict(shared)
        m["x"] = np.ascontiguousarray(x[b])
        m["pos"] = np.ascontiguousarray(positions[b])
        in_maps.append(m)
    res = run_bass_kernel_spmd(nc, in_maps, core_ids=list(range(Bn)))
    return np.stack([np.asarray(r["out"]) for r in res.results], axis=0).astype(np.float32)
```
